# Optimizing a Trainium2 kernel written in Bass

```python
import jax, jax.numpy as jnp
from jax import lax
import numpy as np

D_MODEL = 1024
BATCH = 8
SEQ = 4096
DEPTH = 1

A_HEADS = 8
A_KV_HEADS = 2
A_HEAD_DIM = 64
IDX_HEADS = 16
IDX_DIM = 64
IDX_TOPK_MAX = 256
Q_BLOCK = 128
B_HEADS = 8
B_KEY_DIM = 64
B_VAL_DIM = 64
CONV_WIDTH = 4
CHUNK = 64
ROPE_THETA = 500000.0
ROPE_FRACTION_DEN = 4
PEER_HEADS = 8
PEER_KEY_DIM = 128
PEER_N_KEYS = 128
PEER_N_EXPERTS = PEER_N_KEYS * PEER_N_KEYS
PEER_TOPK = 16
PEER_TOKEN_BLOCK = 128
EPS = 1e-6

A_WIDTH = A_HEADS * A_HEAD_DIM
KV_WIDTH = A_KV_HEADS * A_HEAD_DIM
B_QK_WIDTH = B_HEADS * B_KEY_DIM
B_V_WIDTH = B_HEADS * B_VAL_DIM
CONV_CHANNELS = 2 * B_QK_WIDTH + B_V_WIDTH
IN_SPLITS = (A_WIDTH, KV_WIDTH, KV_WIDTH, IDX_HEADS * IDX_DIM, IDX_DIM, IDX_HEADS,
             B_QK_WIDTH, B_QK_WIDTH, B_V_WIDTH, B_V_WIDTH, B_HEADS, B_HEADS, D_MODEL, D_MODEL)
IN_WIDTH = sum(IN_SPLITS)

kernel_name = 'hybrid_dsa_gdn_peer_adaln'


def rms_norm(x, gain=None):
    xf = x.astype(jnp.float32)
    y = xf * lax.rsqrt(jnp.mean(xf * xf, axis=-1, keepdims=True) + EPS)
    if gain is not None:
        y = y * gain.astype(jnp.float32)
    return y.astype(x.dtype)


def l2_normalize(x):
    return x * lax.rsqrt(jnp.sum(x * x, axis=-1, keepdims=True) + EPS)


def partial_rotary(x, positions):
    hd = x.shape[-1]
    rd = hd // ROPE_FRACTION_DEN
    half = rd // 2
    inv_freq = jnp.power(jnp.float32(ROPE_THETA), -jnp.arange(half, dtype=jnp.float32) * (2.0 / rd))
    ang = positions.astype(jnp.float32)[..., None] * inv_freq
    cos = jnp.cos(ang)[:, :, None, :].astype(x.dtype)
    sin = jnp.sin(ang)[:, :, None, :].astype(x.dtype)
    x1 = x[..., :half]
    x2 = x[..., half:rd]
    return jnp.concatenate([x1 * cos - x2 * sin, x2 * cos + x1 * sin, x[..., rd:]], axis=-1)


def causal_short_conv(x, w):
    width = w.shape[0]
    seq = x.shape[1]
    xp = jnp.pad(x, ((0, 0), (width - 1, 0), (0, 0)))
    out = xp[:, 0:seq] * w[0]
    for i in range(1, width):
        out = out + xp[:, i:i + seq] * w[i]
    return out


def dsa_attention(q, k, v, q_idx, k_idx, w_idx):
    bsz, seq, _, dh = q.shape
    topk = min(IDX_TOPK_MAX, seq // 4)
    n_blocks = seq // Q_BLOCK
    rep = A_HEADS // A_KV_HEADS
    idx_scale = (IDX_HEADS ** -0.5) * (IDX_DIM ** -0.5)
    spos = jnp.arange(seq)

    def block(blk):
        t0 = blk * Q_BLOCK
        q_b = lax.dynamic_slice_in_dim(q, t0, Q_BLOCK, axis=1)
        qi_b = lax.dynamic_slice_in_dim(q_idx, t0, Q_BLOCK, axis=1)
        wi_b = lax.dynamic_slice_in_dim(w_idx, t0, Q_BLOCK, axis=1).astype(jnp.float32) * idx_scale
        tpos = t0 + jnp.arange(Q_BLOCK)
        logits = jnp.einsum('bthd,bsd->bths', qi_b, k_idx).astype(jnp.float32)
        score = jnp.einsum('bth,bths->bts', wi_b, jax.nn.relu(logits))
        causal = spos[None, :] <= tpos[:, None]
        score = jnp.where(causal[None], score, -jnp.inf)
        _, sel = lax.top_k(score, topk)
        valid = sel <= tpos[None, :, None]
        k_sel = jax.vmap(lambda kb_, ib: kb_[ib])(k, sel)
        v_sel = jax.vmap(lambda vb_, ib: vb_[ib])(v, sel)
        qg = q_b.reshape(bsz, Q_BLOCK, A_KV_HEADS, rep, dh)
        s = jnp.einsum('btgrd,btkgd->btgrk', qg, k_sel).astype(jnp.float32) * (dh ** -0.5)
        s = jnp.where(valid[:, :, None, None, :], s, -jnp.inf)
        p = jax.nn.softmax(s, axis=-1).astype(v.dtype)
        o = jnp.einsum('btgrk,btkgd->btgrd', p, v_sel)
        return o.reshape(bsz, Q_BLOCK, A_HEADS * dh)

    out = lax.map(block, jnp.arange(n_blocks))
    return jnp.moveaxis(out, 0, 1).reshape(bsz, seq, A_HEADS * dh)


def gated_delta_rule(q, k, v, g, beta):
    bsz, seq, nh, dk = q.shape
    dv = v.shape[-1]
    n = seq // CHUNK

    def chunks(t):
        t = jnp.moveaxis(t, 2, 1)
        return t.reshape(t.shape[:2] + (n, CHUNK) + t.shape[3:])

    q = chunks(q * (dk ** -0.5))
    k = chunks(k)
    v = chunks(v)
    g = chunks(g)
    beta = chunks(beta)
    gc = jnp.cumsum(g, axis=-1)
    pos = jnp.arange(CHUNK)
    incl = pos[:, None] >= pos[None, :]
    strict = pos[:, None] > pos[None, :]
    decay = jnp.exp(jnp.where(incl, gc[..., :, None] - gc[..., None, :], -jnp.inf))
    kb = k * beta[..., None]
    vb = v * beta[..., None]
    lower = jnp.where(strict, jnp.einsum('bhncd,bhnjd->bhncj', kb, k) * decay, 0.0)
    eye = jnp.eye(CHUNK, dtype=q.dtype)
    tmat = lax.linalg.triangular_solve(eye + lower, jnp.broadcast_to(eye, lower.shape),
                                       left_side=True, lower=True, unit_diagonal=True)
    u = tmat @ vb
    kcd = tmat @ (kb * jnp.exp(gc)[..., None])
    intra = jnp.einsum('bhncd,bhnjd->bhncj', q, k) * decay

    def step(state, xs):
        qc, kc, uc, kcdc, gcc, intrac = xs
        v_new = uc - kcdc @ state
        out = (qc * jnp.exp(gcc)[..., None]) @ state + intrac @ v_new
        glast = gcc[..., -1]
        state = state * jnp.exp(glast)[..., None, None] + jnp.einsum(
            'bhcd,bhce->bhde', kc * jnp.exp(glast[..., None] - gcc)[..., None], v_new)
        return state, out

    xs = tuple(jnp.moveaxis(t, 2, 0) for t in (q, k, u, kcd, gc, intra))
    state0 = jnp.zeros((bsz, nh, dk, dv), q.dtype)
    _, out = lax.scan(step, state0, xs)
    out = jnp.moveaxis(out, 0, 2).reshape(bsz, nh, seq, dv)
    return jnp.moveaxis(out, 1, 2)


def token_mixers(n, positions, w_in, conv_w, a_log, dt_bias, norm_b_w, w_pa, w_pb, w_o):
    bsz, seq, _ = n.shape
    proj = n @ w_in
    offs = np.cumsum(IN_SPLITS)[:-1].tolist()
    (qa, ka, va, qi, ki, wi, qb, kb, vb, zb, bb, ab, gate_a, gate_b) = jnp.split(proj, offs, axis=-1)

    qa = partial_rotary(qa.reshape(bsz, seq, A_HEADS, A_HEAD_DIM), positions)
    ka = partial_rotary(ka.reshape(bsz, seq, A_KV_HEADS, A_HEAD_DIM), positions)
    va = va.reshape(bsz, seq, A_KV_HEADS, A_HEAD_DIM)
    qi = partial_rotary(qi.reshape(bsz, seq, IDX_HEADS, IDX_DIM), positions)
    ki = partial_rotary(ki.reshape(bsz, seq, 1, IDX_DIM), positions)[:, :, 0]
    o_a = dsa_attention(qa, ka, va, qi, ki, wi)

    qkv = jax.nn.silu(causal_short_conv(jnp.concatenate([qb, kb, vb], axis=-1), conv_w))
    qb, kb, vb = jnp.split(qkv, [B_QK_WIDTH, 2 * B_QK_WIDTH], axis=-1)
    qb = l2_normalize(qb.reshape(bsz, seq, B_HEADS, B_KEY_DIM).astype(jnp.float32))
    kb = l2_normalize(kb.reshape(bsz, seq, B_HEADS, B_KEY_DIM).astype(jnp.float32))
    vb = vb.reshape(bsz, seq, B_HEADS, B_VAL_DIM).astype(jnp.float32)
    beta = jax.nn.sigmoid(bb.astype(jnp.float32))
    g = -jnp.exp(a_log.astype(jnp.float32)) * jax.nn.softplus(ab.astype(jnp.float32) + dt_bias.astype(jnp.float32))
    o_b = gated_delta_rule(qb, kb, vb, g, beta)
    z = jax.nn.silu(zb.reshape(bsz, seq, B_HEADS, B_VAL_DIM).astype(jnp.float32))
    o_b = (rms_norm(o_b, norm_b_w) * z).astype(n.dtype).reshape(bsz, seq, B_V_WIDTH)

    merged = jax.nn.sigmoid(gate_a) * (o_a @ w_pa) + jax.nn.sigmoid(gate_b) * (o_b @ w_pb)
    return merged @ w_o


def peer_channel_mixer(xn, wq, subkeys, u_tab, v_tab):
    bsz, seq, d = xn.shape
    kk = PEER_TOPK
    q = (xn @ wq).reshape(bsz, seq, PEER_HEADS, 2, PEER_KEY_DIM // 2)
    s = jnp.einsum('bshpd,hpnd->bshpn', q, subkeys).astype(jnp.float32)
    v1, i1 = lax.top_k(s[..., 0, :], kk)
    v2, i2 = lax.top_k(s[..., 1, :], kk)
    cand = (v1[..., :, None] + v2[..., None, :]).reshape(bsz, seq, PEER_HEADS, kk * kk)
    vals, ci = lax.top_k(cand, kk)
    e1 = jnp.take_along_axis(i1, ci // kk, axis=-1)
    e2 = jnp.take_along_axis(i2, ci % kk, axis=-1)
    experts = e1 * PEER_N_KEYS + e2
    gates = jax.nn.softmax(vals, axis=-1).astype(xn.dtype)
    n_tok = bsz * seq
    nb = n_tok // PEER_TOKEN_BLOCK
    xs = (xn.reshape(nb, PEER_TOKEN_BLOCK, d),
          experts.reshape(nb, PEER_TOKEN_BLOCK, PEER_HEADS, kk),
          gates.reshape(nb, PEER_TOKEN_BLOCK, PEER_HEADS, kk))

    def block(args):
        xb, eb, gb = args
        a = jnp.einsum('td,thkd->thk', xb, u_tab[eb])
        coef = jax.nn.gelu(a, approximate=False) * gb
        return jnp.einsum('thk,thkd->td', coef, v_tab[eb])

    out = lax.map(block, xs)
    return out.reshape(bsz, seq, d)


def setup_inputs(seed: int = 0) -> dict:
    key = jax.random.key(seed)
    ks = jax.random.split(key, 20)
    f32 = jnp.float32
    x = jax.random.normal(ks[0], (BATCH, SEQ, D_MODEL), f32)
    c = jax.random.normal(ks[1], (BATCH, D_MODEL), f32)
    offset = jax.random.randint(ks[2], (BATCH, 1), 0, SEQ)
    positions = (offset + jnp.arange(SEQ)[None, :]).astype(jnp.int32)
    w_ada = jax.random.normal(ks[3], (DEPTH, D_MODEL, 6 * D_MODEL), f32) * D_MODEL ** -0.5
    b_ada = 0.02 * jax.random.normal(ks[4], (DEPTH, 6 * D_MODEL), f32)
    w_in = jax.random.normal(ks[5], (DEPTH, D_MODEL, IN_WIDTH), f32) * D_MODEL ** -0.5
    conv_w = jax.random.normal(ks[6], (DEPTH, CONV_WIDTH, CONV_CHANNELS), f32) * CONV_WIDTH ** -0.5
    a_log = jnp.log(jax.random.uniform(ks[7], (DEPTH, B_HEADS), f32, minval=1.0, maxval=16.0))
    dt = jnp.exp(jax.random.uniform(ks[8], (DEPTH, B_HEADS), f32, minval=math_log(1e-3), maxval=math_log(1e-1)))
    dt_bias = dt + jnp.log(-jnp.expm1(-dt))
    norm_b_w = 1.0 + 0.02 * jax.random.normal(ks[9], (DEPTH, B_VAL_DIM), f32)
    w_pa = jax.random.normal(ks[10], (DEPTH, A_WIDTH, D_MODEL), f32) * A_WIDTH ** -0.5
    w_pb = jax.random.normal(ks[11], (DEPTH, B_V_WIDTH, D_MODEL), f32) * B_V_WIDTH ** -0.5
    w_o = jax.random.normal(ks[12], (DEPTH, D_MODEL, D_MODEL), f32) * D_MODEL ** -0.5
    peer_wq = jax.random.normal(ks[13], (DEPTH, D_MODEL, PEER_HEADS * PEER_KEY_DIM), f32) * D_MODEL ** -0.5
    peer_subkeys = jax.random.normal(ks[14], (DEPTH, PEER_HEADS, 2, PEER_N_KEYS, PEER_KEY_DIM // 2), f32) * (PEER_KEY_DIM // 2) ** -0.5
    peer_u = jax.random.normal(ks[15], (DEPTH, PEER_N_EXPERTS, D_MODEL), f32) * D_MODEL ** -0.5
    peer_v = jax.random.normal(ks[16], (DEPTH, PEER_N_EXPERTS, D_MODEL), f32) * PEER_HEADS ** -0.5
    final_norm_w = 1.0 + 0.02 * jax.random.normal(ks[17], (D_MODEL,), f32)
    return {'x': x, 'c': c, 'positions': positions, 'w_ada': w_ada, 'b_ada': b_ada, 'w_in': w_in,
            'conv_w': conv_w, 'a_log': a_log, 'dt_bias': dt_bias, 'norm_b_w': norm_b_w,
            'w_pa': w_pa, 'w_pb': w_pb, 'w_o': w_o, 'peer_wq': peer_wq, 'peer_subkeys': peer_subkeys,
            'peer_u': peer_u, 'peer_v': peer_v, 'final_norm_w': final_norm_w}


def math_log(v):
    return float(np.log(v))


def reference(x, c, positions, w_ada, b_ada, w_in, conv_w, a_log, dt_bias, norm_b_w,
              w_pa, w_pb, w_o, peer_wq, peer_subkeys, peer_u, peer_v, final_norm_w):
    h = x
    c_act = jax.nn.silu(c)
    for layer in range(DEPTH):
        mod = c_act @ w_ada[layer] + b_ada[layer]
        sh1, sc1, gt1, sh2, sc2, gt2 = jnp.split(mod, 6, axis=-1)
        n1 = rms_norm(h) * (1.0 + sc1[:, None, :]) + sh1[:, None, :]
        y1 = token_mixers(n1, positions, w_in[layer], conv_w[layer], a_log[layer], dt_bias[layer],
                          norm_b_w[layer], w_pa[layer], w_pb[layer], w_o[layer])
        h = h + gt1[:, None, :] * y1
        n2 = rms_norm(h) * (1.0 + sc2[:, None, :]) + sh2[:, None, :]
        y2 = peer_channel_mixer(n2, peer_wq[layer], peer_subkeys[layer], peer_u[layer], peer_v[layer])
        h = h + gt2[:, None, :] * y2
    return rms_norm(h, final_norm_w)
```

```python
import numpy as np
from contextlib import ExitStack
import concourse.bass as bass
import concourse.mybir as mybir
from concourse.bass_utils import run_bass_kernel_spmd

F32 = mybir.dt.float32
BF16 = mybir.dt.bfloat16
I32 = mybir.dt.int32
F32R = mybir.dt.float32r
U32 = mybir.dt.uint32
AF = mybir.ActivationFunctionType
ALU = mybir.AluOpType
AX = mybir.AxisListType

D = 1024
EPS = 1e-6
NEG = -1.0e30


class Buf:
    def __init__(self, name):
        self.name = name
        self.w = []
        self.r = []
        self.dsem = None


class _Rec:
    def __getattr__(self, name):
        return lambda *a, **kw: (name, a, kw)


_REC = _Rec()


class Sched:
    def __init__(self, nc, stack):
        self.nc = nc
        self.stack = stack
        self.eng = {"pe": nc.tensor, "act": nc.scalar, "dve": nc.vector, "pool": nc.gpsimd, "sp": nc.sync}
        self.sems = {}
        self.cnt = {}
        for e in self.eng:
            self.sems["e_" + e] = stack.enter_context(nc.semaphore("sem_" + e))
            self.cnt["e_" + e] = 0
        self.seen = {e: {} for e in self.eng}
        self.prog = {e: [] for e in self.eng}
        self.ndsem = 0
        self.n_ins = 0

    def dma_sem(self, buf):
        if buf.dsem is None:
            key = "d_%d" % self.ndsem
            self.ndsem += 1
            self.sems[key] = self.stack.enter_context(self.nc.semaphore("dsem_%d" % (self.ndsem - 1)))
            self.cnt[key] = 0
            buf.dsem = key
        return buf.dsem

    def _waits(self, e, deps):
        best = {}
        for (k, v) in deps:
            if e == "pe" and k == "e_pe":
                continue
            if best.get(k, 0) < v:
                best[k] = v
        out = []
        for k, v in best.items():
            if self.seen[e].get(k, 0) < v:
                self.seen[e][k] = v
                out.append((k, v))
        return out

    def op(self, e, fn, reads=(), writes=()):
        deps = []
        for b in reads:
            deps += b.w
        for b in writes:
            deps += b.w + b.r
        waits = self._waits(e, deps)
        key = "e_" + e
        self.cnt[key] += 1
        tok = (key, self.cnt[key])
        self.prog[e].append((waits, fn(_REC), key, 1))
        for b in writes:
            b.w = [tok]
            b.r = []
        for b in reads:
            if b not in writes:
                b.r.append(tok)
        self.n_ins += 1

    def dma(self, fn, sbuf, reads=(), writes=(), q="sp"):
        key = self.dma_sem(sbuf)
        deps = [(key, self.cnt[key])] if self.cnt[key] else []
        for b in reads:
            deps += b.w
        for b in writes:
            deps += b.w + b.r
        waits = self._waits(q, deps)
        self.cnt[key] += 16
        tok = (key, self.cnt[key])
        self.prog[q].append((waits, fn(_REC), key, 16))
        for b in writes:
            b.w = [tok]
            b.r = []
        for b in reads:
            if b not in writes:
                b.r.append(tok)
        self.n_ins += 1

    def barrier(self):
        allk = [(k, v) for k, v in self.cnt.items() if v > 0]
        for e in self.eng:
            waits = self._waits(e, allk)
            if waits:
                self.prog[e].append((waits, None, None, 0))

    def emit(self):
        nc = self.nc
        sems = self.sems
        prog = self.prog
        with nc.Block() as block:
            def run(e, engine):
                for waits, fn, key, inc in prog[e]:
                    if fn is None:
                        for (k, v) in waits:
                            engine.wait_ge(sems[k], v)
                        continue
                    for (k, v) in waits[1:]:
                        engine.wait_ge(sems[k], v)
                    ins = getattr(engine, fn[0])(*fn[1], **fn[2])
                    if waits:
                        ins._wait_ge(sems[waits[0][0]], waits[0][1])
                    ins.then_inc(sems[key], inc)

            @block.tensor
            def _(eng):
                run("pe", eng)

            @block.scalar
            def _(eng):
                run("act", eng)

            @block.vector
            def _(eng):
                run("dve", eng)

            @block.gpsimd
            def _(eng):
                run("pool", eng)

            @block.sync
            def _(eng):
                run("sp", eng)


A_HEADS, KV_HEADS, HD = 8, 2, 64
IDX_HEADS = 16
B_HEADS = 8
OFF_QA = 0
OFF_KA = 512
OFF_VA = 640
OFF_QI = 768
OFF_KI = 1792
OFF_WI = 1856
OFF_QB = 1872
OFF_KB = 2384
OFF_VB = 2896
OFF_ZB = 3408
OFF_BB = 3920
OFF_AB = 3928
OFF_GA = 3936
OFF_GB = 4960
IN_WIDTH = 5984

ROT_STARTS = [OFF_QA + 64 * h for h in range(8)] + [OFF_KA + 64 * g for g in range(2)] + \
             [OFF_QI + 64 * h for h in range(16)] + [OFF_KI]
N_ROT = len(ROT_STARTS)
CONV_STARTS = [OFF_QB + 64 * h for h in range(24)]
SWAP64 = np.concatenate([np.arange(8, 16), np.arange(0, 8), np.arange(16, 64)])


def host_layout(inputs, b, S):
    f = np.float32
    x = np.ascontiguousarray(inputs["x"][b, :S]).astype(f)
    c = inputs["c"][b].astype(f)
    w_in = inputs["w_in"][0].astype(f)
    m = {}
    m["x"] = x
    m["c_col"] = np.ascontiguousarray(c.reshape(8, 128).T)
    m["pos_rep"] = np.ascontiguousarray(np.broadcast_to(inputs["positions"][b, :S].astype(np.int32)[None, :], (64, S)))
    m["w_ada"] = np.ascontiguousarray(inputs["w_ada"][0].astype(f))
    b_ada = inputs["b_ada"][0].astype(f)
    m["b_ada_col"] = np.ascontiguousarray(b_ada.reshape(48, 128).T)
    m["b_ada_rep"] = np.ascontiguousarray(np.broadcast_to(b_ada[None, :], (128, 6144)))
    rot_cols = np.concatenate([np.arange(s, s + 64) for s in ROT_STARTS])
    rot_cols_sw = np.concatenate([s + SWAP64 for s in ROT_STARTS])
    m["w_rot"] = np.ascontiguousarray(w_in[:, rot_cols])
    m["w_rot_sw"] = np.ascontiguousarray(w_in[:, rot_cols_sw])
    m["w_conv"] = np.ascontiguousarray(w_in[:, OFF_QB:OFF_QB + 1536])
    m["w_gate"] = np.ascontiguousarray(w_in[:, OFF_GA:OFF_GA + 2048])
    m["w_tok"] = np.ascontiguousarray(np.concatenate(
        [w_in[:, OFF_VA:OFF_VA + 128], w_in[:, OFF_WI:OFF_WI + 16]], axis=1))
    m["w_z"] = np.ascontiguousarray(w_in[:, OFF_ZB:OFF_ZB + 512])
    m["w_bg"] = np.ascontiguousarray(w_in[:, OFF_BB:OFF_BB + 16])
    conv_w = inputs["conv_w"][0].astype(f)
    m["conv_col"] = np.ascontiguousarray(conv_w.T.reshape(24, 64, 4).transpose(1, 0, 2))
    m["a_log_rep"] = np.ascontiguousarray(np.broadcast_to(inputs["a_log"][0].astype(f)[None, :], (64, 8)))
    m["dt_bias_rep"] = np.ascontiguousarray(np.broadcast_to(inputs["dt_bias"][0].astype(f)[None, :], (64, 8)))
    m["norm_b_rep"] = np.ascontiguousarray(np.broadcast_to(inputs["norm_b_w"][0].astype(f)[None, :], (64, 64)))
    m["w_pa"] = np.ascontiguousarray(inputs["w_pa"][0].astype(f).reshape(8, 64, 1024).transpose(1, 0, 2))
    m["w_pb"] = np.ascontiguousarray(inputs["w_pb"][0].astype(f).reshape(8, 64, 1024).transpose(1, 0, 2))
    m["w_o"] = np.ascontiguousarray(inputs["w_o"][0].astype(f))
    m["peer_wq"] = np.ascontiguousarray(inputs["peer_wq"][0].astype(f))
    sk = inputs["peer_subkeys"][0].astype(f)
    m["subT"] = np.ascontiguousarray(sk.transpose(3, 0, 1, 2).reshape(64, 16, 128))
    m["peer_u"] = np.ascontiguousarray(inputs["peer_u"][0].astype(f))
    m["peer_v"] = np.ascontiguousarray(inputs["peer_v"][0].astype(f))
    m["fnw_rep"] = np.ascontiguousarray(np.broadcast_to(inputs["final_norm_w"].astype(f)[None, :], (128, 1024)))
    m.update(host_consts())
    return m


def host_consts():
    f = np.float32
    k = {}
    k["ident"] = np.eye(128, dtype=f)
    half = 8
    inv_freq = np.power(f(500000.0), -np.arange(half, dtype=f) * f(2.0 / 16)).astype(f)
    fr = np.zeros((64, 1), f)
    sg = np.zeros((64, 1), f)
    fr[0:8, 0] = inv_freq
    fr[8:16, 0] = inv_freq
    sg[0:8, 0] = -1.0
    sg[8:16, 0] = 1.0
    k["freq_col"] = fr
    k["sign_col"] = sg
    p = np.arange(128)
    k["cmask"] = np.where(p[None, :] <= p[:, None], 0.0, NEG).astype(f)
    i = np.arange(64)
    k["ut64"] = (i[:, None] <= i[None, :]).astype(f)
    k["sl64"] = (i[:, None] > i[None, :]).astype(f)
    k["il64"] = (i[:, None] >= i[None, :]).astype(f)
    k["iu64"] = (i[:, None] <= i[None, :]).astype(f)
    k["su64"] = (i[:, None] < i[None, :]).astype(f)
    return k


def build_nc(S, dbg=False, stop_after=None):
    NT = S // 128
    NS = S // 512
    NCH = S // 64
    TOPK = min(256, S // 4)
    nc = bass.Bass("TRN2", target_bir_lowering=False)
    stack = ExitStack()
    K = Sched(nc, stack)

    def din(name, shape, dt=F32):
        return nc.dram_tensor(name, list(shape), dt, kind="ExternalInput").ap()

    def dscr(name, shape, dt=F32):
        return nc.dram_tensor(name, list(shape), dt).ap()

    x_d = din("x", [S, D])
    c_col_d = din("c_col", [128, 8])
    pos_d = din("pos_rep", [64, S], I32)
    w_ada_d = din("w_ada", [D, 6 * D])
    b_ada_col_d = din("b_ada_col", [128, 48])
    b_ada_rep_d = din("b_ada_rep", [128, 6 * D])
    w_rot_d = din("w_rot", [D, N_ROT * 64])
    w_rot_sw_d = din("w_rot_sw", [D, N_ROT * 64])
    w_conv_d = din("w_conv", [D, 1536])
    w_gate_d = din("w_gate", [D, 2048])
    w_tok_d = din("w_tok", [D, 144])
    w_z_d = din("w_z", [D, 512])
    w_bg_d = din("w_bg", [D, 16])
    conv_col_d = din("conv_col", [64, 24, 4])
    a_log_d = din("a_log_rep", [64, 8])
    dt_bias_d = din("dt_bias_rep", [64, 8])
    norm_b_d = din("norm_b_rep", [64, 64])
    w_pa_d = din("w_pa", [64, 8, D])
    w_pb_d = din("w_pb", [64, 8, D])
    w_o_d = din("w_o", [D, D])
    wq_d = din("peer_wq", [D, D])
    subT_d = din("subT", [64, 16, 128])
    u_d = din("peer_u", [16384, D])
    v_d = din("peer_v", [16384, D])
    fnw_d = din("fnw_rep", [128, D])
    ident_d = din("ident", [128, 128])
    freq_d = din("freq_col", [64, 1])
    sign_d = din("sign_col", [64, 1])
    cmask_d = din("cmask", [128, 128])
    ut64_d = din("ut64", [64, 64])
    sl64_d = din("sl64", [64, 64])
    il64_d = din("il64", [64, 64])
    iu64_d = din("iu64", [64, 64])
    su64_d = din("su64", [64, 64])
    out_d = nc.dram_tensor("out", [S, D], F32, kind="ExternalOutput").ap()

    ROT_d = dscr("ROT", [N_ROT, 64, S], BF16)
    CONV_d = dscr("CONVO", [24, 64, S], F32)
    GATE_d = dscr("GATE", [16, 128, S], BF16)
    VW_d = dscr("VW", [S, 144])
    ZS_d = dscr("ZS", [S, 512])
    BG_d = dscr("BG", [S, 16])
    OA_d = dscr("OA", [8, 64, S], BF16)
    OB_d = dscr("OB", [8, 64, S], BF16)
    H_d = dscr("H", [S, D])
    UV_d = dscr("UV", [16384, 2 * D], BF16)
    dUV = Buf("UV")
    dROT, dCONV, dGATE, dVW, dZS, dBG, dOA, dOB, dH = [Buf(n) for n in
                                                      ("ROT", "CONV", "GATE", "VW", "ZS", "BG", "OA", "OB", "H")]
    dOUT = Buf("OUT")
    dIN = Buf("IN")

    dbg_out = {}

    def dbg_tensor(name, shape, dt=F32):
        t = nc.dram_tensor(name, list(shape), dt, kind="ExternalOutput").ap()
        dbg_out[name] = t
        return t

    def sb(st, name, shape, dt=F32):
        t = st.enter_context(nc.sbuf_tensor("sb_" + name, list(shape), dt))
        return t, Buf(name)

    def ps(st, name, shape=(128, 512), dt=F32):
        t = st.enter_context(nc.psum_tensor(name, list(shape), dt))
        return t, Buf(name)

    def load(dst_ap, src_ap, buf, src_buf=dIN, q="sp"):
        K.dma(lambda e, o=dst_ap, i=src_ap: e.dma_start(out=o, in_=i), buf, reads=[src_buf], writes=[buf], q=q)

    def store(dst_ap, src_ap, buf, dst_buf, q="sp"):
        K.dma(lambda e, o=dst_ap, i=src_ap: e.dma_start(out=o, in_=i), buf, reads=[buf], writes=[dst_buf], q=q)

    PS = [ps(stack, "psb%d" % i) for i in range(8)]

    ident, b_ident = sb(stack, "ident", [128, 128])
    load(ident[:], ident_d, b_ident)
    modcol, b_modcol = sb(stack, "modcol", [128, 48])
    modbc, b_modbc = sb(stack, "modbc", [128, 4, D])
    ones128, b_ones = sb(stack, "ones128", [128, 128])
    K.op("pool", lambda e: e.memset(ones128[:], 1.0), writes=[b_ones])

    uvsem = [Buf("uvsem%d" % i) for i in range(8)]
    for qi_ in range(4):
        rs = slice(qi_ * 4096, (qi_ + 1) * 4096)
        K.dma(lambda e: e.dma_start(out=UV_d[rs, 0:D], in_=u_d[rs, :]), uvsem[qi_], reads=[dIN], writes=[], q="pool")
        K.dma(lambda e: e.dma_start(out=UV_d[rs, D:2 * D], in_=v_d[rs, :]), uvsem[4 + qi_], reads=[dIN], writes=[], q="pool")
    for bsem in uvsem:
        dUV.w.append((bsem.dsem, K.cnt[bsem.dsem]))

    with ExitStack() as st:
        ccol, b_ccol = sb(st, "ccol", [128, 8])
        cact, b_cact = sb(st, "cact", [128, 8])
        crep, b_crep = sb(st, "crep", [128, 8, 128])
        bcol, b_bcol = sb(st, "bcol", [128, 48])
        wst = [sb(st, "wada%d" % i, [128, 8, 1024]) for i in range(2)]
        brep, b_brep = sb(st, "brep", [128, 1024])
        load(ccol[:], c_col_d, b_ccol)
        load(bcol[:], b_ada_col_d, b_bcol)
        K.op("act", lambda e: e.activation(out=cact[:], in_=ccol[:], func=AF.Silu), reads=[b_ccol], writes=[b_cact])
        for k in range(8):
            K.op("dve", lambda e, k=k: e.tensor_copy(out=crep[:, k, :], in_=cact[:, k:k + 1].to_broadcast([128, 128])),
                 reads=[b_cact], writes=[b_crep])
        w_ada_v = w_ada_d.rearrange("(k p) n -> p k n", p=128)
        pcol_t, pcol_b = PS[0]
        bc_slot = {2: 0, 3: 1, 4: 2, 5: 3}
        for gi in range(6):
            wt, wb = wst[gi % 2]
            load(wt[:], w_ada_v[:, :, gi * 1024:(gi + 1) * 1024], wb)
            for jj in range(8):
                j = gi * 8 + jj
                for k in range(8):
                    K.op("pe", lambda e, j=j, jj=jj, k=k, wt=wt: e.matmul(
                        pcol_t[:, j:j + 1], lhsT=wt[:, k, jj * 128:(jj + 1) * 128], rhs=cact[:, k:k + 1],
                        start=(k == 0), stop=(k == 7)), reads=[wb, b_cact], writes=[pcol_b])
            if gi in bc_slot:
                sl = bc_slot[gi]
                load(brep[:], b_ada_rep_d[:, gi * 1024:(gi + 1) * 1024], b_brep)
                for hh in range(2):
                    pt, pb = PS[1 + hh]
                    for k in range(8):
                        K.op("pe", lambda e, hh=hh, k=k, wt=wt, pt=pt: e.matmul(
                            pt[:, :], lhsT=crep[:, k, :], rhs=wt[:, k, hh * 512:(hh + 1) * 512],
                            start=(k == 0), stop=(k == 7)), reads=[wb, b_crep], writes=[pb])
                    K.op("dve", lambda e, hh=hh, sl=sl, pt=pt: e.tensor_tensor(
                        out=modbc[:, sl, hh * 512:(hh + 1) * 512], in0=pt[:, :], in1=brep[:, hh * 512:(hh + 1) * 512],
                        op=ALU.add), reads=[pb, b_brep], writes=[b_modbc])
        K.op("dve", lambda e: e.tensor_tensor(out=modcol[:], in0=pcol_t[:, 0:48], in1=bcol[:], op=ALU.add),
             reads=[pcol_b, b_bcol], writes=[b_modcol])
        K.op("dve", lambda e: e.tensor_scalar(out=modcol[:, 8:16], in0=modcol[:, 8:16], scalar1=1.0, scalar2=None,
                                              op0=ALU.add), reads=[b_modcol], writes=[b_modcol])
        K.op("dve", lambda e: e.tensor_scalar(out=modbc[:, 2, :], in0=modbc[:, 2, :], scalar1=1.0, scalar2=None,
                                              op0=ALU.add), reads=[b_modbc], writes=[b_modbc])
        K.barrier()

    if dbg:
        t = dbg_tensor("dbg_modcol", [128, 48])
        store(t, modcol[:], b_modcol, Buf("x"))
        t = dbg_tensor("dbg_modbc", [128, 4 * D])
        store(t, modbc[:].rearrange("p a d -> p (a d)"), b_modbc, Buf("x"))

    def finish():
        K.barrier()
        K.emit()
        stack.close()
        return nc, dbg_out

    if stop_after == "A0":
        return finish()

    with ExitStack() as st:
        n1T, _ = sb(st, "n1T", [128, 8, S], BF16)
        b_n1T = [Buf("n1T%d" % i) for i in range(NT)]
        xts = [sb(st, "xt%d" % i, [128, D]) for i in range(2)]
        junk, b_junk = sb(st, "junkA", [128, D])
        stat, b_stat = sb(st, "statA", [128, 4])
        psT = [PS[0], PS[1]]
        for i in range(NT):
            xt, xb = xts[i % 2]
            load(xt[:], x_d[i * 128:(i + 1) * 128, :], xb)
            K.op("act", lambda e, xt=xt: e.activation(out=junk[:], in_=xt[:], func=AF.Square, accum_out=stat[:, 0:1]),
                 reads=[xb], writes=[b_junk, b_stat])
            K.op("act", lambda e: e.activation(out=stat[:, 1:2], in_=stat[:, 0:1], func=AF.Sqrt, bias=EPS, scale=1.0 / D),
                 reads=[b_stat], writes=[b_stat])
            K.op("dve", lambda e: e.reciprocal(out=stat[:, 2:3], in_=stat[:, 1:2]), reads=[b_stat], writes=[b_stat])
            K.op("dve", lambda e, xt=xt: e.tensor_scalar(out=junk[:], in0=xt[:], scalar1=stat[:, 2:3], scalar2=None,
                                                         op0=ALU.mult), reads=[xb, b_stat], writes=[b_junk])
            for k in range(8):
                pt, pb = psT[k // 4]
                K.op("pe", lambda e, k=k, pt=pt: e.transpose(pt[:, (k % 4) * 128:(k % 4 + 1) * 128],
                                                             junk[:, k * 128:(k + 1) * 128], ident[:]),
                     reads=[b_junk, b_ident], writes=[pb])
            for k in range(8):
                pt, pb = psT[k // 4]
                K.op("act", lambda e, k=k, pt=pt, i=i: e.activation(
                    out=n1T[:, k, i * 128:(i + 1) * 128], in_=pt[:, (k % 4) * 128:(k % 4 + 1) * 128],
                    func=AF.Identity, bias=modcol[:, k:k + 1], scale=modcol[:, 8 + k:9 + k]),
                    reads=[pb, b_modcol], writes=[b_n1T[i]])
        if dbg:
            t = dbg_tensor("dbg_n1T", [128, 8 * S], BF16)
            K.barrier()
            bx = Buf("n1Tall")
            store(t, n1T[:].rearrange("p k s -> p (k s)"), bx, Buf("x"))
        if stop_after == "A1":
            return finish()

        wstg = [sb(st, "wstg%d" % i, [128, 8, 144]) for i in range(2)]
        wbfs = [sb(st, "wbf%d" % i, [128, 8, 144], BF16) for i in range(3)]
        wstg_big = sb(st, "wstg_big", [128, 8, 512])
        wbf_big = sb(st, "wbf_big", [128, 8, 512], BF16)
        cnt = {"stg": 0, "bf": 0}

        def load_w(src_ap, ncols):
            if ncols > 144:
                stg, bs = wstg_big
                wbf, bw = wbf_big
            else:
                stg, bs = wstg[cnt["stg"] % 2]
                wbf, bw = wbfs[cnt["bf"] % 3]
                cnt["stg"] += 1
                cnt["bf"] += 1
            load(stg[:, :, 0:ncols], src_ap.rearrange("(k p) n -> p k n", p=128), bs)
            K.op("pool", lambda e: e.tensor_copy(out=wbf[:, :, 0:ncols], in_=stg[:, :, 0:ncols]), reads=[bs], writes=[bw])
            return wbf, bw

        def allb():
            return list(b_n1T)

        tmpa, b_tmpa = sb(st, "tmpa", [64, 512])
        tmpb, b_tmpb = sb(st, "tmpb", [64, 512])
        st_rot = ExitStack()
        cosT, b_cos = sb(st_rot, "cosT", [64, S])
        sinT, b_sin = sb(st_rot, "sinT", [64, S])
        with ExitStack() as st2:
            SH = S // 2
            posi, b_posi = sb(st2, "posi", [64, SH], I32)
            ang, b_ang = sb(st2, "ang", [64, SH])
            t1, b_t1 = sb(st2, "rr_t1", [64, SH])
            t2, b_t2 = sb(st2, "rr_t2", [64, SH])
            fcol, b_fcol = sb(st2, "fcol", [64, 2])
            load(fcol[:, 0:1], freq_d, b_fcol)
            load(fcol[:, 1:2], sign_d, b_fcol)
            MAGIC = 12582912.0
            C1 = 6.28125
            C2 = float(2.0 * np.pi - 6.28125)
            PIL = 3.1415925
            for hf in range(2):
                cs = slice(hf * SH, (hf + 1) * SH)
                load(posi[:], pos_d[:, cs], b_posi)
                K.op("dve", lambda e: e.tensor_copy(out=ang[:], in_=posi[:]), reads=[b_posi], writes=[b_ang])
                K.op("dve", lambda e: e.tensor_scalar(out=ang[:], in0=ang[:], scalar1=fcol[:, 0:1], scalar2=None, op0=ALU.mult),
                     reads=[b_ang, b_fcol], writes=[b_ang])

                def sin_of(dst, b_dst, shift):
                    K.op("dve", lambda e: e.tensor_scalar(out=t2[:], in0=ang[:], scalar1=float(shift), scalar2=None, op0=ALU.add),
                         reads=[b_ang], writes=[b_t2])
                    K.op("dve", lambda e: e.tensor_scalar(out=t1[:], in0=t2[:], scalar1=float(1.0 / (2 * np.pi)), scalar2=MAGIC,
                                                          op0=ALU.mult, op1=ALU.add), reads=[b_t2], writes=[b_t1])
                    K.op("dve", lambda e: e.tensor_scalar(out=t1[:], in0=t1[:], scalar1=-MAGIC, scalar2=None, op0=ALU.add),
                         reads=[b_t1], writes=[b_t1])
                    K.op("dve", lambda e: e.scalar_tensor_tensor(out=t2[:], in0=t1[:], scalar=-C1, in1=t2[:], op0=ALU.mult,
                                                                 op1=ALU.add), reads=[b_t1, b_t2], writes=[b_t2])
                    K.op("dve", lambda e: e.scalar_tensor_tensor(out=t2[:], in0=t1[:], scalar=-C2, in1=t2[:], op0=ALU.mult,
                                                                 op1=ALU.add), reads=[b_t1, b_t2], writes=[b_t2])
                    K.op("dve", lambda e: e.tensor_scalar(out=t2[:], in0=t2[:], scalar1=PIL, scalar2=-PIL, op0=ALU.min,
                                                          op1=ALU.max), reads=[b_t2], writes=[b_t2])
                    K.op("act", lambda e: e.activation(out=dst[:, cs], in_=t2[:], func=AF.Sin), reads=[b_t2], writes=[b_dst])

                sin_of(sinT, b_sin, 0.0)
                sin_of(cosT, b_cos, np.pi / 2)
            K.op("dve", lambda e: e.tensor_scalar(out=sinT[:], in0=sinT[:], scalar1=fcol[:, 1:2], scalar2=None, op0=ALU.mult),
                 reads=[b_sin, b_fcol], writes=[b_sin])
            K.barrier()

        rot_o = [sb(st_rot, "rot_o%d" % i, [64, S], BF16) for i in range(2)]
        for gi in range(N_ROT):
            wa, bwa = load_w(w_rot_d[:, gi * 64:(gi + 1) * 64], 64)
            ws, bws = load_w(w_rot_sw_d[:, gi * 64:(gi + 1) * 64], 64)
            ro, bro = rot_o[gi % 2]
            for sc in range(NS):
                (p1, pb1), (p2, pb2) = PS[2 + (sc % 2) * 2], PS[3 + (sc % 2) * 2]
                tok = slice(sc * 512, (sc + 1) * 512)
                for k in range(8):
                    K.op("pe", lambda e, k=k, wa=wa, p1=p1, tok=tok: e.matmul(p1[0:64, :], lhsT=wa[:, k, 0:64], rhs=n1T[:, k, tok],
                                                                              start=(k == 0), stop=(k == 7)),
                         reads=[bwa] + allb(), writes=[pb1])
                for k in range(8):
                    K.op("pe", lambda e, k=k, ws=ws, p2=p2, tok=tok: e.matmul(p2[0:64, :], lhsT=ws[:, k, 0:64], rhs=n1T[:, k, tok],
                                                                              start=(k == 0), stop=(k == 7)),
                         reads=[bws] + allb(), writes=[pb2])
                K.op("dve", lambda e, p1=p1, tok=tok: e.tensor_tensor(out=tmpa[:], in0=p1[0:64, :], in1=cosT[:, tok], op=ALU.mult),
                     reads=[pb1, b_cos], writes=[b_tmpa])
                K.op("dve", lambda e, p2=p2, tok=tok: e.tensor_tensor(out=tmpb[:], in0=p2[0:64, :], in1=sinT[:, tok], op=ALU.mult),
                     reads=[pb2, b_sin], writes=[b_tmpb])
                K.op("pool", lambda e, ro=ro, tok=tok: e.tensor_tensor(out=ro[:, tok], in0=tmpa[:], in1=tmpb[:], op=ALU.add),
                     reads=[b_tmpa, b_tmpb], writes=[bro])
            store(ROT_d[gi], ro[:], bro, dROT)

        K.barrier()
        st_rot.close()
        st_cv = ExitStack()
        ccol, b_ccol = sb(st, "convcol", [64, 24, 4])
        load(ccol[:], conv_col_d, b_ccol)
        ones64 = ones128[0:64, 0:64]
        xc, b_xc = sb(st_cv, "xc", [64, S + 3])
        yc, b_yc = sb(st_cv, "yc", [64, S])
        conv_o = [sb(st_cv, "conv_o%d" % i, [64, S]) for i in range(1)]
        K.op("pool", lambda e: e.memset(xc[:, 0:3], 0.0), writes=[b_xc])
        for cg in range(24):
            wa, bwa = load_w(w_conv_d[:, cg * 64:(cg + 1) * 64], 64)
            co, bco = conv_o[0]
            for sc in range(NS):
                p1, pb1 = PS[2 + (sc % 2)]
                tok = slice(sc * 512, (sc + 1) * 512)
                for k in range(8):
                    K.op("pe", lambda e, k=k, wa=wa, p1=p1, tok=tok: e.matmul(p1[0:64, :], lhsT=wa[:, k, 0:64], rhs=n1T[:, k, tok],
                                                                              start=(k == 0), stop=(k == 7)),
                         reads=[bwa] + allb(), writes=[pb1])
                K.op("act", lambda e, p1=p1, sc=sc: e.activation(out=xc[:, 3 + sc * 512:3 + (sc + 1) * 512], in_=p1[0:64, :],
                                                                 func=AF.Copy), reads=[pb1], writes=[b_xc])
            K.op("dve", lambda e, cg=cg: e.tensor_scalar(out=yc[:], in0=xc[:, 3:S + 3], scalar1=ccol[:, cg, 3:4], scalar2=None,
                                                         op0=ALU.mult), reads=[b_xc, b_ccol], writes=[b_yc])
            for i in range(3):
                K.op("dve", lambda e, cg=cg, i=i: e.scalar_tensor_tensor(out=yc[:], in0=xc[:, i:S + i], scalar=ccol[:, cg, i:i + 1],
                                                                         in1=yc[:], op0=ALU.mult, op1=ALU.add),
                     reads=[b_xc, b_ccol, b_yc], writes=[b_yc])
            if cg >= 16:
                K.op("act", lambda e, co=co: e.activation(out=co[:], in_=yc[:], func=AF.Silu), reads=[b_yc], writes=[bco])
            else:
                K.op("act", lambda e: e.activation(out=yc[:], in_=yc[:], func=AF.Silu), reads=[b_yc], writes=[b_yc])
                for sc in range(NS):
                    p1, pb1 = PS[4 + (sc % 2)]
                    tok = slice(sc * 512, (sc + 1) * 512)
                    K.op("pool", lambda e, tok=tok: e.tensor_tensor(out=tmpa[:], in0=yc[:, tok], in1=yc[:, tok], op=ALU.mult),
                         reads=[b_yc], writes=[b_tmpa])
                    K.op("pe", lambda e, p1=p1: e.matmul(p1[0:64, :], lhsT=ones64, rhs=tmpa[:], start=True, stop=True),
                         reads=[b_ones, b_tmpa], writes=[pb1])
                    K.op("act", lambda e, p1=p1: e.activation(out=tmpb[:], in_=p1[0:64, :], func=AF.Sqrt, bias=EPS, scale=1.0),
                         reads=[pb1], writes=[b_tmpb])
                    K.op("dve", lambda e: e.reciprocal(out=tmpb[:], in_=tmpb[:]), reads=[b_tmpb], writes=[b_tmpb])
                    qs = 0.125 if cg < 8 else 1.0
                    K.op("dve", lambda e, co=co, tok=tok, qs=qs: e.scalar_tensor_tensor(
                        out=co[:, tok], in0=yc[:, tok], scalar=qs, in1=tmpb[:], op0=ALU.mult, op1=ALU.mult),
                        reads=[b_yc, b_tmpb], writes=[bco])
            store(CONV_d[cg], co[:], bco, dCONV)

        K.barrier()
        st_cv.close()
        gate_o = [sb(st, "gate_o%d" % i, [128, S], BF16) for i in range(2)]
        for gc in range(16):
            wa, bwa = load_w(w_gate_d[:, gc * 128:(gc + 1) * 128], 128)
            go, bgo = gate_o[gc % 2]
            for sc in range(NS):
                p1, pb1 = PS[2 + (sc % 2)]
                tok = slice(sc * 512, (sc + 1) * 512)
                for k in range(8):
                    K.op("pe", lambda e, k=k, wa=wa, p1=p1, tok=tok: e.matmul(p1[:, :], lhsT=wa[:, k, 0:128], rhs=n1T[:, k, tok],
                                                                              start=(k == 0), stop=(k == 7)),
                         reads=[bwa] + allb(), writes=[pb1])
                K.op("act", lambda e, p1=p1, go=go, tok=tok: e.activation(out=go[:, tok], in_=p1[:, :], func=AF.Sigmoid),
                     reads=[pb1], writes=[bgo])
            store(GATE_d[gc], go[:], bgo, dGATE)

        wt_, bwt = load_w(w_tok_d, 144)
        wz_, bwz = load_w(w_z_d, 512)
        wg_, bwg = load_w(w_bg_d, 16)
        alog, b_alog = sb(st, "alog", [128, 8])
        dtb, b_dtb = sb(st, "dtb", [128, 8])
        load(alog[0:64, :], a_log_d, b_alog)
        load(alog[64:128, :], a_log_d, b_alog)
        load(dtb[0:64, :], dt_bias_d, b_dtb)
        load(dtb[64:128, :], dt_bias_d, b_dtb)
        K.op("act", lambda e: e.activation(out=alog[:], in_=alog[:], func=AF.Exp), reads=[b_alog], writes=[b_alog])
        K.op("dve", lambda e: e.tensor_scalar(out=alog[:], in0=alog[:], scalar1=-1.0, scalar2=None, op0=ALU.mult),
             reads=[b_alog], writes=[b_alog])
        vw_o = [sb(st, "vw_o%d" % i, [128, 144]) for i in range(2)]
        zs_o = [sb(st, "zs_o%d" % i, [128, 512]) for i in range(2)]
        bg_o = [sb(st, "bg_o%d" % i, [128, 16]) for i in range(2)]
        for i in range(NT):
            tk = slice(i * 128, (i + 1) * 128)
            (p1, pb1), (p2, pb2), (p3, pb3) = PS[2], PS[3], PS[4]
            vo, bvo = vw_o[i % 2]
            zo, bzo = zs_o[i % 2]
            bo, bbo = bg_o[i % 2]
            for k in range(8):
                K.op("pe", lambda e, k=k, tk=tk: e.matmul(p1[:, 0:144], lhsT=n1T[:, k, tk], rhs=wt_[:, k, 0:144],
                                                          start=(k == 0), stop=(k == 7)), reads=[bwt, b_n1T[i]], writes=[pb1])
            for k in range(8):
                K.op("pe", lambda e, k=k, tk=tk: e.matmul(p2[:, 0:512], lhsT=n1T[:, k, tk], rhs=wz_[:, k, 0:512],
                                                          start=(k == 0), stop=(k == 7)), reads=[bwz, b_n1T[i]], writes=[pb2])
            for k in range(8):
                K.op("pe", lambda e, k=k, tk=tk: e.matmul(p3[:, 0:16], lhsT=n1T[:, k, tk], rhs=wg_[:, k, 0:16],
                                                          start=(k == 0), stop=(k == 7)), reads=[bwg, b_n1T[i]], writes=[pb3])
            K.op("act", lambda e, vo=vo: e.activation(out=vo[:], in_=p1[:, 0:144], func=AF.Copy), reads=[pb1], writes=[bvo])
            K.op("act", lambda e, zo=zo: e.activation(out=zo[:], in_=p2[:, 0:512], func=AF.Silu), reads=[pb2], writes=[bzo])
            K.op("act", lambda e, bo=bo: e.activation(out=bo[:, 0:8], in_=p3[:, 0:8], func=AF.Sigmoid), reads=[pb3], writes=[bbo])
            K.op("dve", lambda e, bo=bo: e.tensor_tensor(out=bo[:, 8:16], in0=p3[:, 8:16], in1=dtb[:], op=ALU.add),
                 reads=[pb3, b_dtb, bbo], writes=[bbo])
            K.op("act", lambda e, bo=bo: e.activation(out=bo[:, 8:16], in_=bo[:, 8:16], func=AF.Exp), reads=[bbo], writes=[bbo])
            K.op("act", lambda e, bo=bo: e.activation(out=bo[:, 8:16], in_=bo[:, 8:16], func=AF.Ln, bias=1.0, scale=1.0),
                 reads=[bbo], writes=[bbo])
            K.op("dve", lambda e, bo=bo: e.tensor_tensor(out=bo[:, 8:16], in0=bo[:, 8:16], in1=alog[:], op=ALU.mult),
                 reads=[bbo, b_alog], writes=[bbo])
            store(VW_d[tk, :], vo[:], bvo, dVW)
            store(ZS_d[tk, :], zo[:], bzo, dZS)
            store(BG_d[tk, :], bo[:], bbo, dBG)
        K.barrier()

    if dbg:
        for nm, src in (("ROT", ROT_d), ("CONVO", CONV_d), ("GATE", GATE_d), ("VW", VW_d), ("ZS", ZS_d), ("BG", BG_d)):
            pass
    if stop_after == "A":
        return finish()

    with ExitStack() as st:
        kiT, b_kiT = sb(st, "kiT", [64, S], BF16)
        kaT, b_kaT = sb(st, "kaT", [64, 2, S], BF16)
        load(kiT[:], ROT_d[26], b_kiT, dROT)
        for g in range(2):
            load(kaT[:, g, :], ROT_d[8 + g], b_kaT, dROT)
        va_bf, b_va = sb(st, "va_bf", [128, NT, 128], BF16)
        wi_s, b_wi = sb(st, "wi_s", [128, NT, 16])
        with ExitStack() as st2:
            vw_f, b_vwf = sb(st2, "vw_f", [128, NT, 144])
            load(vw_f[:], VW_d.rearrange("(n p) c -> p n c", p=128), b_vwf, dVW)
            K.op("pool", lambda e: e.tensor_copy(out=va_bf[:], in_=vw_f[:, :, 0:128]), reads=[b_vwf], writes=[b_va])
            K.op("dve", lambda e: e.tensor_scalar(out=wi_s[:], in0=vw_f[:, :, 128:144], scalar1=1.0 / 32.0, scalar2=None,
                                                  op0=ALU.mult), reads=[b_vwf], writes=[b_wi])
            K.barrier()
        ones_bf, b_onesbf = sb(st, "ones_bf", [128, 64], BF16)
        K.op("pool", lambda e: e.memset(ones_bf[:], 1.0), writes=[b_onesbf])
        id4, b_id4 = sb(st, "id4", [128, 4, 128], BF16)
        for r4 in range(4):
            K.op("pool", lambda e, r4=r4: e.tensor_copy(out=id4[:, r4, :], in_=ident[:]), reads=[b_ident], writes=[b_id4])
        cmask, b_cmask = sb(st, "cmask", [128, 128])
        load(cmask[:], cmask_d, b_cmask)
        qi_ts = [sb(st, "qi_t%d" % i, [64, 16, 128], BF16) for i in range(2)]
        qa_ts = [sb(st, "qa_t%d" % i, [64, 8, 128], BF16) for i in range(2)]
        scores = [sb(st, "score%d" % i, [128, S]) for i in range(2)]
        work, b_work = sb(st, "work", [128, S])
        mnegs = [sb(st, "mneg%d" % i, [128, S], BF16) for i in range(2)]
        m8, b_m8 = sb(st, "m8", [128, 8])
        thr, b_thr = sb(st, "thr", [128, 8])
        rls = [sb(st, "rl%d" % i, [128, 512]) for i in range(2)]
        pTs = [sb(st, "pT%d" % i, [128, 512], BF16) for i in range(2)]
        rden, b_rden = sb(st, "rden", [64, 512])
        oas = [sb(st, "oa%d" % i, [64, 4, 128], BF16) for i in range(2)]
        wdiags = [sb(st, "wdiag%d" % i, [128, 16, 128], BF16) for i in range(2)]
        rlbs = [sb(st, "rlb%d" % i, [128, 512], BF16) for i in range(3)]
        ctr = {"nlog": 0, "nst": 0, "nch": 0}

        def tile_ctx(i):
            return dict(Si=128 * (i + 1), tk=slice(i * 128, (i + 1) * 128), qi=qi_ts[i % 2], qa=qa_ts[i % 2],
                        score=scores[i % 2], mneg=mnegs[i % 2], wd=wdiags[i % 2])

        def indexer(i):
            c = tile_ctx(i)
            Si, tk = c["Si"], c["tk"]
            qi_t, b_qi = c["qi"]
            qa_t, b_qa = c["qa"]
            score, b_score = c["score"]
            wd, b_wd = c["wd"]
            load(qi_t[:], ROT_d[10:26, :, tk].rearrange("h p t -> p h t"), b_qi, dROT)
            load(qa_t[:], ROT_d[0:8, :, tk].rearrange("h p t -> p h t"), b_qa, dROT)
            for h in range(16):
                K.op("pool", lambda e: e.tensor_scalar(out=wd[:, h, :], in0=ident[:], scalar1=wi_s[:, i, h:h + 1], scalar2=None, op0=ALU.mult),
                     reads=[b_ident, b_wi], writes=[b_wd])
            for c0 in range(0, Si, 512):
                wc = min(512, Si - c0)
                psc, pbsc = PS[6 + ctr["nch"] % 2]
                ctr["nch"] += 1
                for h in range(16):
                    pl, pbl = PS[ctr["nlog"] % 2]
                    rl, brl = rlbs[ctr["nlog"] % 3]
                    ctr["nlog"] += 1
                    K.op("pe", lambda e: e.matmul(pl[:, 0:wc], lhsT=qi_t[:, h, :], rhs=kiT[:, c0:c0 + wc], start=True, stop=True),
                         reads=[b_qi, b_kiT], writes=[pbl])
                    K.op("act", lambda e: e.activation(out=rl[:, 0:wc], in_=pl[:, 0:wc], func=AF.Relu), reads=[pbl], writes=[brl])
                    K.op("pe", lambda e: e.matmul(psc[:, 0:wc], lhsT=wd[:, h, :], rhs=rl[:, 0:wc], start=(h == 0), stop=(h == 15)),
                         reads=[b_wd, brl], writes=[pbsc])
                K.op("dve", lambda e: e.tensor_copy(out=score[:, c0:c0 + wc], in_=psc[:, 0:wc]), reads=[pbsc], writes=[b_score])

        def threshold(i):
            c = tile_ctx(i)
            Si, tk = c["Si"], c["tk"]
            score, b_score = c["score"]
            mneg, b_mneg = c["mneg"]
            if Si > TOPK:
                K.op("dve", lambda e: e.tensor_reduce(out=thr[:, 2:3], in_=score[:, 0:Si], axis=AX.X, op=ALU.min), reads=[b_score], writes=[b_thr])
                K.op("dve", lambda e: e.tensor_reduce(out=thr[:, 3:4], in_=score[:, 0:Si], axis=AX.X, op=ALU.max), reads=[b_score], writes=[b_thr])
                NIT = 24
                K.op("dve", lambda e: e.tensor_tensor(out=score[:, tk], in0=score[:, tk], in1=cmask[:], op=ALU.add),
                     reads=[b_score, b_cmask], writes=[b_score])
                K.op("dve", lambda e: e.tensor_scalar(out=thr[:, 0:1], in0=thr[:, 2:3], scalar1=1.0, scalar2=None, op0=ALU.mult),
                     reads=[b_thr], writes=[b_thr])
                K.op("dve", lambda e: e.tensor_tensor(out=thr[:, 1:2], in0=thr[:, 3:4], in1=thr[:, 2:3], op=ALU.subtract),
                     reads=[b_thr], writes=[b_thr])
                for it in range(1, NIT + 1):
                    sc_ = float(2.0 ** (-it))
                    K.op("dve", lambda e: e.scalar_tensor_tensor(out=thr[:, 4:5], in0=thr[:, 1:2], scalar=sc_, in1=thr[:, 0:1],
                                                                 op0=ALU.mult, op1=ALU.add), reads=[b_thr], writes=[b_thr])
                    K.op("dve", lambda e: e.tensor_scalar(out=work[:, 0:Si], in0=score[:, 0:Si], scalar1=thr[:, 4:5], scalar2=0.0,
                                                          op0=ALU.is_ge, op1=ALU.add, accum_out=thr[:, 5:6]),
                         reads=[b_score, b_thr], writes=[b_work, b_thr])
                    K.op("dve", lambda e: e.tensor_scalar(out=thr[:, 6:7], in0=thr[:, 5:6], scalar1=float(TOPK) - 0.5, scalar2=sc_,
                                                          op0=ALU.is_ge, op1=ALU.mult), reads=[b_thr], writes=[b_thr])
                    K.op("dve", lambda e: e.scalar_tensor_tensor(out=thr[:, 0:1], in0=thr[:, 6:7], scalar=thr[:, 1:2], in1=thr[:, 0:1],
                                                                 op0=ALU.mult, op1=ALU.add), reads=[b_thr], writes=[b_thr])
                K.op("dve", lambda e: e.tensor_scalar(out=mneg[:, 0:Si], in0=score[:, 0:Si], scalar1=thr[:, 0:1], scalar2=-30000.0,
                                                      op0=ALU.is_lt, op1=ALU.mult), reads=[b_score, b_thr], writes=[b_mneg])
            else:
                K.op("dve", lambda e: e.tensor_tensor(out=score[:, tk], in0=score[:, tk], in1=cmask[:], op=ALU.add),
                     reads=[b_score, b_cmask], writes=[b_score])
                K.op("dve", lambda e: e.tensor_scalar(out=mneg[:, 0:Si], in0=score[:, 0:Si], scalar1=-1.0e29, scalar2=-30000.0,
                                                      op0=ALU.is_lt, op1=ALU.mult), reads=[b_score], writes=[b_mneg])

        def attention(i):
            c = tile_ctx(i)
            tk = c["tk"]
            qa_t, b_qa = c["qa"]
            mneg, b_mneg = c["mneg"]
            for g in range(2):
                pn, pbn = PS[4]
                pd, pbd = PS[5]
                for j in range(i + 1):
                    kk = slice(j * 128, (j + 1) * 128)
                    psT_, pbs = PS[2 + ctr["nst"] % 2]
                    pT, bpT = pTs[ctr["nst"] % 2]
                    ctr["nst"] += 1
                    K.op("pe", lambda e: e.matmul(psT_[:, :], lhsT=kaT[:, g, kk], rhs=qa_t[:, 4 * g:4 * g + 4, :], start=True, stop=False),
                         reads=[b_kaT, b_qa], writes=[pbs])
                    K.op("pe", lambda e: e.matmul(psT_[:, :], lhsT=mneg[:, kk], rhs=id4[:], start=False, stop=True),
                         reads=[b_mneg, b_id4], writes=[pbs])
                    K.op("act", lambda e: e.activation(out=pT[:], in_=psT_[:, :], func=AF.Exp, scale=0.125), reads=[pbs], writes=[bpT])
                    K.op("pe", lambda e: e.matmul(pn[0:64, :], lhsT=va_bf[:, j, g * 64:(g + 1) * 64], rhs=pT[:], start=(j == 0), stop=(j == i)),
                         reads=[b_va, bpT], writes=[pbn])
                    K.op("pe", lambda e: e.matmul(pd[0:64, :], lhsT=ones_bf[:], rhs=pT[:], start=(j == 0), stop=(j == i)),
                         reads=[b_onesbf, bpT], writes=[pbd])
                oa, boa = oas[g]
                K.op("dve", lambda e: e.reciprocal(out=rden[:], in_=pd[0:64, :]), reads=[pbd], writes=[b_rden])
                K.op("dve", lambda e: e.tensor_tensor(out=oa[:].rearrange("p h t -> p (h t)"), in0=pn[0:64, :], in1=rden[:], op=ALU.mult),
                     reads=[pbn, b_rden], writes=[boa])
                store(OA_d[4 * g:4 * g + 4, :, tk].rearrange("h p t -> p h t"), oa[:], boa, dOA)

        indexer(0)
        threshold(0)
        for i in range(NT):
            if i + 1 < NT:
                indexer(i + 1)
            attention(i)
            if i + 1 < NT:
                threshold(i + 1)
        K.barrier()
    if stop_after == "B":
        return finish()

    with ExitStack() as st:
        def cst(name, src):
            t, b = sb(st, name, [64, 64])
            load(t[:], src, b)
            return t, b
        ut64, b_ut = cst("ut64", ut64_d)
        sl64, b_sl = cst("sl64", sl64_d)
        iu64, b_iu = cst("iu64", iu64_d)
        normb, b_normb = cst("normb", norm_b_d)
        id64 = ident[0:64, 0:64]
        ones64f = ones128[0:64, 0:64]
        nones, b_nones = sb(st, "nones64", [64, 64])
        K.op("pool", lambda e: e.memset(nones[:], -1.0), writes=[b_nones])
        bg, b_bg = sb(st, "bg_all", [64, NCH, 16])
        load(bg[:], BG_d.rearrange("(n c) f -> c n f", c=64), b_bg, dBG)
        nbeta, b_nbeta = sb(st, "nbeta", [64, NCH, 8])
        K.op("dve", lambda e: e.tensor_scalar(out=nbeta[:], in0=bg[:, :, 0:8], scalar1=-1.0, scalar2=None, op0=ALU.mult),
             reads=[b_bg], writes=[b_nbeta])
        obT, b_obT = sb(st, "obT_all", [64, 8, S], BF16)
        Sst, b_S = sb(st, "Sstate", [64, 8, 64])
        K.op("pool", lambda e: e.memset(Sst[:], 0.0), writes=[b_S])

        def bc_h(t2d):
            return t2d.rearrange("p (o c) -> p o c", o=1).to_broadcast([64, 8, 64])

        def bc_l(tp8):
            return tp8.rearrange("p (h o) -> p h o", o=1).to_broadcast([64, 8, 64])

        def scr(name, shape=(64, 8, 64)):
            return [sb(st, "%s_%d" % (name, i), list(shape)) for i in range(2)]
        names = ("qc", "kc", "vc", "Gm", "tt", "Esl", "Eu", "M", "MT", "A", "AT", "P",
                 "VB", "KBg", "Kd", "IT", "U", "KC", "vn", "o2", "oo", "obc")
        S3 = {nm: scr(nm) for nm in names}
        s_zc = scr("zc", (64, 512))
        s_ex = scr("ex3", (64, 3, 8))
        s_sm = scr("smallc", (64, 4, 8))

        def pv(bank):
            return PS[bank][0][0:64, :].rearrange("p (h c) -> p h c", h=8), PS[bank][1]

        def mm(dst_ap, dst_b, lhsT, rhs, reads, start=True, stop=True, full=False):
            K.op("pe", lambda e: e.matmul(dst_ap, lhsT=lhsT, rhs=rhs, start=start, stop=stop), reads=reads, writes=[dst_b])

        def tr(dst_ap, dst_b, src, reads):
            K.op("pe", lambda e: e.transpose(dst_ap, src, id64), reads=reads + [b_ident], writes=[dst_b])

        for n in range(NCH):
            cs = slice(n * 64, (n + 1) * 64)
            pz = n % 2
            T = {nm: S3[nm][pz] for nm in names}
            (qc, b_q), (kc, b_k), (vc, b_v) = T["qc"], T["kc"], T["vc"]
            (Gm, bGm), (tt, btt), (Esl, bEsl), (Eu, bEu) = T["Gm"], T["tt"], T["Esl"], T["Eu"]
            (M, bM), (MT, bMT), (A, bA), (AT, bAT), (P, bP) = T["M"], T["MT"], T["A"], T["AT"], T["P"]
            (VB, bVB), (KBg, bKBg), (Kd, bKd), (IT, bIT) = T["VB"], T["KBg"], T["Kd"], T["IT"]
            (U, bU), (KC, bKC), (vn, bvn), (o2, bo2), (oo, boo), (obc, bobc) = T["U"], T["KC"], T["vn"], T["o2"], T["oo"], T["obc"]
            zc, b_z = s_zc[pz]
            ex3, bex = s_ex[pz]
            sm, bsm = s_sm[pz]
            load(qc[:], CONV_d[0:8, :, cs].rearrange("h p t -> p h t"), b_q, dCONV)
            load(kc[:], CONV_d[8:16, :, cs].rearrange("h p t -> p h t"), b_k, dCONV)
            load(vc[:], CONV_d[16:24, :, cs].rearrange("h p t -> p h t"), b_v, dCONV)
            load(zc[:], ZS_d[cs, :], b_z, dZS)
            g8 = bg[:, n, 8:16]
            beta8 = bg[:, n, 0:8]
            nb8 = nbeta[:, n, :]
            K.op("dve", lambda e: e.tensor_tensor(out=Gm[:], in0=bc_h(ut64[:]), in1=bc_l(g8), op=ALU.mult),
                 reads=[b_ut, b_bg], writes=[bGm])
            pD, bpD = pv(0)
            mm(PS[0][0][0:64, :], bpD, nones[:], Gm[:].rearrange("p h c -> p (h c)"), [bGm, b_nones], True, False, full=True)
            for h in range(8):
                mm(pD[:, h, :], bpD, Gm[:, h, :], ones64f, [bGm, b_ones], False, h == 7, full=True)
            pG, bpG = PS[1][0][0:64, 0:24].rearrange("p (a h) -> p a h", a=3), PS[1][1]
            mm(pG[:, 0, :], bpG, ut64[:], g8, [b_ut, b_bg], full=True)
            mm(pG[:, 1, :], bpG, ones64f, g8, [b_ones, b_bg], full=True)
            mm(pG[:, 2, :], bpG, sl64[:], g8, [b_sl, b_bg], full=True)
            K.op("act", lambda e: e.activation(out=ex3[:], in_=pG, func=AF.Exp), reads=[bpG], writes=[bex])
            K.op("dve", lambda e: e.tensor_scalar(out=tt[:], in0=pD, scalar1=0.0, scalar2=None, op0=ALU.min), reads=[bpD], writes=[btt])
            K.op("act", lambda e: e.activation(out=Esl[:], in_=tt[:], func=AF.Exp), reads=[btt], writes=[bEsl])
            K.op("pool", lambda e: e.tensor_tensor(out=Esl[:], in0=Esl[:], in1=bc_h(sl64[:]), op=ALU.mult), reads=[bEsl, b_sl], writes=[bEsl])
            K.op("dve", lambda e: e.tensor_scalar(out=tt[:], in0=pD, scalar1=-1.0, scalar2=0.0, op0=ALU.mult, op1=ALU.min),
                 reads=[bpD, bEsl], writes=[btt])
            K.op("act", lambda e: e.activation(out=Eu[:], in_=tt[:], func=AF.Exp), reads=[btt], writes=[bEu])
            K.op("pool", lambda e: e.tensor_tensor(out=Eu[:], in0=Eu[:], in1=bc_h(iu64[:]), op=ALU.mult), reads=[bEu, b_iu], writes=[bEu])
            pKK, bpKK = pv(2)
            for h in range(8):
                mm(pKK[:, h, :], bpKK, kc[:, h, :], kc[:, h, :], [b_k], full=True)
            K.op("dve", lambda e: e.tensor_tensor(out=M[:], in0=pKK, in1=bc_l(nb8), op=ALU.mult), reads=[bpKK, b_nbeta], writes=[bM])
            K.op("pool", lambda e: e.tensor_tensor(out=M[:], in0=M[:], in1=Esl[:], op=ALU.mult), reads=[bM, bEsl], writes=[bM])
            pMt, bpMt = pv(3)
            for h in range(8):
                tr(pMt[:, h, :], bpMt, M[:, h, :], [bM])
            K.op("act", lambda e: e.activation(out=MT[:], in_=pMt, func=AF.Copy), reads=[bpMt], writes=[bMT])
            K.op("pool", lambda e: e.tensor_tensor(out=P[:], in0=MT[:], in1=bc_h(id64), op=ALU.add), reads=[bMT, b_ident], writes=[bP])
            cA, cbA, cAT, cbAT = M, bM, MT, bMT
            pA2, bpA2 = pv(4)
            pA2T, bpA2T = pv(5)
            pPp, bpPp = pv(6)
            for lv in range(5):
                for h in range(8):
                    mm(pA2[:, h, :], bpA2, cAT[:, h, :], cA[:, h, :], [cbA, cbAT], full=True)
                if lv < 4:
                    for h in range(8):
                        mm(pA2T[:, h, :], bpA2T, cA[:, h, :], cAT[:, h, :], [cbA, cbAT], full=True)
                K.op("act", lambda e: e.activation(out=A[:], in_=pA2, func=AF.Copy), reads=[bpA2, cbA], writes=[bA])
                if lv < 4:
                    K.op("dve", lambda e: e.tensor_copy(out=AT[:], in_=pA2T), reads=[bpA2T, cbAT], writes=[bAT])
                cA, cbA, cAT, cbAT = A, bA, AT, bAT
                for h in range(8):
                    mm(pPp[:, h, :], bpPp, A[:, h, :], P[:, h, :], [bA, bP], full=True)
                K.op("dve", lambda e: e.tensor_tensor(out=P[:], in0=pPp, in1=P[:], op=ALU.add), reads=[bpPp, bP], writes=[bP])
            pVt, bpVt = pv(7)
            pKt, bpKt = pv(3)
            for h in range(8):
                tr(pVt[:, h, :], bpVt, vc[:, h, :], [b_v])
            for h in range(8):
                tr(pKt[:, h, :], bpKt, kc[:, h, :], [b_k])
            K.op("dve", lambda e: e.tensor_tensor(out=VB[:], in0=pVt, in1=bc_l(beta8), op=ALU.mult), reads=[bpVt, b_bg], writes=[bVB])
            K.op("pool", lambda e: e.tensor_tensor(out=sm[:, 0, :], in0=beta8, in1=ex3[:, 0, :], op=ALU.mult), reads=[b_bg, bex], writes=[bsm])
            K.op("dve", lambda e: e.tensor_tensor(out=KBg[:], in0=pKt, in1=bc_l(sm[:, 0, :]), op=ALU.mult), reads=[bpKt, bsm], writes=[bKBg])
            K.op("dve", lambda e: e.tensor_tensor(out=Kd[:], in0=pKt, in1=bc_l(ex3[:, 2, :]), op=ALU.mult), reads=[bpKt, bex], writes=[bKd])
            pU, bpU = pv(0)
            pKC, bpKC = pv(2)
            for h in range(8):
                mm(pU[:, h, :], bpU, P[:, h, :], VB[:, h, :], [bP, bVB])
            for h in range(8):
                mm(pKC[:, h, :], bpKC, KBg[:, h, :], P[:, h, :], [bP, bKBg])
            K.op("act", lambda e: e.activation(out=U[:], in_=pU, func=AF.Copy), reads=[bpU], writes=[bU])
            K.op("act", lambda e: e.activation(out=KC[:], in_=pKC, func=AF.Copy), reads=[bpKC], writes=[bKC])
            pKQ, bpKQ = pv(7)
            for h in range(8):
                mm(pKQ[:, h, :], bpKQ, kc[:, h, :], qc[:, h, :], [b_k, b_q])
            K.op("dve", lambda e: e.tensor_tensor(out=IT[:], in0=pKQ, in1=Eu[:], op=ALU.mult), reads=[bpKQ, bEu], writes=[bIT])
            pVN, bpVN = pv(4)
            pO1, bpO1 = pv(5)
            for h in range(8):
                mm(pVN[:, h, :], bpVN, KC[:, h, :], Sst[:, h, :], [bKC, b_S])
            for h in range(8):
                mm(pO1[:, h, :], bpO1, qc[:, h, :], Sst[:, h, :], [b_q, b_S])
            K.op("dve", lambda e: e.tensor_tensor(out=vn[:], in0=U[:], in1=pVN, op=ALU.subtract), reads=[bU, bpVN], writes=[bvn])
            pO2, bpO2 = pv(6)
            pSd, bpSd = pv(3)
            for h in range(8):
                mm(pO2[:, h, :], bpO2, IT[:, h, :], vn[:, h, :], [bIT, bvn])
            for h in range(8):
                mm(pSd[:, h, :], bpSd, Kd[:, h, :], vn[:, h, :], [bKd, bvn])
            K.op("act", lambda e: e.activation(out=o2[:], in_=pO2, func=AF.Copy), reads=[bpO2], writes=[bo2])
            K.op("dve", lambda e: e.tensor_tensor(out=oo[:], in0=pO1, in1=bc_l(ex3[:, 0, :]), op=ALU.mult), reads=[bpO1, bex], writes=[boo])
            K.op("pool", lambda e: e.tensor_tensor(out=oo[:], in0=oo[:], in1=o2[:], op=ALU.add), reads=[boo, bo2], writes=[boo])
            K.op("pool", lambda e: e.tensor_tensor(out=Sst[:], in0=Sst[:], in1=bc_l(ex3[:, 1, :]), op=ALU.mult), reads=[b_S, bex], writes=[b_S])
            K.op("dve", lambda e: e.tensor_tensor(out=Sst[:], in0=pSd, in1=Sst[:], op=ALU.add), reads=[bpSd, b_S], writes=[b_S])
            K.op("pool", lambda e: e.tensor_tensor(out=o2[:], in0=oo[:], in1=oo[:], op=ALU.mult), reads=[boo, bo2], writes=[bo2])
            K.op("dve", lambda e: e.tensor_reduce(out=sm[:, 1, :], in_=o2[:], axis=AX.X, op=ALU.add), reads=[bo2], writes=[bsm])
            K.op("act", lambda e: e.activation(out=sm[:, 2, :], in_=sm[:, 1, :], func=AF.Sqrt, bias=EPS, scale=1.0 / 64.0), reads=[bsm], writes=[bsm])
            K.op("dve", lambda e: e.reciprocal(out=sm[:, 3, :], in_=sm[:, 2, :]), reads=[bsm], writes=[bsm])
            K.op("dve", lambda e: e.tensor_tensor(out=obc[:], in0=oo[:], in1=bc_l(sm[:, 3, :]), op=ALU.mult), reads=[boo, bsm], writes=[bobc])
            K.op("pool", lambda e: e.tensor_tensor(out=obc[:], in0=obc[:], in1=bc_h(normb[:]), op=ALU.mult), reads=[bobc, b_normb], writes=[bobc])
            K.op("dve", lambda e: e.tensor_tensor(out=obc[:], in0=obc[:], in1=zc[:].rearrange("p (h c) -> p h c", h=8), op=ALU.mult),
                 reads=[bobc, b_z], writes=[bobc])
            pOT, bpOT = pv(1)
            for h in range(8):
                tr(pOT[:, h, :], bpOT, obc[:, h, :], [bobc])
            K.op("act", lambda e: e.activation(out=obT[:, :, cs], in_=pOT, func=AF.Copy), reads=[bpOT], writes=[b_obT])
        for h in range(8):
            store(OB_d[h], obT[:, h, :], b_obT, dOB)
        K.barrier()
    if stop_after == "C":
        return finish()

    with ExitStack() as st:
        wstage, b_wstage = sb(st, "wstageD", [128, 8, D])
        wpa, b_wpa = sb(st, "wpa_bf", [64, 8, D], BF16)
        wpb, b_wpb = sb(st, "wpb_bf", [64, 8, D], BF16)
        wo, b_wo = sb(st, "wo_bf", [128, 8, D], BF16)
        load(wstage[0:64], w_pa_d, b_wstage)
        K.op("pool", lambda e: e.tensor_copy(out=wpa[:], in_=wstage[0:64]), reads=[b_wstage], writes=[b_wpa])
        load(wstage[0:64], w_pb_d, b_wstage)
        K.op("pool", lambda e: e.tensor_copy(out=wpb[:], in_=wstage[0:64]), reads=[b_wstage], writes=[b_wpb])
        load(wstage[:], w_o_d.rearrange("(k p) n -> p k n", p=128), b_wstage)
        K.op("pool", lambda e: e.tensor_copy(out=wo[:], in_=wstage[:]), reads=[b_wstage], writes=[b_wo])
        oa_ss = [sb(st, "oa_s%d" % i, [64, 8, 512], BF16) for i in range(2)]
        ob_ss = [sb(st, "ob_s%d" % i, [64, 8, 512], BF16) for i in range(2)]
        ga_ss = [sb(st, "ga_s%d" % i, [128, 512], BF16) for i in range(2)]
        gb_ss = [sb(st, "gb_s%d" % i, [128, 512], BF16) for i in range(2)]
        mrgs = [sb(st, "mrg%d" % i, [128, 8, 512], BF16) for i in range(2)]
        m1, b_m1 = sb(st, "m1", [128, 512])
        m2, b_m2 = sb(st, "m2", [128, 512])
        xds = [sb(st, "xd%d" % i, [128, D]) for i in range(2)]
        hds = [sb(st, "hd%d" % i, [128, D]) for i in range(2)]
        ng = 0
        nt_ = 0
        for sc in range(NS):
            tok = slice(sc * 512, (sc + 1) * 512)
            oa_s, b_oas = oa_ss[sc % 2]
            ob_s, b_obs = ob_ss[sc % 2]
            mrg, b_mrg = mrgs[sc % 2]
            load(oa_s[:], OA_d[:, :, tok].rearrange("h p t -> p h t"), b_oas, dOA)
            load(ob_s[:], OB_d[:, :, tok].rearrange("h p t -> p h t"), b_obs, dOB)
            for nn in range(8):
                ga_s, b_gas = ga_ss[ng % 2]
                gb_s, b_gbs = gb_ss[ng % 2]
                (pa, pba), (pb_, pbb) = PS[(ng % 2) * 2], PS[(ng % 2) * 2 + 1]
                ng += 1
                load(ga_s[:], GATE_d[nn][:, tok], b_gas, dGATE)
                load(gb_s[:], GATE_d[8 + nn][:, tok], b_gbs, dGATE)
                for hh in range(8):
                    K.op("pe", lambda e: e.matmul(pa[:, :], lhsT=wpa[:, hh, nn * 128:(nn + 1) * 128], rhs=oa_s[:, hh, :],
                                                  start=(hh == 0), stop=(hh == 7)), reads=[b_wpa, b_oas], writes=[pba])
                for hh in range(8):
                    K.op("pe", lambda e: e.matmul(pb_[:, :], lhsT=wpb[:, hh, nn * 128:(nn + 1) * 128], rhs=ob_s[:, hh, :],
                                                  start=(hh == 0), stop=(hh == 7)), reads=[b_wpb, b_obs], writes=[pbb])
                K.op("dve", lambda e: e.tensor_tensor(out=m1[:], in0=pa[:, :], in1=ga_s[:], op=ALU.mult), reads=[pba, b_gas], writes=[b_m1])
                K.op("dve", lambda e: e.tensor_tensor(out=m2[:], in0=pb_[:, :], in1=gb_s[:], op=ALU.mult), reads=[pbb, b_gbs], writes=[b_m2])
                K.op("pool", lambda e: e.tensor_tensor(out=mrg[:, nn, :], in0=m1[:], in1=m2[:], op=ALU.add), reads=[b_m1, b_m2], writes=[b_mrg])
            for tt in range(4):
                ti = sc * 4 + tt
                tk = slice(ti * 128, (ti + 1) * 128)
                xd, b_xd = xds[nt_ % 2]
                hd, b_hd = hds[nt_ % 2]
                nt_ += 1
                load(xd[:], x_d[tk, :], b_xd)
                for mh in range(2):
                    py, pby = PS[4 + mh]
                    for nn in range(8):
                        K.op("pe", lambda e: e.matmul(py[:, :], lhsT=mrg[:, nn, tt * 128:(tt + 1) * 128], rhs=wo[:, nn, mh * 512:(mh + 1) * 512],
                                                      start=(nn == 0), stop=(nn == 7)), reads=[b_mrg, b_wo], writes=[pby])
                    K.op("dve", lambda e: e.tensor_tensor(out=hd[:, mh * 512:(mh + 1) * 512], in0=py[:, :], in1=modbc[:, 0, mh * 512:(mh + 1) * 512],
                                                          op=ALU.mult), reads=[pby, b_modbc], writes=[b_hd])
                K.op("pool", lambda e: e.tensor_tensor(out=hd[:], in0=hd[:], in1=xd[:], op=ALU.add), reads=[b_hd, b_xd], writes=[b_hd])
                store(H_d[tk, :], hd[:], b_hd, dH)
        K.barrier()
    if stop_after == "D":
        return finish()

    with ExitStack() as st:
        fnw, b_fnw = sb(st, "fnw", [128, D])
        load(fnw[:], fnw_d, b_fnw)
        subT, b_subT = sb(st, "subT", [64, 16, 128])
        load(subT[:], subT_d, b_subT)
        wq, b_wq = sb(st, "wq_bf", [128, 8, D], BF16)
        with ExitStack() as st2:
            wstage, b_wstage = sb(st2, "wstageE", [128, 8, D])
            load(wstage[:], wq_d.rearrange("(k p) n -> p k n", p=128), b_wstage)
            K.op("pool", lambda e: e.tensor_copy(out=wq[:], in_=wstage[:]), reads=[b_wstage], writes=[b_wq])
            K.barrier()
        hts = [sb(st, "ht%d" % i, [128, D]) for i in range(2)]
        n2, b_n2 = sb(st, "n2", [128, D])
        n2T, b_n2T = sb(st, "n2T", [128, 8, 128], BF16)
        qpT, b_qpT = sb(st, "qpT", [64, 16, 128])
        s_sb, b_ssb = sb(st, "s_sb", [128, 16, 128])
        v16, b_v16 = sb(st, "v16", [128, 16, 16])
        i16, b_i16 = sb(st, "i16", [128, 16, 16], U32)
        i16f, b_i16f = sb(st, "i16f", [128, 16, 16])
        wk2, b_wk2 = sb(st, "wk2", [128, 128])
        cand, b_cand = sb(st, "cand", [128, 8, 16, 16])
        eid, b_eid = sb(st, "eid", [128, 8, 16, 16])
        wk3, b_wk3 = sb(st, "wk3", [128, 256])
        jk3, b_jk3 = sb(st, "jk3", [128, 256])
        vals, b_vals = sb(st, "vals", [128, 8, 16])
        eids, b_eids = sb(st, "eids", [128, 128])
        idx, b_idx = sb(st, "idx", [128, 128], I32)
        gl, b_gl = sb(st, "gl", [128, 8, 16])
        gs, b_gs = sb(st, "gs", [128, 16])
        av, b_av = sb(st, "av", [128, 128])
        coef, b_coef = sb(st, "coef", [128, 128])
        acc, b_acc = sb(st, "acc", [128, D])
        jk, b_jk = sb(st, "jkE", [128, D])
        stE, b_stE = sb(st, "stE", [128, 8])
        outs = [sb(st, "outt%d" % i, [128, D]) for i in range(2)]
        NG = 16
        gbuf = [sb(st, "gath%d" % i, [128, 2 * D], BF16) for i in range(NG)]
        tvs = [sb(st, "tv%d" % i, [128, D], BF16) for i in range(3)]
        coefs = [sb(st, "coefg%d" % i, [128, 8]) for i in range(2)]
        ident_bf, b_identbf = sb(st, "ident_bf", [128, 128], BF16)
        K.op("pool", lambda e: e.tensor_copy(out=ident_bf[:], in_=ident[:]), reads=[b_ident], writes=[b_identbf])
        ngat = 0
        for i in range(NT):
            tk = slice(i * 128, (i + 1) * 128)
            ht, b_ht = hts[i % 2]
            ot, b_ot = outs[i % 2]
            load(ht[:], H_d[tk, :], b_ht, dH)
            K.op("act", lambda e: e.activation(out=jk[:], in_=ht[:], func=AF.Square, accum_out=stE[:, 0:1]), reads=[b_ht], writes=[b_jk, b_stE])
            K.op("act", lambda e: e.activation(out=stE[:, 1:2], in_=stE[:, 0:1], func=AF.Sqrt, bias=EPS, scale=1.0 / D), reads=[b_stE], writes=[b_stE])
            K.op("dve", lambda e: e.reciprocal(out=stE[:, 2:3], in_=stE[:, 1:2]), reads=[b_stE], writes=[b_stE])
            K.op("dve", lambda e: e.scalar_tensor_tensor(out=n2[:], in0=ht[:], scalar=stE[:, 2:3], in1=modbc[:, 2, :], op0=ALU.mult, op1=ALU.mult),
                 reads=[b_ht, b_stE, b_modbc], writes=[b_n2])
            K.op("pool", lambda e: e.tensor_tensor(out=n2[:], in0=n2[:], in1=modbc[:, 1, :], op=ALU.add), reads=[b_n2, b_modbc], writes=[b_n2])
            for k in range(8):
                pt, pb = PS[k // 4]
                K.op("pe", lambda e: e.transpose(pt[:, (k % 4) * 128:(k % 4 + 1) * 128], n2[:, k * 128:(k + 1) * 128], ident[:]),
                     reads=[b_n2, b_ident], writes=[pb])
            for k2 in range(2):
                pt, pb = PS[k2]
                K.op("act", lambda e: e.activation(out=n2T[:, k2 * 4:(k2 + 1) * 4, :].rearrange("p a b -> p (a b)"), in_=pt[:, :], func=AF.Copy),
                     reads=[pb], writes=[b_n2T])
            for hp in range(16):
                pq, pbq = PS[2 + (hp // 4) % 2]
                c0 = (hp % 4) * 128
                for k in range(8):
                    K.op("pe", lambda e: e.matmul(pq[0:64, c0:c0 + 128], lhsT=wq[:, k, hp * 64:(hp + 1) * 64], rhs=n2T[:, k, :],
                                                  start=(k == 0), stop=(k == 7)), reads=[b_wq, b_n2T], writes=[pbq])
                if hp % 4 == 3:
                    g4 = hp // 4
                    K.op("act", lambda e: e.activation(out=qpT[:, g4 * 4:(g4 + 1) * 4, :].rearrange("p a b -> p (a b)"), in_=pq[0:64, :], func=AF.Copy),
                         reads=[pbq], writes=[b_qpT])
            for hp in range(16):
                psc, pbsc = PS[4 + (hp // 4) % 2]
                c0 = (hp % 4) * 128
                K.op("pe", lambda e: e.matmul(psc[:, c0:c0 + 128], lhsT=qpT[:, hp, :], rhs=subT[:, hp, :], start=True, stop=True),
                     reads=[b_qpT, b_subT], writes=[pbsc])
                if hp % 4 == 3:
                    g4 = hp // 4
                    K.op("act", lambda e: e.activation(out=s_sb[:, g4 * 4:(g4 + 1) * 4, :].rearrange("p a b -> p (a b)"), in_=psc[:, :], func=AF.Copy),
                         reads=[pbsc], writes=[b_ssb])
            for hp in range(16):
                K.op("dve", lambda e: e.max(out=v16[:, hp, 0:8], in_=s_sb[:, hp, :]), reads=[b_ssb], writes=[b_v16])
                K.op("dve", lambda e: e.max_index(out=i16[:, hp, 0:8], in_max=v16[:, hp, 0:8], in_values=s_sb[:, hp, :]),
                     reads=[b_ssb, b_v16], writes=[b_i16])
                K.op("dve", lambda e: e.match_replace(out=wk2[:], in_to_replace=v16[:, hp, 0:8], in_values=s_sb[:, hp, :], imm_value=NEG),
                     reads=[b_ssb, b_v16], writes=[b_wk2])
                K.op("dve", lambda e: e.max(out=v16[:, hp, 8:16], in_=wk2[:]), reads=[b_wk2], writes=[b_v16])
                K.op("dve", lambda e: e.max_index(out=i16[:, hp, 8:16], in_max=v16[:, hp, 8:16], in_values=wk2[:]),
                     reads=[b_wk2, b_v16], writes=[b_i16])
            K.op("dve", lambda e: e.tensor_copy(out=i16f[:], in_=i16[:]), reads=[b_i16], writes=[b_i16f])
            v16v = v16[:].rearrange("p (h two) k -> p h two k", two=2)
            i16v = i16f[:].rearrange("p (h two) k -> p h two k", two=2)
            K.op("dve", lambda e: e.tensor_scalar(out=i16v[:, :, 0, :], in0=i16v[:, :, 0, :], scalar1=128.0, scalar2=None, op0=ALU.mult),
                 reads=[b_i16f], writes=[b_i16f])
            for a in range(16):
                K.op("dve", lambda e: e.tensor_tensor(out=cand[:, :, a, :], in0=v16v[:, :, 1, :], in1=v16v[:, :, 0, a:a + 1].to_broadcast([128, 8, 16]),
                                                      op=ALU.add), reads=[b_v16], writes=[b_cand])
                K.op("pool", lambda e: e.tensor_tensor(out=eid[:, :, a, :], in0=i16v[:, :, 1, :], in1=i16v[:, :, 0, a:a + 1].to_broadcast([128, 8, 16]),
                                                       op=ALU.add), reads=[b_i16f], writes=[b_eid])
            for hh in range(8):
                ch = cand[:, hh].rearrange("p a b -> p (a b)")
                eh = eid[:, hh].rearrange("p a b -> p (a b)")
                K.op("dve", lambda e: e.max(out=vals[:, hh, 0:8], in_=ch), reads=[b_cand], writes=[b_vals])
                K.op("dve", lambda e: e.match_replace(out=wk3[:], in_to_replace=vals[:, hh, 0:8], in_values=ch, imm_value=NEG),
                     reads=[b_cand, b_vals], writes=[b_wk3])
                K.op("dve", lambda e: e.max(out=vals[:, hh, 8:16], in_=wk3[:]), reads=[b_wk3], writes=[b_vals])
                for j in range(16):
                    K.op("dve", lambda e: e.scalar_tensor_tensor(out=jk3[:], in0=ch, scalar=vals[:, hh, j:j + 1], in1=eh, op0=ALU.is_equal, op1=ALU.mult,
                                                                 accum_out=eids[:, hh * 16 + j:hh * 16 + j + 1]),
                         reads=[b_cand, b_vals, b_eid], writes=[b_jk3, b_eids])
            K.op("dve", lambda e: e.tensor_scalar(out=eids[:], in0=eids[:], scalar1=16383.0, scalar2=0.0, op0=ALU.min, op1=ALU.max),
                 reads=[b_eids], writes=[b_eids])
            K.op("dve", lambda e: e.tensor_copy(out=idx[:], in_=eids[:]), reads=[b_eids], writes=[b_idx])
            K.op("dve", lambda e: e.tensor_tensor(out=gl[:], in0=vals[:], in1=vals[:, :, 0:1].to_broadcast([128, 8, 16]), op=ALU.subtract),
                 reads=[b_vals], writes=[b_gl])
            K.op("act", lambda e: e.activation(out=gl[:], in_=gl[:], func=AF.Exp), reads=[b_gl], writes=[b_gl])
            K.op("dve", lambda e: e.tensor_reduce(out=gs[:, 0:8], in_=gl[:], axis=AX.X, op=ALU.add), reads=[b_gl], writes=[b_gs])
            K.op("dve", lambda e: e.reciprocal(out=gs[:, 8:16], in_=gs[:, 0:8]), reads=[b_gs], writes=[b_gs])
            K.op("dve", lambda e: e.tensor_tensor(out=gl[:], in0=gl[:], in1=gs[:, 8:16].rearrange("p (h o) -> p h o", o=1).to_broadcast([128, 8, 16]),
                                                  op=ALU.mult), reads=[b_gl, b_gs], writes=[b_gl])
            for grp in range(16):
                gsl = []
                for s_ in range(8):
                    j = grp * 8 + s_
                    gb_, bgb = gbuf[ngat % NG]
                    ngat += 1
                    gsl.append((gb_, bgb))
                    K.dma(lambda e: e.indirect_dma_start(out=gb_[:], out_offset=None, in_=UV_d,
                                                         in_offset=bass.IndirectOffsetOnAxis(ap=idx[:, j:j + 1], axis=0)),
                          bgb, reads=[b_idx, dUV], writes=[bgb], q="pool")
                    K.op("dve", lambda e: e.scalar_tensor_tensor(out=jk[:], in0=gb_[:, 0:D], scalar=1.0, in1=n2[:], op0=ALU.mult, op1=ALU.mult,
                                                                 accum_out=av[:, j:j + 1]), reads=[bgb, b_n2], writes=[b_jk, b_av])
                g8 = slice(grp * 8, (grp + 1) * 8)
                cf, b_cf = coefs[grp % 2]
                K.op("act", lambda e: e.activation(out=cf[:], in_=av[:, g8], func=AF.Gelu), reads=[b_av], writes=[b_cf])
                K.op("dve", lambda e: e.tensor_tensor(out=cf[:], in0=cf[:], in1=gl[:].rearrange("p h k -> p (h k)")[:, g8], op=ALU.mult),
                     reads=[b_cf, b_gl], writes=[b_cf])
                for s_ in range(8):
                    j = grp * 8 + s_
                    gb_, bgb = gsl[s_]
                    tv, b_tv = tvs[j % 3]
                    K.op("act", lambda e: e.activation(out=tv[:], in_=gb_[:, D:2 * D], func=AF.Copy, scale=cf[:, s_:s_ + 1]),
                         reads=[bgb, b_cf], writes=[b_tv])
                    for mh in range(2):
                        K.op("pe", lambda e: e.matmul(PS[6 + mh][0][:, :], lhsT=ident_bf[:], rhs=tv[:, mh * 512:(mh + 1) * 512],
                                                      start=(j == 0), stop=(j == 127)), reads=[b_tv, b_identbf], writes=[PS[6 + mh][1]])
            for mh in range(2):
                K.op("dve", lambda e: e.tensor_tensor(out=acc[:, mh * 512:(mh + 1) * 512], in0=PS[6 + mh][0][:, :], in1=modbc[:, 3, mh * 512:(mh + 1) * 512],
                                                      op=ALU.mult), reads=[PS[6 + mh][1], b_modbc], writes=[b_acc])
            K.op("pool", lambda e: e.tensor_tensor(out=acc[:], in0=acc[:], in1=ht[:], op=ALU.add), reads=[b_acc, b_ht], writes=[b_acc])
            K.op("act", lambda e: e.activation(out=jk[:], in_=acc[:], func=AF.Square, accum_out=stE[:, 4:5]), reads=[b_acc], writes=[b_jk, b_stE])
            K.op("act", lambda e: e.activation(out=stE[:, 5:6], in_=stE[:, 4:5], func=AF.Sqrt, bias=EPS, scale=1.0 / D), reads=[b_stE], writes=[b_stE])
            K.op("dve", lambda e: e.reciprocal(out=stE[:, 6:7], in_=stE[:, 5:6]), reads=[b_stE], writes=[b_stE])
            K.op("dve", lambda e: e.scalar_tensor_tensor(out=ot[:], in0=acc[:], scalar=stE[:, 6:7], in1=fnw[:], op0=ALU.mult, op1=ALU.mult),
                 reads=[b_acc, b_stE, b_fnw], writes=[b_ot])
            store(out_d[tk, :], ot[:], b_ot, dOUT)
    return finish()


_NC_CACHE = {}


def kernel(**inputs):
    S = inputs["x"].shape[1]
    B = inputs["x"].shape[0]
    if S not in _NC_CACHE:
        _NC_CACHE[S] = build_nc(S)[0]
    nc = _NC_CACHE[S]
    in_maps = [host_layout(inputs, b, S) for b in range(B)]
    res = run_bass_kernel_spmd(nc, in_maps, core_ids=list(range(B)))
    return np.stack([np.asarray(r["out"], dtype=np.float32) for r in res.results], axis=0)
```

```python
import numpy as np
from contextlib import ExitStack
import concourse.bass as bass
import concourse.mybir as mybir
from concourse.bass_utils import run_bass_kernel_spmd

F32 = mybir.dt.float32
BF16 = mybir.dt.bfloat16
I32 = mybir.dt.int32
F32R = mybir.dt.float32r
U32 = mybir.dt.uint32
AF = mybir.ActivationFunctionType
ALU = mybir.AluOpType
AX = mybir.AxisListType

D = 1024
EPS = 1e-6
NEG = -1.0e30


class Buf:
    def __init__(self, name):
        self.name = name
        self.w = []
        self.r = []
        self.dsem = None


class _Rec:
    def __getattr__(self, name):
        return lambda *a, **kw: (name, a, kw)


_REC = _Rec()


class Sched:
    def __init__(self, nc, stack):
        self.nc = nc
        self.stack = stack
        self.eng = {"pe": nc.tensor, "act": nc.scalar, "dve": nc.vector, "pool": nc.gpsimd, "sp": nc.sync}
        self.sems = {}
        self.cnt = {}
        for e in self.eng:
            self.sems["e_" + e] = stack.enter_context(nc.semaphore("sem_" + e))
            self.cnt["e_" + e] = 0
        self.seen = {e: {} for e in self.eng}
        self.prog = {e: [] for e in self.eng}
        self.ndsem = 0
        self.n_ins = 0

    def dma_sem(self, buf):
        if buf.dsem is None:
            key = "d_%d" % self.ndsem
            self.ndsem += 1
            self.sems[key] = self.stack.enter_context(self.nc.semaphore("dsem_%d" % (self.ndsem - 1)))
            self.cnt[key] = 0
            buf.dsem = key
        return buf.dsem

    def _waits(self, e, deps):
        best = {}
        for (k, v) in deps:
            if e == "pe" and k == "e_pe":
                continue
            if best.get(k, 0) < v:
                best[k] = v
        out = []
        for k, v in best.items():
            if self.seen[e].get(k, 0) < v:
                self.seen[e][k] = v
                out.append((k, v))
        return out

    def op(self, e, fn, reads=(), writes=()):
        deps = []
        for b in reads:
            deps += b.w
        for b in writes:
            deps += b.w + b.r
        waits = self._waits(e, deps)
        key = "e_" + e
        self.cnt[key] += 1
        tok = (key, self.cnt[key])
        self.prog[e].append((waits, fn(_REC), key, 1))
        for b in writes:
            b.w = [tok]
            b.r = []
        for b in reads:
            if b not in writes:
                b.r.append(tok)
        self.n_ins += 1

    def dma(self, fn, sbuf, reads=(), writes=(), q="sp"):
        key = self.dma_sem(sbuf)
        deps = [(key, self.cnt[key])] if self.cnt[key] else []
        for b in reads:
            deps += b.w
        for b in writes:
            deps += b.w + b.r
        waits = self._waits(q, deps)
        self.cnt[key] += 16
        tok = (key, self.cnt[key])
        self.prog[q].append((waits, fn(_REC), key, 16))
        for b in writes:
            b.w = [tok]
            b.r = []
        for b in reads:
            if b not in writes:
                b.r.append(tok)
        self.n_ins += 1

    def barrier(self):
        allk = [(k, v) for k, v in self.cnt.items() if v > 0]
        for e in self.eng:
            waits = self._waits(e, allk)
            if waits:
                self.prog[e].append((waits, None, None, 0))

    def emit(self):
        nc = self.nc
        sems = self.sems
        prog = self.prog
        with nc.Block() as block:
            def run(e, engine):
                for waits, fn, key, inc in prog[e]:
                    if fn is None:
                        for (k, v) in waits:
                            engine.wait_ge(sems[k], v)
                        continue
                    for (k, v) in waits[1:]:
                        engine.wait_ge(sems[k], v)
                    ins = getattr(engine, fn[0])(*fn[1], **fn[2])
                    if waits:
                        ins._wait_ge(sems[waits[0][0]], waits[0][1])
                    ins.then_inc(sems[key], inc)

            @block.tensor
            def _(eng):
                run("pe", eng)

            @block.scalar
            def _(eng):
                run("act", eng)

            @block.vector
            def _(eng):
                run("dve", eng)

            @block.gpsimd
            def _(eng):
                run("pool", eng)

            @block.sync
            def _(eng):
                run("sp", eng)


A_HEADS, KV_HEADS, HD = 8, 2, 64
IDX_HEADS = 16
B_HEADS = 8
OFF_QA = 0
OFF_KA = 512
OFF_VA = 640
OFF_QI = 768
OFF_KI = 1792
OFF_WI = 1856
OFF_QB = 1872
OFF_KB = 2384
OFF_VB = 2896
OFF_ZB = 3408
OFF_BB = 3920
OFF_AB = 3928
OFF_GA = 3936
OFF_GB = 4960
IN_WIDTH = 5984

ROT_STARTS = [OFF_QA + 64 * h for h in range(8)] + [OFF_KA + 64 * g for g in range(2)] + \
             [OFF_QI + 64 * h for h in range(16)] + [OFF_KI]
N_ROT = len(ROT_STARTS)
CONV_STARTS = [OFF_QB + 64 * h for h in range(24)]
SWAP64 = np.concatenate([np.arange(8, 16), np.arange(0, 8), np.arange(16, 64)])


def host_layout(inputs, b, S):
    f = np.float32
    x = np.ascontiguousarray(inputs["x"][b, :S]).astype(f)
    c = inputs["c"][b].astype(f)
    w_in = inputs["w_in"][0].astype(f)
    m = {}
    m["x"] = x
    m["c_col"] = np.ascontiguousarray(c.reshape(8, 128).T)
    m["pos_rep"] = np.ascontiguousarray(np.broadcast_to(inputs["positions"][b, :S].astype(np.int32)[None, :], (64, S)))
    m["w_ada"] = np.ascontiguousarray(inputs["w_ada"][0].astype(f))
    b_ada = inputs["b_ada"][0].astype(f)
    m["b_ada_col"] = np.ascontiguousarray(b_ada.reshape(48, 128).T)
    m["b_ada_rep"] = np.ascontiguousarray(np.broadcast_to(b_ada[None, :], (128, 6144)))
    rot_cols = np.concatenate([np.arange(s, s + 64) for s in ROT_STARTS])
    rot_cols_sw = np.concatenate([s + SWAP64 for s in ROT_STARTS])
    m["w_rot"] = np.ascontiguousarray(w_in[:, rot_cols])
    m["w_rot_sw"] = np.ascontiguousarray(w_in[:, rot_cols_sw])
    m["w_conv"] = np.ascontiguousarray(w_in[:, OFF_QB:OFF_QB + 1536])
    m["w_gate"] = np.ascontiguousarray(w_in[:, OFF_GA:OFF_GA + 2048])
    m["w_tok"] = np.ascontiguousarray(np.concatenate(
        [w_in[:, OFF_VA:OFF_VA + 128], w_in[:, OFF_WI:OFF_WI + 16]], axis=1))
    m["w_z"] = np.ascontiguousarray(w_in[:, OFF_ZB:OFF_ZB + 512])
    m["w_bg"] = np.ascontiguousarray(w_in[:, OFF_BB:OFF_BB + 16])
    conv_w = inputs["conv_w"][0].astype(f)
    m["conv_col"] = np.ascontiguousarray(conv_w.T.reshape(24, 64, 4).transpose(1, 0, 2))
    m["a_log_rep"] = np.ascontiguousarray(np.broadcast_to(inputs["a_log"][0].astype(f)[None, :], (64, 8)))
    m["dt_bias_rep"] = np.ascontiguousarray(np.broadcast_to(inputs["dt_bias"][0].astype(f)[None, :], (64, 8)))
    m["norm_b_rep"] = np.ascontiguousarray(np.broadcast_to(inputs["norm_b_w"][0].astype(f)[None, :], (64, 64)))
    m["w_pa"] = np.ascontiguousarray(inputs["w_pa"][0].astype(f).reshape(8, 64, 1024).transpose(1, 0, 2))
    m["w_pb"] = np.ascontiguousarray(inputs["w_pb"][0].astype(f).reshape(8, 64, 1024).transpose(1, 0, 2))
    m["w_o"] = np.ascontiguousarray(inputs["w_o"][0].astype(f))
    m["peer_wq"] = np.ascontiguousarray(inputs["peer_wq"][0].astype(f))
    sk = inputs["peer_subkeys"][0].astype(f)
    m["subT"] = np.ascontiguousarray(sk.transpose(3, 0, 1, 2).reshape(64, 16, 128))
    m["peer_u"] = np.ascontiguousarray(inputs["peer_u"][0].astype(f))
    m["peer_v"] = np.ascontiguousarray(inputs["peer_v"][0].astype(f))
    m["fnw_rep"] = np.ascontiguousarray(np.broadcast_to(inputs["final_norm_w"].astype(f)[None, :], (128, 1024)))
    m.update(host_consts())
    return m


def host_consts():
    f = np.float32
    k = {}
    k["ident"] = np.eye(128, dtype=f)
    half = 8
    inv_freq = np.power(f(500000.0), -np.arange(half, dtype=f) * f(2.0 / 16)).astype(f)
    fr = np.zeros((64, 1), f)
    sg = np.zeros((64, 1), f)
    fr[0:8, 0] = inv_freq
    fr[8:16, 0] = inv_freq
    sg[0:8, 0] = -1.0
    sg[8:16, 0] = 1.0
    k["freq_col"] = fr
    k["sign_col"] = sg
    p = np.arange(128)
    k["cmask"] = np.where(p[None, :] <= p[:, None], 0.0, NEG).astype(f)
    i = np.arange(64)
    k["ut64"] = (i[:, None] <= i[None, :]).astype(f)
    k["sl64"] = (i[:, None] > i[None, :]).astype(f)
    k["il64"] = (i[:, None] >= i[None, :]).astype(f)
    k["iu64"] = (i[:, None] <= i[None, :]).astype(f)
    k["su64"] = (i[:, None] < i[None, :]).astype(f)
    return k


def build_nc(S, dbg=False, stop_after=None):
    NT = S // 128
    NS = S // 512
    NCH = S // 64
    TOPK = min(256, S // 4)
    nc = bass.Bass("TRN2", target_bir_lowering=False)
    stack = ExitStack()
    K = Sched(nc, stack)

    def din(name, shape, dt=F32):
        return nc.dram_tensor(name, list(shape), dt, kind="ExternalInput").ap()

    def dscr(name, shape, dt=F32):
        return nc.dram_tensor(name, list(shape), dt).ap()

    x_d = din("x", [S, D])
    c_col_d = din("c_col", [128, 8])
    pos_d = din("pos_rep", [64, S], I32)
    w_ada_d = din("w_ada", [D, 6 * D])
    b_ada_col_d = din("b_ada_col", [128, 48])
    b_ada_rep_d = din("b_ada_rep", [128, 6 * D])
    w_rot_d = din("w_rot", [D, N_ROT * 64])
    w_rot_sw_d = din("w_rot_sw", [D, N_ROT * 64])
    w_conv_d = din("w_conv", [D, 1536])
    w_gate_d = din("w_gate", [D, 2048])
    w_tok_d = din("w_tok", [D, 144])
    w_z_d = din("w_z", [D, 512])
    w_bg_d = din("w_bg", [D, 16])
    conv_col_d = din("conv_col", [64, 24, 4])
    a_log_d = din("a_log_rep", [64, 8])
    dt_bias_d = din("dt_bias_rep", [64, 8])
    norm_b_d = din("norm_b_rep", [64, 64])
    w_pa_d = din("w_pa", [64, 8, D])
    w_pb_d = din("w_pb", [64, 8, D])
    w_o_d = din("w_o", [D, D])
    wq_d = din("peer_wq", [D, D])
    subT_d = din("subT", [64, 16, 128])
    u_d = din("peer_u", [16384, D])
    v_d = din("peer_v", [16384, D])
    fnw_d = din("fnw_rep", [128, D])
    ident_d = din("ident", [128, 128])
    freq_d = din("freq_col", [64, 1])
    sign_d = din("sign_col", [64, 1])
    cmask_d = din("cmask", [128, 128])
    ut64_d = din("ut64", [64, 64])
    sl64_d = din("sl64", [64, 64])
    il64_d = din("il64", [64, 64])
    iu64_d = din("iu64", [64, 64])
    su64_d = din("su64", [64, 64])
    out_d = nc.dram_tensor("out", [S, D], F32, kind="ExternalOutput").ap()

    ROT_d = dscr("ROT", [N_ROT, 64, S], BF16)
    CONV_d = dscr("CONVO", [24, 64, S], F32)
    GATE_d = dscr("GATE", [16, 128, S], BF16)
    VW_d = dscr("VW", [S, 144])
    ZS_d = dscr("ZS", [S, 512])
    BG_d = dscr("BG", [S, 16])
    OA_d = dscr("OA", [8, 64, S], BF16)
    OB_d = dscr("OB", [8, 64, S], BF16)
    H_d = dscr("H", [S, D])
    UV_d = dscr("UV", [16384, 2 * D], BF16)
    dUV = Buf("UV")
    dROT, dCONV, dGATE, dVW, dZS, dBG, dOA, dOB, dH = [Buf(n) for n in
                                                      ("ROT", "CONV", "GATE", "VW", "ZS", "BG", "OA", "OB", "H")]
    dOUT = Buf("OUT")
    dIN = Buf("IN")

    dbg_out = {}

    def dbg_tensor(name, shape, dt=F32):
        t = nc.dram_tensor(name, list(shape), dt, kind="ExternalOutput").ap()
        dbg_out[name] = t
        return t

    def sb(st, name, shape, dt=F32):
        t = st.enter_context(nc.sbuf_tensor("sb_" + name, list(shape), dt))
        return t, Buf(name)

    def ps(st, name, shape=(128, 512), dt=F32):
        t = st.enter_context(nc.psum_tensor(name, list(shape), dt))
        return t, Buf(name)

    def load(dst_ap, src_ap, buf, src_buf=dIN, q="sp"):
        K.dma(lambda e, o=dst_ap, i=src_ap: e.dma_start(out=o, in_=i), buf, reads=[src_buf], writes=[buf], q=q)

    def store(dst_ap, src_ap, buf, dst_buf, q="sp"):
        K.dma(lambda e, o=dst_ap, i=src_ap: e.dma_start(out=o, in_=i), buf, reads=[buf], writes=[dst_buf], q=q)

    PS = [ps(stack, "psb%d" % i) for i in range(8)]

    ident, b_ident = sb(stack, "ident", [128, 128])
    load(ident[:], ident_d, b_ident)
    modcol, b_modcol = sb(stack, "modcol", [128, 48])
    modbc, b_modbc = sb(stack, "modbc", [128, 4, D])
    ones128, b_ones = sb(stack, "ones128", [128, 128])
    K.op("pool", lambda e: e.memset(ones128[:], 1.0), writes=[b_ones])

    uvsem = [Buf("uvsem%d" % i) for i in range(8)]
    for qi_ in range(4):
        rs = slice(qi_ * 4096, (qi_ + 1) * 4096)
        K.dma(lambda e: e.dma_start(out=UV_d[rs, 0:D], in_=u_d[rs, :]), uvsem[qi_], reads=[dIN], writes=[], q="pool")
        K.dma(lambda e: e.dma_start(out=UV_d[rs, D:2 * D], in_=v_d[rs, :]), uvsem[4 + qi_], reads=[dIN], writes=[], q="pool")
    for bsem in uvsem:
        dUV.w.append((bsem.dsem, K.cnt[bsem.dsem]))

    with ExitStack() as st:
        ccol, b_ccol = sb(st, "ccol", [128, 8])
        cact, b_cact = sb(st, "cact", [128, 8])
        crep, b_crep = sb(st, "crep", [128, 8, 128])
        bcol, b_bcol = sb(st, "bcol", [128, 48])
        wst = [sb(st, "wada%d" % i, [128, 8, 1024]) for i in range(2)]
        brep, b_brep = sb(st, "brep", [128, 1024])
        load(ccol[:], c_col_d, b_ccol)
        load(bcol[:], b_ada_col_d, b_bcol)
        K.op("act", lambda e: e.activation(out=cact[:], in_=ccol[:], func=AF.Silu), reads=[b_ccol], writes=[b_cact])
        for k in range(8):
            K.op("dve", lambda e, k=k: e.tensor_copy(out=crep[:, k, :], in_=cact[:, k:k + 1].to_broadcast([128, 128])),
                 reads=[b_cact], writes=[b_crep])
        w_ada_v = w_ada_d.rearrange("(k p) n -> p k n", p=128)
        pcol_t, pcol_b = PS[0]
        bc_slot = {2: 0, 3: 1, 4: 2, 5: 3}
        for gi in range(6):
            wt, wb = wst[gi % 2]
            load(wt[:], w_ada_v[:, :, gi * 1024:(gi + 1) * 1024], wb)
            for jj in range(8):
                j = gi * 8 + jj
                for k in range(8):
                    K.op("pe", lambda e, j=j, jj=jj, k=k, wt=wt: e.matmul(
                        pcol_t[:, j:j + 1], lhsT=wt[:, k, jj * 128:(jj + 1) * 128], rhs=cact[:, k:k + 1],
                        start=(k == 0), stop=(k == 7)), reads=[wb, b_cact], writes=[pcol_b])
            if gi in bc_slot:
                sl = bc_slot[gi]
                load(brep[:], b_ada_rep_d[:, gi * 1024:(gi + 1) * 1024], b_brep)
                for hh in range(2):
                    pt, pb = PS[1 + hh]
                    for k in range(8):
                        K.op("pe", lambda e, hh=hh, k=k, wt=wt, pt=pt: e.matmul(
                            pt[:, :], lhsT=crep[:, k, :], rhs=wt[:, k, hh * 512:(hh + 1) * 512],
                            start=(k == 0), stop=(k == 7)), reads=[wb, b_crep], writes=[pb])
                    K.op("dve", lambda e, hh=hh, sl=sl, pt=pt: e.tensor_tensor(
                        out=modbc[:, sl, hh * 512:(hh + 1) * 512], in0=pt[:, :], in1=brep[:, hh * 512:(hh + 1) * 512],
                        op=ALU.add), reads=[pb, b_brep], writes=[b_modbc])
        K.op("dve", lambda e: e.tensor_tensor(out=modcol[:], in0=pcol_t[:, 0:48], in1=bcol[:], op=ALU.add),
             reads=[pcol_b, b_bcol], writes=[b_modcol])
        K.op("dve", lambda e: e.tensor_scalar(out=modcol[:, 8:16], in0=modcol[:, 8:16], scalar1=1.0, scalar2=None,
                                              op0=ALU.add), reads=[b_modcol], writes=[b_modcol])
        K.op("dve", lambda e: e.tensor_scalar(out=modbc[:, 2, :], in0=modbc[:, 2, :], scalar1=1.0, scalar2=None,
                                              op0=ALU.add), reads=[b_modbc], writes=[b_modbc])
        K.barrier()

    if dbg:
        t = dbg_tensor("dbg_modcol", [128, 48])
        store(t, modcol[:], b_modcol, Buf("x"))
        t = dbg_tensor("dbg_modbc", [128, 4 * D])
        store(t, modbc[:].rearrange("p a d -> p (a d)"), b_modbc, Buf("x"))

    def finish():
        K.barrier()
        K.emit()
        stack.close()
        return nc, dbg_out

    if stop_after == "A0":
        return finish()

    with ExitStack() as st:
        n1T, _ = sb(st, "n1T", [128, 8, S], BF16)
        b_n1T = [Buf("n1T%d" % i) for i in range(NT)]
        xts = [sb(st, "xt%d" % i, [128, D]) for i in range(2)]
        junk, b_junk = sb(st, "junkA", [128, D])
        stat, b_stat = sb(st, "statA", [128, 4])
        psT = [PS[0], PS[1]]
        for i in range(NT):
            xt, xb = xts[i % 2]
            load(xt[:], x_d[i * 128:(i + 1) * 128, :], xb)
            K.op("act", lambda e, xt=xt: e.activation(out=junk[:], in_=xt[:], func=AF.Square, accum_out=stat[:, 0:1]),
                 reads=[xb], writes=[b_junk, b_stat])
            K.op("act", lambda e: e.activation(out=stat[:, 1:2], in_=stat[:, 0:1], func=AF.Sqrt, bias=EPS, scale=1.0 / D),
                 reads=[b_stat], writes=[b_stat])
            K.op("dve", lambda e: e.reciprocal(out=stat[:, 2:3], in_=stat[:, 1:2]), reads=[b_stat], writes=[b_stat])
            K.op("dve", lambda e, xt=xt: e.tensor_scalar(out=junk[:], in0=xt[:], scalar1=stat[:, 2:3], scalar2=None,
                                                         op0=ALU.mult), reads=[xb, b_stat], writes=[b_junk])
            for k in range(8):
                pt, pb = psT[k // 4]
                K.op("pe", lambda e, k=k, pt=pt: e.transpose(pt[:, (k % 4) * 128:(k % 4 + 1) * 128],
                                                             junk[:, k * 128:(k + 1) * 128], ident[:]),
                     reads=[b_junk, b_ident], writes=[pb])
            for k in range(8):
                pt, pb = psT[k // 4]
                K.op("act", lambda e, k=k, pt=pt, i=i: e.activation(
                    out=n1T[:, k, i * 128:(i + 1) * 128], in_=pt[:, (k % 4) * 128:(k % 4 + 1) * 128],
                    func=AF.Identity, bias=modcol[:, k:k + 1], scale=modcol[:, 8 + k:9 + k]),
                    reads=[pb, b_modcol], writes=[b_n1T[i]])
        if dbg:
            t = dbg_tensor("dbg_n1T", [128, 8 * S], BF16)
            K.barrier()
            bx = Buf("n1Tall")
            store(t, n1T[:].rearrange("p k s -> p (k s)"), bx, Buf("x"))
        if stop_after == "A1":
            return finish()

        wstg = [sb(st, "wstg%d" % i, [128, 8, 144]) for i in range(2)]
        wbfs = [sb(st, "wbf%d" % i, [128, 8, 144], BF16) for i in range(3)]
        wstg_big = sb(st, "wstg_big", [128, 8, 512])
        wbf_big = sb(st, "wbf_big", [128, 8, 512], BF16)
        cnt = {"stg": 0, "bf": 0}

        def load_w(src_ap, ncols):
            if ncols > 144:
                stg, bs = wstg_big
                wbf, bw = wbf_big
            else:
                stg, bs = wstg[cnt["stg"] % 2]
                wbf, bw = wbfs[cnt["bf"] % 3]
                cnt["stg"] += 1
                cnt["bf"] += 1
            load(stg[:, :, 0:ncols], src_ap.rearrange("(k p) n -> p k n", p=128), bs)
            K.op("pool", lambda e: e.tensor_copy(out=wbf[:, :, 0:ncols], in_=stg[:, :, 0:ncols]), reads=[bs], writes=[bw])
            return wbf, bw

        def allb():
            return list(b_n1T)

        tmpa, b_tmpa = sb(st, "tmpa", [64, 512])
        tmpb, b_tmpb = sb(st, "tmpb", [64, 512])
        st_rot = ExitStack()
        cosT, b_cos = sb(st_rot, "cosT", [64, S])
        sinT, b_sin = sb(st_rot, "sinT", [64, S])
        with ExitStack() as st2:
            SH = S // 2
            posi, b_posi = sb(st2, "posi", [64, SH], I32)
            ang, b_ang = sb(st2, "ang", [64, SH])
            t1, b_t1 = sb(st2, "rr_t1", [64, SH])
            t2, b_t2 = sb(st2, "rr_t2", [64, SH])
            fcol, b_fcol = sb(st2, "fcol", [64, 2])
            load(fcol[:, 0:1], freq_d, b_fcol)
            load(fcol[:, 1:2], sign_d, b_fcol)
            MAGIC = 12582912.0
            C1 = 6.28125
            C2 = float(2.0 * np.pi - 6.28125)
            PIL = 3.1415925
            for hf in range(2):
                cs = slice(hf * SH, (hf + 1) * SH)
                load(posi[:], pos_d[:, cs], b_posi)
                K.op("dve", lambda e: e.tensor_copy(out=ang[:], in_=posi[:]), reads=[b_posi], writes=[b_ang])
                K.op("dve", lambda e: e.tensor_scalar(out=ang[:], in0=ang[:], scalar1=fcol[:, 0:1], scalar2=None, op0=ALU.mult),
                     reads=[b_ang, b_fcol], writes=[b_ang])

                def sin_of(dst, b_dst, shift):
                    K.op("dve", lambda e: e.tensor_scalar(out=t2[:], in0=ang[:], scalar1=float(shift), scalar2=None, op0=ALU.add),
                         reads=[b_ang], writes=[b_t2])
                    K.op("dve", lambda e: e.tensor_scalar(out=t1[:], in0=t2[:], scalar1=float(1.0 / (2 * np.pi)), scalar2=MAGIC,
                                                          op0=ALU.mult, op1=ALU.add), reads=[b_t2], writes=[b_t1])
                    K.op("dve", lambda e: e.tensor_scalar(out=t1[:], in0=t1[:], scalar1=-MAGIC, scalar2=None, op0=ALU.add),
                         reads=[b_t1], writes=[b_t1])
                    K.op("dve", lambda e: e.scalar_tensor_tensor(out=t2[:], in0=t1[:], scalar=-C1, in1=t2[:], op0=ALU.mult,
                                                                 op1=ALU.add), reads=[b_t1, b_t2], writes=[b_t2])
                    K.op("dve", lambda e: e.scalar_tensor_tensor(out=t2[:], in0=t1[:], scalar=-C2, in1=t2[:], op0=ALU.mult,
                                                                 op1=ALU.add), reads=[b_t1, b_t2], writes=[b_t2])
                    K.op("dve", lambda e: e.tensor_scalar(out=t2[:], in0=t2[:], scalar1=PIL, scalar2=-PIL, op0=ALU.min,
                                                          op1=ALU.max), reads=[b_t2], writes=[b_t2])
                    K.op("act", lambda e: e.activation(out=dst[:, cs], in_=t2[:], func=AF.Sin), reads=[b_t2], writes=[b_dst])

                sin_of(sinT, b_sin, 0.0)
                sin_of(cosT, b_cos, np.pi / 2)
            K.op("dve", lambda e: e.tensor_scalar(out=sinT[:], in0=sinT[:], scalar1=fcol[:, 1:2], scalar2=None, op0=ALU.mult),
                 reads=[b_sin, b_fcol], writes=[b_sin])
            K.barrier()

        rot_o = [sb(st_rot, "rot_o%d" % i, [64, S], BF16) for i in range(2)]
        for gi in range(N_ROT):
            wa, bwa = load_w(w_rot_d[:, gi * 64:(gi + 1) * 64], 64)
            ws, bws = load_w(w_rot_sw_d[:, gi * 64:(gi + 1) * 64], 64)
            ro, bro = rot_o[gi % 2]
            for sc in range(NS):
                (p1, pb1), (p2, pb2) = PS[2 + (sc % 2) * 2], PS[3 + (sc % 2) * 2]
                tok = slice(sc * 512, (sc + 1) * 512)
                for k in range(8):
                    K.op("pe", lambda e, k=k, wa=wa, p1=p1, tok=tok: e.matmul(p1[0:64, :], lhsT=wa[:, k, 0:64], rhs=n1T[:, k, tok],
                                                                              start=(k == 0), stop=(k == 7)),
                         reads=[bwa] + allb(), writes=[pb1])
                for k in range(8):
                    K.op("pe", lambda e, k=k, ws=ws, p2=p2, tok=tok: e.matmul(p2[0:64, :], lhsT=ws[:, k, 0:64], rhs=n1T[:, k, tok],
                                                                              start=(k == 0), stop=(k == 7)),
                         reads=[bws] + allb(), writes=[pb2])
                K.op("dve", lambda e, p1=p1, tok=tok: e.tensor_tensor(out=tmpa[:], in0=p1[0:64, :], in1=cosT[:, tok], op=ALU.mult),
                     reads=[pb1, b_cos], writes=[b_tmpa])
                K.op("dve", lambda e, p2=p2, tok=tok: e.tensor_tensor(out=tmpb[:], in0=p2[0:64, :], in1=sinT[:, tok], op=ALU.mult),
                     reads=[pb2, b_sin], writes=[b_tmpb])
                K.op("pool", lambda e, ro=ro, tok=tok: e.tensor_tensor(out=ro[:, tok], in0=tmpa[:], in1=tmpb[:], op=ALU.add),
                     reads=[b_tmpa, b_tmpb], writes=[bro])
            store(ROT_d[gi], ro[:], bro, dROT)

        K.barrier()
        st_rot.close()
        st_cv = ExitStack()
        ccol, b_ccol = sb(st, "convcol", [64, 24, 4])
        load(ccol[:], conv_col_d, b_ccol)
        ones64 = ones128[0:64, 0:64]
        xc, b_xc = sb(st_cv, "xc", [64, S + 3])
        yc, b_yc = sb(st_cv, "yc", [64, S])
        conv_o = [sb(st_cv, "conv_o%d" % i, [64, S]) for i in range(1)]
        K.op("pool", lambda e: e.memset(xc[:, 0:3], 0.0), writes=[b_xc])
        for cg in range(24):
            wa, bwa = load_w(w_conv_d[:, cg * 64:(cg + 1) * 64], 64)
            co, bco = conv_o[0]
            for sc in range(NS):
                p1, pb1 = PS[2 + (sc % 2)]
                tok = slice(sc * 512, (sc + 1) * 512)
                for k in range(8):
                    K.op("pe", lambda e, k=k, wa=wa, p1=p1, tok=tok: e.matmul(p1[0:64, :], lhsT=wa[:, k, 0:64], rhs=n1T[:, k, tok],
                                                                              start=(k == 0), stop=(k == 7)),
                         reads=[bwa] + allb(), writes=[pb1])
                K.op("act", lambda e, p1=p1, sc=sc: e.activation(out=xc[:, 3 + sc * 512:3 + (sc + 1) * 512], in_=p1[0:64, :],
                                                                 func=AF.Copy), reads=[pb1], writes=[b_xc])
            K.op("dve", lambda e, cg=cg: e.tensor_scalar(out=yc[:], in0=xc[:, 3:S + 3], scalar1=ccol[:, cg, 3:4], scalar2=None,
                                                         op0=ALU.mult), reads=[b_xc, b_ccol], writes=[b_yc])
            for i in range(3):
                K.op("dve", lambda e, cg=cg, i=i: e.scalar_tensor_tensor(out=yc[:], in0=xc[:, i:S + i], scalar=ccol[:, cg, i:i + 1],
                                                                         in1=yc[:], op0=ALU.mult, op1=ALU.add),
                     reads=[b_xc, b_ccol, b_yc], writes=[b_yc])
            if cg >= 16:
                K.op("act", lambda e, co=co: e.activation(out=co[:], in_=yc[:], func=AF.Silu), reads=[b_yc], writes=[bco])
            else:
                K.op("act", lambda e: e.activation(out=yc[:], in_=yc[:], func=AF.Silu), reads=[b_yc], writes=[b_yc])
                for sc in range(NS):
                    p1, pb1 = PS[4 + (sc % 2)]
                    tok = slice(sc * 512, (sc + 1) * 512)
                    K.op("pool", lambda e, tok=tok: e.tensor_tensor(out=tmpa[:], in0=yc[:, tok], in1=yc[:, tok], op=ALU.mult),
                         reads=[b_yc], writes=[b_tmpa])
                    K.op("pe", lambda e, p1=p1: e.matmul(p1[0:64, :], lhsT=ones64, rhs=tmpa[:], start=True, stop=True),
                         reads=[b_ones, b_tmpa], writes=[pb1])
                    K.op("act", lambda e, p1=p1: e.activation(out=tmpb[:], in_=p1[0:64, :], func=AF.Sqrt, bias=EPS, scale=1.0),
                         reads=[pb1], writes=[b_tmpb])
                    K.op("dve", lambda e: e.reciprocal(out=tmpb[:], in_=tmpb[:]), reads=[b_tmpb], writes=[b_tmpb])
                    qs = 0.125 if cg < 8 else 1.0
                    K.op("dve", lambda e, co=co, tok=tok, qs=qs: e.scalar_tensor_tensor(
                        out=co[:, tok], in0=yc[:, tok], scalar=qs, in1=tmpb[:], op0=ALU.mult, op1=ALU.mult),
                        reads=[b_yc, b_tmpb], writes=[bco])
            store(CONV_d[cg], co[:], bco, dCONV)

        K.barrier()
        st_cv.close()
        gate_o = [sb(st, "gate_o%d" % i, [128, S], BF16) for i in range(2)]
        for gc in range(16):
            wa, bwa = load_w(w_gate_d[:, gc * 128:(gc + 1) * 128], 128)
            go, bgo = gate_o[gc % 2]
            for sc in range(NS):
                p1, pb1 = PS[2 + (sc % 2)]
                tok = slice(sc * 512, (sc + 1) * 512)
                for k in range(8):
                    K.op("pe", lambda e, k=k, wa=wa, p1=p1, tok=tok: e.matmul(p1[:, :], lhsT=wa[:, k, 0:128], rhs=n1T[:, k, tok],
                                                                              start=(k == 0), stop=(k == 7)),
                         reads=[bwa] + allb(), writes=[pb1])
                K.op("act", lambda e, p1=p1, go=go, tok=tok: e.activation(out=go[:, tok], in_=p1[:, :], func=AF.Sigmoid),
                     reads=[pb1], writes=[bgo])
            store(GATE_d[gc], go[:], bgo, dGATE)

        wt_, bwt = load_w(w_tok_d, 144)
        wz_, bwz = load_w(w_z_d, 512)
        wg_, bwg = load_w(w_bg_d, 16)
        alog, b_alog = sb(st, "alog", [128, 8])
        dtb, b_dtb = sb(st, "dtb", [128, 8])
        load(alog[0:64, :], a_log_d, b_alog)
        load(alog[64:128, :], a_log_d, b_alog)
        load(dtb[0:64, :], dt_bias_d, b_dtb)
        load(dtb[64:128, :], dt_bias_d, b_dtb)
        K.op("act", lambda e: e.activation(out=alog[:], in_=alog[:], func=AF.Exp), reads=[b_alog], writes=[b_alog])
        K.op("dve", lambda e: e.tensor_scalar(out=alog[:], in0=alog[:], scalar1=-1.0, scalar2=None, op0=ALU.mult),
             reads=[b_alog], writes=[b_alog])
        vw_o = [sb(st, "vw_o%d" % i, [128, 144]) for i in range(2)]
        zs_o = [sb(st, "zs_o%d" % i, [128, 512]) for i in range(2)]
        bg_o = [sb(st, "bg_o%d" % i, [128, 16]) for i in range(2)]
        for i in range(NT):
            tk = slice(i * 128, (i + 1) * 128)
            (p1, pb1), (p2, pb2), (p3, pb3) = PS[2], PS[3], PS[4]
            vo, bvo = vw_o[i % 2]
            zo, bzo = zs_o[i % 2]
            bo, bbo = bg_o[i % 2]
            for k in range(8):
                K.op("pe", lambda e, k=k, tk=tk: e.matmul(p1[:, 0:144], lhsT=n1T[:, k, tk], rhs=wt_[:, k, 0:144],
                                                          start=(k == 0), stop=(k == 7)), reads=[bwt, b_n1T[i]], writes=[pb1])
            for k in range(8):
                K.op("pe", lambda e, k=k, tk=tk: e.matmul(p2[:, 0:512], lhsT=n1T[:, k, tk], rhs=wz_[:, k, 0:512],
                                                          start=(k == 0), stop=(k == 7)), reads=[bwz, b_n1T[i]], writes=[pb2])
            for k in range(8):
                K.op("pe", lambda e, k=k, tk=tk: e.matmul(p3[:, 0:16], lhsT=n1T[:, k, tk], rhs=wg_[:, k, 0:16],
                                                          start=(k == 0), stop=(k == 7)), reads=[bwg, b_n1T[i]], writes=[pb3])
            K.op("act", lambda e, vo=vo: e.activation(out=vo[:], in_=p1[:, 0:144], func=AF.Copy), reads=[pb1], writes=[bvo])
            K.op("act", lambda e, zo=zo: e.activation(out=zo[:], in_=p2[:, 0:512], func=AF.Silu), reads=[pb2], writes=[bzo])
            K.op("act", lambda e, bo=bo: e.activation(out=bo[:, 0:8], in_=p3[:, 0:8], func=AF.Sigmoid), reads=[pb3], writes=[bbo])
            K.op("dve", lambda e, bo=bo: e.tensor_tensor(out=bo[:, 8:16], in0=p3[:, 8:16], in1=dtb[:], op=ALU.add),
                 reads=[pb3, b_dtb, bbo], writes=[bbo])
            K.op("act", lambda e, bo=bo: e.activation(out=bo[:, 8:16], in_=bo[:, 8:16], func=AF.Exp), reads=[bbo], writes=[bbo])
            K.op("act", lambda e, bo=bo: e.activation(out=bo[:, 8:16], in_=bo[:, 8:16], func=AF.Ln, bias=1.0, scale=1.0),
                 reads=[bbo], writes=[bbo])
            K.op("dve", lambda e, bo=bo: e.tensor_tensor(out=bo[:, 8:16], in0=bo[:, 8:16], in1=alog[:], op=ALU.mult),
                 reads=[bbo, b_alog], writes=[bbo])
            store(VW_d[tk, :], vo[:], bvo, dVW)
            store(ZS_d[tk, :], zo[:], bzo, dZS)
            store(BG_d[tk, :], bo[:], bbo, dBG)
        K.barrier()

    if dbg:
        for nm, src in (("ROT", ROT_d), ("CONVO", CONV_d), ("GATE", GATE_d), ("VW", VW_d), ("ZS", ZS_d), ("BG", BG_d)):
            pass
    if stop_after == "A":
        return finish()

    with ExitStack() as st:
        kiT, b_kiT = sb(st, "kiT", [64, S], BF16)
        kaT, b_kaT = sb(st, "kaT", [64, 2, S], BF16)
        load(kiT[:], ROT_d[26], b_kiT, dROT)
        for g in range(2):
            load(kaT[:, g, :], ROT_d[8 + g], b_kaT, dROT)
        va_bf, b_va = sb(st, "va_bf", [128, NT, 128], BF16)
        wi_s, b_wi = sb(st, "wi_s", [128, NT, 16])
        with ExitStack() as st2:
            vw_f, b_vwf = sb(st2, "vw_f", [128, NT, 144])
            load(vw_f[:], VW_d.rearrange("(n p) c -> p n c", p=128), b_vwf, dVW)
            K.op("pool", lambda e: e.tensor_copy(out=va_bf[:], in_=vw_f[:, :, 0:128]), reads=[b_vwf], writes=[b_va])
            K.op("dve", lambda e: e.tensor_scalar(out=wi_s[:], in0=vw_f[:, :, 128:144], scalar1=1.0 / 32.0, scalar2=None,
                                                  op0=ALU.mult), reads=[b_vwf], writes=[b_wi])
            K.barrier()
        ones_bf, b_onesbf = sb(st, "ones_bf", [128, 64], BF16)
        K.op("pool", lambda e: e.memset(ones_bf[:], 1.0), writes=[b_onesbf])
        id4, b_id4 = sb(st, "id4", [128, 4, 128], BF16)
        for r4 in range(4):
            K.op("pool", lambda e, r4=r4: e.tensor_copy(out=id4[:, r4, :], in_=ident[:]), reads=[b_ident], writes=[b_id4])
        cmask, b_cmask = sb(st, "cmask", [128, 128])
        load(cmask[:], cmask_d, b_cmask)
        qi_ts = [sb(st, "qi_t%d" % i, [64, 16, 128], BF16) for i in range(2)]
        qa_ts = [sb(st, "qa_t%d" % i, [64, 8, 128], BF16) for i in range(2)]
        scores = [sb(st, "score%d" % i, [128, S]) for i in range(2)]
        work, b_work = sb(st, "work", [128, S])
        mnegs = [sb(st, "mneg%d" % i, [128, S], BF16) for i in range(2)]
        m8, b_m8 = sb(st, "m8", [128, 8])
        thr, b_thr = sb(st, "thr", [128, 8])
        rls = [sb(st, "rl%d" % i, [128, 512]) for i in range(2)]
        pTs = [sb(st, "pT%d" % i, [128, 512], BF16) for i in range(2)]
        rden, b_rden = sb(st, "rden", [64, 512])
        oas = [sb(st, "oa%d" % i, [64, 4, 128], BF16) for i in range(2)]
        wdiags = [sb(st, "wdiag%d" % i, [128, 16, 128], BF16) for i in range(2)]
        rlbs = [sb(st, "rlb%d" % i, [128, 512], BF16) for i in range(3)]
        ctr = {"nlog": 0, "nst": 0, "nch": 0}

        def tile_ctx(i):
            return dict(Si=128 * (i + 1), tk=slice(i * 128, (i + 1) * 128), qi=qi_ts[i % 2], qa=qa_ts[i % 2],
                        score=scores[i % 2], mneg=mnegs[i % 2], wd=wdiags[i % 2])

        def indexer(i):
            c = tile_ctx(i)
            Si, tk = c["Si"], c["tk"]
            qi_t, b_qi = c["qi"]
            qa_t, b_qa = c["qa"]
            score, b_score = c["score"]
            wd, b_wd = c["wd"]
            load(qi_t[:], ROT_d[10:26, :, tk].rearrange("h p t -> p h t"), b_qi, dROT)
            load(qa_t[:], ROT_d[0:8, :, tk].rearrange("h p t -> p h t"), b_qa, dROT)
            for h in range(16):
                K.op("pool", lambda e: e.tensor_scalar(out=wd[:, h, :], in0=ident[:], scalar1=wi_s[:, i, h:h + 1], scalar2=None, op0=ALU.mult),
                     reads=[b_ident, b_wi], writes=[b_wd])
            steps = [(c0, min(512, Si - c0), h) for c0 in range(0, Si, 512) for h in range(16)]
            state = {}

            def logits(k):
                c0, wc, h = steps[k]
                if h == 0:
                    state[c0] = PS[6 + ctr["nch"] % 2]
                    ctr["nch"] += 1
                pl, pbl = PS[ctr["nlog"] % 2]
                rl, brl = rlbs[ctr["nlog"] % 3]
                ctr["nlog"] += 1
                K.op("pe", lambda e: e.matmul(pl[:, 0:wc], lhsT=qi_t[:, h, :], rhs=kiT[:, c0:c0 + wc], start=True, stop=True),
                     reads=[b_qi, b_kiT], writes=[pbl])
                K.op("act", lambda e: e.activation(out=rl[:, 0:wc], in_=pl[:, 0:wc], func=AF.Relu), reads=[pbl], writes=[brl])
                return rl, brl

            def accum(k, rl, brl):
                c0, wc, h = steps[k]
                psc, pbsc = state[c0]
                K.op("pe", lambda e: e.matmul(psc[:, 0:wc], lhsT=wd[:, h, :], rhs=rl[:, 0:wc], start=(h == 0), stop=(h == 15)),
                     reads=[b_wd, brl], writes=[pbsc])
                if h == 15:
                    K.op("act", lambda e: e.activation(out=score[:, c0:c0 + wc], in_=psc[:, 0:wc], func=AF.Copy), reads=[pbsc], writes=[b_score])

            prev = logits(0)
            for k in range(len(steps)):
                nxt = logits(k + 1) if k + 1 < len(steps) else None
                accum(k, *prev)
                prev = nxt

        def threshold(i):
            c = tile_ctx(i)
            Si, tk = c["Si"], c["tk"]
            score, b_score = c["score"]
            mneg, b_mneg = c["mneg"]
            if Si > TOPK:
                K.op("dve", lambda e: e.tensor_reduce(out=thr[:, 2:3], in_=score[:, 0:Si], axis=AX.X, op=ALU.min), reads=[b_score], writes=[b_thr])
                K.op("dve", lambda e: e.tensor_reduce(out=thr[:, 3:4], in_=score[:, 0:Si], axis=AX.X, op=ALU.max), reads=[b_score], writes=[b_thr])
                NIT = 24
                K.op("dve", lambda e: e.tensor_tensor(out=score[:, tk], in0=score[:, tk], in1=cmask[:], op=ALU.add),
                     reads=[b_score, b_cmask], writes=[b_score])
                K.op("dve", lambda e: e.tensor_scalar(out=thr[:, 0:1], in0=thr[:, 2:3], scalar1=1.0, scalar2=None, op0=ALU.mult),
                     reads=[b_thr], writes=[b_thr])
                K.op("dve", lambda e: e.tensor_tensor(out=thr[:, 1:2], in0=thr[:, 3:4], in1=thr[:, 2:3], op=ALU.subtract),
                     reads=[b_thr], writes=[b_thr])
                for it in range(1, NIT + 1):
                    sc_ = float(2.0 ** (-it))
                    K.op("dve", lambda e: e.scalar_tensor_tensor(out=thr[:, 4:5], in0=thr[:, 1:2], scalar=sc_, in1=thr[:, 0:1],
                                                                 op0=ALU.mult, op1=ALU.add), reads=[b_thr], writes=[b_thr])
                    K.op("dve", lambda e: e.tensor_scalar(out=work[:, 0:Si], in0=score[:, 0:Si], scalar1=thr[:, 4:5], scalar2=0.0,
                                                          op0=ALU.is_ge, op1=ALU.add, accum_out=thr[:, 5:6]),
                         reads=[b_score, b_thr], writes=[b_work, b_thr])
                    K.op("dve", lambda e: e.tensor_scalar(out=thr[:, 6:7], in0=thr[:, 5:6], scalar1=float(TOPK) - 0.5, scalar2=sc_,
                                                          op0=ALU.is_ge, op1=ALU.mult), reads=[b_thr], writes=[b_thr])
                    K.op("dve", lambda e: e.scalar_tensor_tensor(out=thr[:, 0:1], in0=thr[:, 6:7], scalar=thr[:, 1:2], in1=thr[:, 0:1],
                                                                 op0=ALU.mult, op1=ALU.add), reads=[b_thr], writes=[b_thr])
                K.op("dve", lambda e: e.tensor_scalar(out=mneg[:, 0:Si], in0=score[:, 0:Si], scalar1=thr[:, 0:1], scalar2=-30000.0,
                                                      op0=ALU.is_lt, op1=ALU.mult), reads=[b_score, b_thr], writes=[b_mneg])
            else:
                K.op("dve", lambda e: e.tensor_tensor(out=score[:, tk], in0=score[:, tk], in1=cmask[:], op=ALU.add),
                     reads=[b_score, b_cmask], writes=[b_score])
                K.op("dve", lambda e: e.tensor_scalar(out=mneg[:, 0:Si], in0=score[:, 0:Si], scalar1=-1.0e29, scalar2=-30000.0,
                                                      op0=ALU.is_lt, op1=ALU.mult), reads=[b_score], writes=[b_mneg])

        def attention(i):
            c = tile_ctx(i)
            tk = c["tk"]
            qa_t, b_qa = c["qa"]
            mneg, b_mneg = c["mneg"]
            for g in range(2):
                pn, pbn = PS[4]
                pd, pbd = PS[5]

                def qk(j):
                    kk = slice(j * 128, (j + 1) * 128)
                    psT_, pbs = PS[2 + ctr["nst"] % 2]
                    pT, bpT = pTs[ctr["nst"] % 2]
                    ctr["nst"] += 1
                    K.op("pe", lambda e: e.matmul(psT_[:, :], lhsT=kaT[:, g, kk], rhs=qa_t[:, 4 * g:4 * g + 4, :], start=True, stop=False),
                         reads=[b_kaT, b_qa], writes=[pbs])
                    K.op("pe", lambda e: e.matmul(psT_[:, :], lhsT=mneg[:, kk], rhs=id4[:], start=False, stop=True),
                         reads=[b_mneg, b_id4], writes=[pbs])
                    K.op("act", lambda e: e.activation(out=pT[:], in_=psT_[:, :], func=AF.Exp, scale=0.125), reads=[pbs], writes=[bpT])
                    return pT, bpT

                def pv_(j, pT, bpT):
                    K.op("pe", lambda e: e.matmul(pn[0:64, :], lhsT=va_bf[:, j, g * 64:(g + 1) * 64], rhs=pT[:], start=(j == 0), stop=(j == i)),
                         reads=[b_va, bpT], writes=[pbn])
                    K.op("pe", lambda e: e.matmul(pd[0:64, :], lhsT=ones_bf[:], rhs=pT[:], start=(j == 0), stop=(j == i)),
                         reads=[b_onesbf, bpT], writes=[pbd])

                prev = qk(0)
                for j in range(i + 1):
                    nxt = qk(j + 1) if j + 1 <= i else None
                    pv_(j, *prev)
                    prev = nxt
                oa, boa = oas[g]
                K.op("dve", lambda e: e.reciprocal(out=rden[:], in_=pd[0:64, :]), reads=[pbd], writes=[b_rden])
                K.op("dve", lambda e: e.tensor_tensor(out=oa[:].rearrange("p h t -> p (h t)"), in0=pn[0:64, :], in1=rden[:], op=ALU.mult),
                     reads=[pbn, b_rden], writes=[boa])
                store(OA_d[4 * g:4 * g + 4, :, tk].rearrange("h p t -> p h t"), oa[:], boa, dOA)

        indexer(0)
        threshold(0)
        for i in range(NT):
            if i + 1 < NT:
                indexer(i + 1)
            attention(i)
            if i + 1 < NT:
                threshold(i + 1)
        K.barrier()
    if stop_after == "B":
        return finish()

    with ExitStack() as st:
        def cst(name, src):
            t, b = sb(st, name, [64, 64])
            load(t[:], src, b)
            return t, b
        ut64, b_ut = cst("ut64", ut64_d)
        sl64, b_sl = cst("sl64", sl64_d)
        iu64, b_iu = cst("iu64", iu64_d)
        normb, b_normb = cst("normb", norm_b_d)
        id64 = ident[0:64, 0:64]
        ones64f = ones128[0:64, 0:64]
        nones, b_nones = sb(st, "nones64", [64, 64])
        K.op("pool", lambda e: e.memset(nones[:], -1.0), writes=[b_nones])
        bg, b_bg = sb(st, "bg_all", [64, NCH, 16])
        load(bg[:], BG_d.rearrange("(n c) f -> c n f", c=64), b_bg, dBG)
        nbeta, b_nbeta = sb(st, "nbeta", [64, NCH, 8])
        K.op("dve", lambda e: e.tensor_scalar(out=nbeta[:], in0=bg[:, :, 0:8], scalar1=-1.0, scalar2=None, op0=ALU.mult),
             reads=[b_bg], writes=[b_nbeta])
        obT, b_obT = sb(st, "obT_all", [64, 8, S], BF16)
        Sst, b_S = sb(st, "Sstate", [64, 8, 64])
        K.op("pool", lambda e: e.memset(Sst[:], 0.0), writes=[b_S])

        def bc_h(t2d):
            return t2d.rearrange("p (o c) -> p o c", o=1).to_broadcast([64, 8, 64])

        def bc_l(tp8):
            return tp8.rearrange("p (h o) -> p h o", o=1).to_broadcast([64, 8, 64])

        def scr(name, shape=(64, 8, 64)):
            return [sb(st, "%s_%d" % (name, i), list(shape)) for i in range(2)]
        names = ("qc", "kc", "vc", "Gm", "tt", "Esl", "Eu", "M", "MT", "A", "AT", "P",
                 "VB", "KBg", "Kd", "IT", "U", "KC", "vn", "o2", "oo", "obc")
        S3 = {nm: scr(nm) for nm in names}
        s_zc = scr("zc", (64, 512))
        s_ex = scr("ex3", (64, 3, 8))
        s_sm = scr("smallc", (64, 4, 8))

        def pv(bank):
            return PS[bank][0][0:64, :].rearrange("p (h c) -> p h c", h=8), PS[bank][1]

        def mm(dst_ap, dst_b, lhsT, rhs, reads, start=True, stop=True, full=False):
            K.op("pe", lambda e: e.matmul(dst_ap, lhsT=lhsT, rhs=rhs, start=start, stop=stop), reads=reads, writes=[dst_b])

        def tr(dst_ap, dst_b, src, reads):
            K.op("pe", lambda e: e.transpose(dst_ap, src, id64), reads=reads + [b_ident], writes=[dst_b])

        for n in range(NCH):
            cs = slice(n * 64, (n + 1) * 64)
            pz = n % 2
            T = {nm: S3[nm][pz] for nm in names}
            (qc, b_q), (kc, b_k), (vc, b_v) = T["qc"], T["kc"], T["vc"]
            (Gm, bGm), (tt, btt), (Esl, bEsl), (Eu, bEu) = T["Gm"], T["tt"], T["Esl"], T["Eu"]
            (M, bM), (MT, bMT), (A, bA), (AT, bAT), (P, bP) = T["M"], T["MT"], T["A"], T["AT"], T["P"]
            (VB, bVB), (KBg, bKBg), (Kd, bKd), (IT, bIT) = T["VB"], T["KBg"], T["Kd"], T["IT"]
            (U, bU), (KC, bKC), (vn, bvn), (o2, bo2), (oo, boo), (obc, bobc) = T["U"], T["KC"], T["vn"], T["o2"], T["oo"], T["obc"]
            zc, b_z = s_zc[pz]
            ex3, bex = s_ex[pz]
            sm, bsm = s_sm[pz]
            load(qc[:], CONV_d[0:8, :, cs].rearrange("h p t -> p h t"), b_q, dCONV)
            load(kc[:], CONV_d[8:16, :, cs].rearrange("h p t -> p h t"), b_k, dCONV)
            load(vc[:], CONV_d[16:24, :, cs].rearrange("h p t -> p h t"), b_v, dCONV)
            load(zc[:], ZS_d[cs, :], b_z, dZS)
            g8 = bg[:, n, 8:16]
            beta8 = bg[:, n, 0:8]
            nb8 = nbeta[:, n, :]
            K.op("dve", lambda e: e.tensor_tensor(out=Gm[:], in0=bc_h(ut64[:]), in1=bc_l(g8), op=ALU.mult),
                 reads=[b_ut, b_bg], writes=[bGm])
            pD, bpD = pv(0)
            mm(PS[0][0][0:64, :], bpD, nones[:], Gm[:].rearrange("p h c -> p (h c)"), [bGm, b_nones], True, False, full=True)
            for h in range(8):
                mm(pD[:, h, :], bpD, Gm[:, h, :], ones64f, [bGm, b_ones], False, h == 7, full=True)
            pG, bpG = PS[1][0][0:64, 0:24].rearrange("p (a h) -> p a h", a=3), PS[1][1]
            mm(pG[:, 0, :], bpG, ut64[:], g8, [b_ut, b_bg], full=True)
            mm(pG[:, 1, :], bpG, ones64f, g8, [b_ones, b_bg], full=True)
            mm(pG[:, 2, :], bpG, sl64[:], g8, [b_sl, b_bg], full=True)
            K.op("act", lambda e: e.activation(out=ex3[:], in_=pG, func=AF.Exp), reads=[bpG], writes=[bex])
            K.op("dve", lambda e: e.tensor_scalar(out=tt[:], in0=pD, scalar1=0.0, scalar2=None, op0=ALU.min), reads=[bpD], writes=[btt])
            K.op("act", lambda e: e.activation(out=Esl[:], in_=tt[:], func=AF.Exp), reads=[btt], writes=[bEsl])
            K.op("pool", lambda e: e.tensor_tensor(out=Esl[:], in0=Esl[:], in1=bc_h(sl64[:]), op=ALU.mult), reads=[bEsl, b_sl], writes=[bEsl])
            K.op("dve", lambda e: e.tensor_scalar(out=tt[:], in0=pD, scalar1=-1.0, scalar2=0.0, op0=ALU.mult, op1=ALU.min),
                 reads=[bpD, bEsl], writes=[btt])
            K.op("act", lambda e: e.activation(out=Eu[:], in_=tt[:], func=AF.Exp), reads=[btt], writes=[bEu])
            K.op("pool", lambda e: e.tensor_tensor(out=Eu[:], in0=Eu[:], in1=bc_h(iu64[:]), op=ALU.mult), reads=[bEu, b_iu], writes=[bEu])
            pKK, bpKK = pv(2)
            for h in range(8):
                mm(pKK[:, h, :], bpKK, kc[:, h, :], kc[:, h, :], [b_k], full=True)
            K.op("dve", lambda e: e.tensor_tensor(out=M[:], in0=pKK, in1=bc_l(nb8), op=ALU.mult), reads=[bpKK, b_nbeta], writes=[bM])
            K.op("pool", lambda e: e.tensor_tensor(out=M[:], in0=M[:], in1=Esl[:], op=ALU.mult), reads=[bM, bEsl], writes=[bM])
            pMt, bpMt = pv(3)
            for h in range(8):
                tr(pMt[:, h, :], bpMt, M[:, h, :], [bM])
            K.op("act", lambda e: e.activation(out=MT[:], in_=pMt, func=AF.Copy), reads=[bpMt], writes=[bMT])
            K.op("pool", lambda e: e.tensor_tensor(out=P[:], in0=MT[:], in1=bc_h(id64), op=ALU.add), reads=[bMT, b_ident], writes=[bP])
            cA, cbA, cAT, cbAT = M, bM, MT, bMT
            pA2, bpA2 = pv(4)
            pA2T, bpA2T = pv(5)
            pPp, bpPp = pv(6)
            for lv in range(5):
                for h in range(8):
                    mm(pA2[:, h, :], bpA2, cAT[:, h, :], cA[:, h, :], [cbA, cbAT], full=True)
                if lv < 4:
                    for h in range(8):
                        mm(pA2T[:, h, :], bpA2T, cA[:, h, :], cAT[:, h, :], [cbA, cbAT], full=True)
                K.op("act", lambda e: e.activation(out=A[:], in_=pA2, func=AF.Copy), reads=[bpA2, cbA], writes=[bA])
                if lv < 4:
                    K.op("dve", lambda e: e.tensor_copy(out=AT[:], in_=pA2T), reads=[bpA2T, cbAT], writes=[bAT])
                cA, cbA, cAT, cbAT = A, bA, AT, bAT
                for h in range(8):
                    mm(pPp[:, h, :], bpPp, A[:, h, :], P[:, h, :], [bA, bP], full=True)
                K.op("dve", lambda e: e.tensor_tensor(out=P[:], in0=pPp, in1=P[:], op=ALU.add), reads=[bpPp, bP], writes=[bP])
            pVt, bpVt = pv(7)
            pKt, bpKt = pv(3)
            for h in range(8):
                tr(pVt[:, h, :], bpVt, vc[:, h, :], [b_v])
            for h in range(8):
                tr(pKt[:, h, :], bpKt, kc[:, h, :], [b_k])
            K.op("dve", lambda e: e.tensor_tensor(out=VB[:], in0=pVt, in1=bc_l(beta8), op=ALU.mult), reads=[bpVt, b_bg], writes=[bVB])
            K.op("pool", lambda e: e.tensor_tensor(out=sm[:, 0, :], in0=beta8, in1=ex3[:, 0, :], op=ALU.mult), reads=[b_bg, bex], writes=[bsm])
            K.op("dve", lambda e: e.tensor_tensor(out=KBg[:], in0=pKt, in1=bc_l(sm[:, 0, :]), op=ALU.mult), reads=[bpKt, bsm], writes=[bKBg])
            K.op("dve", lambda e: e.tensor_tensor(out=Kd[:], in0=pKt, in1=bc_l(ex3[:, 2, :]), op=ALU.mult), reads=[bpKt, bex], writes=[bKd])
            pU, bpU = pv(0)
            pKC, bpKC = pv(2)
            for h in range(8):
                mm(pU[:, h, :], bpU, P[:, h, :], VB[:, h, :], [bP, bVB])
            for h in range(8):
                mm(pKC[:, h, :], bpKC, KBg[:, h, :], P[:, h, :], [bP, bKBg])
            K.op("act", lambda e: e.activation(out=U[:], in_=pU, func=AF.Copy), reads=[bpU], writes=[bU])
            K.op("act", lambda e: e.activation(out=KC[:], in_=pKC, func=AF.Copy), reads=[bpKC], writes=[bKC])
            pKQ, bpKQ = pv(7)
            for h in range(8):
                mm(pKQ[:, h, :], bpKQ, kc[:, h, :], qc[:, h, :], [b_k, b_q])
            K.op("dve", lambda e: e.tensor_tensor(out=IT[:], in0=pKQ, in1=Eu[:], op=ALU.mult), reads=[bpKQ, bEu], writes=[bIT])
            pVN, bpVN = pv(4)
            pO1, bpO1 = pv(5)
            for h in range(8):
                mm(pVN[:, h, :], bpVN, KC[:, h, :], Sst[:, h, :], [bKC, b_S])
            for h in range(8):
                mm(pO1[:, h, :], bpO1, qc[:, h, :], Sst[:, h, :], [b_q, b_S])
            K.op("dve", lambda e: e.tensor_tensor(out=vn[:], in0=U[:], in1=pVN, op=ALU.subtract), reads=[bU, bpVN], writes=[bvn])
            pO2, bpO2 = pv(6)
            pSd, bpSd = pv(3)
            for h in range(8):
                mm(pO2[:, h, :], bpO2, IT[:, h, :], vn[:, h, :], [bIT, bvn])
            for h in range(8):
                mm(pSd[:, h, :], bpSd, Kd[:, h, :], vn[:, h, :], [bKd, bvn])
            K.op("act", lambda e: e.activation(out=o2[:], in_=pO2, func=AF.Copy), reads=[bpO2], writes=[bo2])
            K.op("dve", lambda e: e.tensor_tensor(out=oo[:], in0=pO1, in1=bc_l(ex3[:, 0, :]), op=ALU.mult), reads=[bpO1, bex], writes=[boo])
            K.op("pool", lambda e: e.tensor_tensor(out=oo[:], in0=oo[:], in1=o2[:], op=ALU.add), reads=[boo, bo2], writes=[boo])
            K.op("pool", lambda e: e.tensor_tensor(out=Sst[:], in0=Sst[:], in1=bc_l(ex3[:, 1, :]), op=ALU.mult), reads=[b_S, bex], writes=[b_S])
            K.op("dve", lambda e: e.tensor_tensor(out=Sst[:], in0=pSd, in1=Sst[:], op=ALU.add), reads=[bpSd, b_S], writes=[b_S])
            K.op("pool", lambda e: e.tensor_tensor(out=o2[:], in0=oo[:], in1=oo[:], op=ALU.mult), reads=[boo, bo2], writes=[bo2])
            K.op("dve", lambda e: e.tensor_reduce(out=sm[:, 1, :], in_=o2[:], axis=AX.X, op=ALU.add), reads=[bo2], writes=[bsm])
            K.op("act", lambda e: e.activation(out=sm[:, 2, :], in_=sm[:, 1, :], func=AF.Sqrt, bias=EPS, scale=1.0 / 64.0), reads=[bsm], writes=[bsm])
            K.op("dve", lambda e: e.reciprocal(out=sm[:, 3, :], in_=sm[:, 2, :]), reads=[bsm], writes=[bsm])
            K.op("dve", lambda e: e.tensor_tensor(out=obc[:], in0=oo[:], in1=bc_l(sm[:, 3, :]), op=ALU.mult), reads=[boo, bsm], writes=[bobc])
            K.op("pool", lambda e: e.tensor_tensor(out=obc[:], in0=obc[:], in1=bc_h(normb[:]), op=ALU.mult), reads=[bobc, b_normb], writes=[bobc])
            K.op("dve", lambda e: e.tensor_tensor(out=obc[:], in0=obc[:], in1=zc[:].rearrange("p (h c) -> p h c", h=8), op=ALU.mult),
                 reads=[bobc, b_z], writes=[bobc])
            pOT, bpOT = pv(1)
            for h in range(8):
                tr(pOT[:, h, :], bpOT, obc[:, h, :], [bobc])
            K.op("act", lambda e: e.activation(out=obT[:, :, cs], in_=pOT, func=AF.Copy), reads=[bpOT], writes=[b_obT])
        for h in range(8):
            store(OB_d[h], obT[:, h, :], b_obT, dOB)
        K.barrier()
    if stop_after == "C":
        return finish()

    with ExitStack() as st:
        wstage, b_wstage = sb(st, "wstageD", [128, 8, D])
        wpa, b_wpa = sb(st, "wpa_bf", [64, 8, D], BF16)
        wpb, b_wpb = sb(st, "wpb_bf", [64, 8, D], BF16)
        wo, b_wo = sb(st, "wo_bf", [128, 8, D], BF16)
        load(wstage[0:64], w_pa_d, b_wstage)
        K.op("pool", lambda e: e.tensor_copy(out=wpa[:], in_=wstage[0:64]), reads=[b_wstage], writes=[b_wpa])
        load(wstage[0:64], w_pb_d, b_wstage)
        K.op("pool", lambda e: e.tensor_copy(out=wpb[:], in_=wstage[0:64]), reads=[b_wstage], writes=[b_wpb])
        load(wstage[:], w_o_d.rearrange("(k p) n -> p k n", p=128), b_wstage)
        K.op("pool", lambda e: e.tensor_copy(out=wo[:], in_=wstage[:]), reads=[b_wstage], writes=[b_wo])
        oa_ss = [sb(st, "oa_s%d" % i, [64, 8, 512], BF16) for i in range(2)]
        ob_ss = [sb(st, "ob_s%d" % i, [64, 8, 512], BF16) for i in range(2)]
        ga_ss = [sb(st, "ga_s%d" % i, [128, 512], BF16) for i in range(2)]
        gb_ss = [sb(st, "gb_s%d" % i, [128, 512], BF16) for i in range(2)]
        mrgs = [sb(st, "mrg%d" % i, [128, 8, 512], BF16) for i in range(2)]
        m1, b_m1 = sb(st, "m1", [128, 512])
        m2, b_m2 = sb(st, "m2", [128, 512])
        xds = [sb(st, "xd%d" % i, [128, D]) for i in range(2)]
        hds = [sb(st, "hd%d" % i, [128, D]) for i in range(2)]
        ng = 0
        nt_ = 0
        for sc in range(NS):
            tok = slice(sc * 512, (sc + 1) * 512)
            oa_s, b_oas = oa_ss[sc % 2]
            ob_s, b_obs = ob_ss[sc % 2]
            mrg, b_mrg = mrgs[sc % 2]
            load(oa_s[:], OA_d[:, :, tok].rearrange("h p t -> p h t"), b_oas, dOA)
            load(ob_s[:], OB_d[:, :, tok].rearrange("h p t -> p h t"), b_obs, dOB)
            for nn in range(8):
                ga_s, b_gas = ga_ss[ng % 2]
                gb_s, b_gbs = gb_ss[ng % 2]
                (pa, pba), (pb_, pbb) = PS[(ng % 2) * 2], PS[(ng % 2) * 2 + 1]
                ng += 1
                load(ga_s[:], GATE_d[nn][:, tok], b_gas, dGATE)
                load(gb_s[:], GATE_d[8 + nn][:, tok], b_gbs, dGATE)
                for hh in range(8):
                    K.op("pe", lambda e: e.matmul(pa[:, :], lhsT=wpa[:, hh, nn * 128:(nn + 1) * 128], rhs=oa_s[:, hh, :],
                                                  start=(hh == 0), stop=(hh == 7)), reads=[b_wpa, b_oas], writes=[pba])
                for hh in range(8):
                    K.op("pe", lambda e: e.matmul(pb_[:, :], lhsT=wpb[:, hh, nn * 128:(nn + 1) * 128], rhs=ob_s[:, hh, :],
                                                  start=(hh == 0), stop=(hh == 7)), reads=[b_wpb, b_obs], writes=[pbb])
                K.op("dve", lambda e: e.tensor_tensor(out=m1[:], in0=pa[:, :], in1=ga_s[:], op=ALU.mult), reads=[pba, b_gas], writes=[b_m1])
                K.op("dve", lambda e: e.tensor_tensor(out=m2[:], in0=pb_[:, :], in1=gb_s[:], op=ALU.mult), reads=[pbb, b_gbs], writes=[b_m2])
                K.op("pool", lambda e: e.tensor_tensor(out=mrg[:, nn, :], in0=m1[:], in1=m2[:], op=ALU.add), reads=[b_m1, b_m2], writes=[b_mrg])
            for tt in range(4):
                ti = sc * 4 + tt
                tk = slice(ti * 128, (ti + 1) * 128)
                xd, b_xd = xds[nt_ % 2]
                hd, b_hd = hds[nt_ % 2]
                nt_ += 1
                load(xd[:], x_d[tk, :], b_xd)
                for mh in range(2):
                    py, pby = PS[4 + mh]
                    for nn in range(8):
                        K.op("pe", lambda e: e.matmul(py[:, :], lhsT=mrg[:, nn, tt * 128:(tt + 1) * 128], rhs=wo[:, nn, mh * 512:(mh + 1) * 512],
                                                      start=(nn == 0), stop=(nn == 7)), reads=[b_mrg, b_wo], writes=[pby])
                    K.op("dve", lambda e: e.tensor_tensor(out=hd[:, mh * 512:(mh + 1) * 512], in0=py[:, :], in1=modbc[:, 0, mh * 512:(mh + 1) * 512],
                                                          op=ALU.mult), reads=[pby, b_modbc], writes=[b_hd])
                K.op("pool", lambda e: e.tensor_tensor(out=hd[:], in0=hd[:], in1=xd[:], op=ALU.add), reads=[b_hd, b_xd], writes=[b_hd])
                store(H_d[tk, :], hd[:], b_hd, dH)
        K.barrier()
    if stop_after == "D":
        return finish()

    with ExitStack() as st:
        fnw, b_fnw = sb(st, "fnw", [128, D])
        load(fnw[:], fnw_d, b_fnw)
        subT, b_subT = sb(st, "subT", [64, 16, 128])
        load(subT[:], subT_d, b_subT)
        wq, b_wq = sb(st, "wq_bf", [128, 8, D], BF16)
        with ExitStack() as st2:
            wstage, b_wstage = sb(st2, "wstageE", [128, 8, D])
            load(wstage[:], wq_d.rearrange("(k p) n -> p k n", p=128), b_wstage)
            K.op("pool", lambda e: e.tensor_copy(out=wq[:], in_=wstage[:]), reads=[b_wstage], writes=[b_wq])
            K.barrier()
        hts = [sb(st, "ht%d" % i, [128, D]) for i in range(2)]
        n2, b_n2 = sb(st, "n2", [128, D])
        n2T, b_n2T = sb(st, "n2T", [128, 8, 128], BF16)
        qpT, b_qpT = sb(st, "qpT", [64, 16, 128])
        s_sb, b_ssb = sb(st, "s_sb", [128, 16, 128])
        v16, b_v16 = sb(st, "v16", [128, 16, 16])
        i16, b_i16 = sb(st, "i16", [128, 16, 16], U32)
        i16f, b_i16f = sb(st, "i16f", [128, 16, 16])
        wk2, b_wk2 = sb(st, "wk2", [128, 128])
        cand, b_cand = sb(st, "cand", [128, 8, 16, 16])
        eid, b_eid = sb(st, "eid", [128, 8, 16, 16])
        wk3, b_wk3 = sb(st, "wk3", [128, 256])
        jk3, b_jk3 = sb(st, "jk3", [128, 256])
        vals, b_vals = sb(st, "vals", [128, 8, 16])
        eids, b_eids = sb(st, "eids", [128, 128])
        idx, b_idx = sb(st, "idx", [128, 128], I32)
        gl, b_gl = sb(st, "gl", [128, 8, 16])
        gs, b_gs = sb(st, "gs", [128, 16])
        av, b_av = sb(st, "av", [128, 128])
        coef, b_coef = sb(st, "coef", [128, 128])
        acc, b_acc = sb(st, "acc", [128, D])
        jk, b_jk = sb(st, "jkE", [128, D])
        stE, b_stE = sb(st, "stE", [128, 8])
        outs = [sb(st, "outt%d" % i, [128, D]) for i in range(2)]
        NG = 16
        gbuf = [sb(st, "gath%d" % i, [128, 2 * D], BF16) for i in range(NG)]
        tvs = [sb(st, "tv%d" % i, [128, D], BF16) for i in range(3)]
        coefs = [sb(st, "coefg%d" % i, [128, 8]) for i in range(2)]
        ident_bf, b_identbf = sb(st, "ident_bf", [128, 128], BF16)
        K.op("pool", lambda e: e.tensor_copy(out=ident_bf[:], in_=ident[:]), reads=[b_ident], writes=[b_identbf])
        ngat = 0
        for i in range(NT):
            tk = slice(i * 128, (i + 1) * 128)
            ht, b_ht = hts[i % 2]
            ot, b_ot = outs[i % 2]
            load(ht[:], H_d[tk, :], b_ht, dH)
            K.op("act", lambda e: e.activation(out=jk[:], in_=ht[:], func=AF.Square, accum_out=stE[:, 0:1]), reads=[b_ht], writes=[b_jk, b_stE])
            K.op("act", lambda e: e.activation(out=stE[:, 1:2], in_=stE[:, 0:1], func=AF.Sqrt, bias=EPS, scale=1.0 / D), reads=[b_stE], writes=[b_stE])
            K.op("dve", lambda e: e.reciprocal(out=stE[:, 2:3], in_=stE[:, 1:2]), reads=[b_stE], writes=[b_stE])
            K.op("dve", lambda e: e.scalar_tensor_tensor(out=n2[:], in0=ht[:], scalar=stE[:, 2:3], in1=modbc[:, 2, :], op0=ALU.mult, op1=ALU.mult),
                 reads=[b_ht, b_stE, b_modbc], writes=[b_n2])
            K.op("pool", lambda e: e.tensor_tensor(out=n2[:], in0=n2[:], in1=modbc[:, 1, :], op=ALU.add), reads=[b_n2, b_modbc], writes=[b_n2])
            for k in range(8):
                pt, pb = PS[k // 4]
                K.op("pe", lambda e: e.transpose(pt[:, (k % 4) * 128:(k % 4 + 1) * 128], n2[:, k * 128:(k + 1) * 128], ident[:]),
                     reads=[b_n2, b_ident], writes=[pb])
            for k2 in range(2):
                pt, pb = PS[k2]
                K.op("act", lambda e: e.activation(out=n2T[:, k2 * 4:(k2 + 1) * 4, :].rearrange("p a b -> p (a b)"), in_=pt[:, :], func=AF.Copy),
                     reads=[pb], writes=[b_n2T])
            for hp in range(16):
                pq, pbq = PS[2 + (hp // 4) % 2]
                c0 = (hp % 4) * 128
                for k in range(8):
                    K.op("pe", lambda e: e.matmul(pq[0:64, c0:c0 + 128], lhsT=wq[:, k, hp * 64:(hp + 1) * 64], rhs=n2T[:, k, :],
                                                  start=(k == 0), stop=(k == 7)), reads=[b_wq, b_n2T], writes=[pbq])
                if hp % 4 == 3:
                    g4 = hp // 4
                    K.op("act", lambda e: e.activation(out=qpT[:, g4 * 4:(g4 + 1) * 4, :].rearrange("p a b -> p (a b)"), in_=pq[0:64, :], func=AF.Copy),
                         reads=[pbq], writes=[b_qpT])
            for hp in range(16):
                psc, pbsc = PS[4 + (hp // 4) % 2]
                c0 = (hp % 4) * 128
                K.op("pe", lambda e: e.matmul(psc[:, c0:c0 + 128], lhsT=qpT[:, hp, :], rhs=subT[:, hp, :], start=True, stop=True),
                     reads=[b_qpT, b_subT], writes=[pbsc])
                if hp % 4 == 3:
                    g4 = hp // 4
                    K.op("act", lambda e: e.activation(out=s_sb[:, g4 * 4:(g4 + 1) * 4, :].rearrange("p a b -> p (a b)"), in_=psc[:, :], func=AF.Copy),
                         reads=[pbsc], writes=[b_ssb])
            for hp in range(16):
                K.op("dve", lambda e: e.max(out=v16[:, hp, 0:8], in_=s_sb[:, hp, :]), reads=[b_ssb], writes=[b_v16])
                K.op("dve", lambda e: e.max_index(out=i16[:, hp, 0:8], in_max=v16[:, hp, 0:8], in_values=s_sb[:, hp, :]),
                     reads=[b_ssb, b_v16], writes=[b_i16])
                K.op("dve", lambda e: e.match_replace(out=wk2[:], in_to_replace=v16[:, hp, 0:8], in_values=s_sb[:, hp, :], imm_value=NEG),
                     reads=[b_ssb, b_v16], writes=[b_wk2])
                K.op("dve", lambda e: e.max(out=v16[:, hp, 8:16], in_=wk2[:]), reads=[b_wk2], writes=[b_v16])
                K.op("dve", lambda e: e.max_index(out=i16[:, hp, 8:16], in_max=v16[:, hp, 8:16], in_values=wk2[:]),
                     reads=[b_wk2, b_v16], writes=[b_i16])
            K.op("dve", lambda e: e.tensor_copy(out=i16f[:], in_=i16[:]), reads=[b_i16], writes=[b_i16f])
            v16v = v16[:].rearrange("p (h two) k -> p h two k", two=2)
            i16v = i16f[:].rearrange("p (h two) k -> p h two k", two=2)
            K.op("dve", lambda e: e.tensor_scalar(out=i16v[:, :, 0, :], in0=i16v[:, :, 0, :], scalar1=128.0, scalar2=None, op0=ALU.mult),
                 reads=[b_i16f], writes=[b_i16f])
            for a in range(16):
                K.op("dve", lambda e: e.tensor_tensor(out=cand[:, :, a, :], in0=v16v[:, :, 1, :], in1=v16v[:, :, 0, a:a + 1].to_broadcast([128, 8, 16]),
                                                      op=ALU.add), reads=[b_v16], writes=[b_cand])
                K.op("pool", lambda e: e.tensor_tensor(out=eid[:, :, a, :], in0=i16v[:, :, 1, :], in1=i16v[:, :, 0, a:a + 1].to_broadcast([128, 8, 16]),
                                                       op=ALU.add), reads=[b_i16f], writes=[b_eid])
            for hh in range(8):
                ch = cand[:, hh].rearrange("p a b -> p (a b)")
                eh = eid[:, hh].rearrange("p a b -> p (a b)")
                K.op("dve", lambda e: e.max(out=vals[:, hh, 0:8], in_=ch), reads=[b_cand], writes=[b_vals])
                K.op("dve", lambda e: e.match_replace(out=wk3[:], in_to_replace=vals[:, hh, 0:8], in_values=ch, imm_value=NEG),
                     reads=[b_cand, b_vals], writes=[b_wk3])
                K.op("dve", lambda e: e.max(out=vals[:, hh, 8:16], in_=wk3[:]), reads=[b_wk3], writes=[b_vals])
                for j in range(16):
                    K.op("dve", lambda e: e.scalar_tensor_tensor(out=jk3[:], in0=ch, scalar=vals[:, hh, j:j + 1], in1=eh, op0=ALU.is_equal, op1=ALU.mult,
                                                                 accum_out=eids[:, hh * 16 + j:hh * 16 + j + 1]),
                         reads=[b_cand, b_vals, b_eid], writes=[b_jk3, b_eids])
            K.op("dve", lambda e: e.tensor_scalar(out=eids[:], in0=eids[:], scalar1=16383.0, scalar2=0.0, op0=ALU.min, op1=ALU.max),
                 reads=[b_eids], writes=[b_eids])
            K.op("dve", lambda e: e.tensor_copy(out=idx[:], in_=eids[:]), reads=[b_eids], writes=[b_idx])
            K.op("dve", lambda e: e.tensor_tensor(out=gl[:], in0=vals[:], in1=vals[:, :, 0:1].to_broadcast([128, 8, 16]), op=ALU.subtract),
                 reads=[b_vals], writes=[b_gl])
            K.op("act", lambda e: e.activation(out=gl[:], in_=gl[:], func=AF.Exp), reads=[b_gl], writes=[b_gl])
            K.op("dve", lambda e: e.tensor_reduce(out=gs[:, 0:8], in_=gl[:], axis=AX.X, op=ALU.add), reads=[b_gl], writes=[b_gs])
            K.op("dve", lambda e: e.reciprocal(out=gs[:, 8:16], in_=gs[:, 0:8]), reads=[b_gs], writes=[b_gs])
            K.op("dve", lambda e: e.tensor_tensor(out=gl[:], in0=gl[:], in1=gs[:, 8:16].rearrange("p (h o) -> p h o", o=1).to_broadcast([128, 8, 16]),
                                                  op=ALU.mult), reads=[b_gl, b_gs], writes=[b_gl])
            for grp in range(16):
                gsl = []
                for s_ in range(8):
                    j = grp * 8 + s_
                    gb_, bgb = gbuf[ngat % NG]
                    ngat += 1
                    gsl.append((gb_, bgb))
                    K.dma(lambda e: e.indirect_dma_start(out=gb_[:], out_offset=None, in_=UV_d,
                                                         in_offset=bass.IndirectOffsetOnAxis(ap=idx[:, j:j + 1], axis=0)),
                          bgb, reads=[b_idx, dUV], writes=[bgb], q="pool")
                    K.op("dve", lambda e: e.scalar_tensor_tensor(out=jk[:], in0=gb_[:, 0:D], scalar=1.0, in1=n2[:], op0=ALU.mult, op1=ALU.mult,
                                                                 accum_out=av[:, j:j + 1]), reads=[bgb, b_n2], writes=[b_jk, b_av])
                g8 = slice(grp * 8, (grp + 1) * 8)
                cf, b_cf = coefs[grp % 2]
                K.op("act", lambda e: e.activation(out=cf[:], in_=av[:, g8], func=AF.Gelu), reads=[b_av], writes=[b_cf])
                K.op("dve", lambda e: e.tensor_tensor(out=cf[:], in0=cf[:], in1=gl[:].rearrange("p h k -> p (h k)")[:, g8], op=ALU.mult),
                     reads=[b_cf, b_gl], writes=[b_cf])
                for s_ in range(8):
                    j = grp * 8 + s_
                    gb_, bgb = gsl[s_]
                    tv, b_tv = tvs[j % 3]
                    K.op("act", lambda e: e.activation(out=tv[:], in_=gb_[:, D:2 * D], func=AF.Copy, scale=cf[:, s_:s_ + 1]),
                         reads=[bgb, b_cf], writes=[b_tv])
                    for mh in range(2):
                        K.op("pe", lambda e: e.matmul(PS[6 + mh][0][:, :], lhsT=ident_bf[:], rhs=tv[:, mh * 512:(mh + 1) * 512],
                                                      start=(j == 0), stop=(j == 127)), reads=[b_tv, b_identbf], writes=[PS[6 + mh][1]])
            for mh in range(2):
                K.op("dve", lambda e: e.tensor_tensor(out=acc[:, mh * 512:(mh + 1) * 512], in0=PS[6 + mh][0][:, :], in1=modbc[:, 3, mh * 512:(mh + 1) * 512],
                                                      op=ALU.mult), reads=[PS[6 + mh][1], b_modbc], writes=[b_acc])
            K.op("pool", lambda e: e.tensor_tensor(out=acc[:], in0=acc[:], in1=ht[:], op=ALU.add), reads=[b_acc, b_ht], writes=[b_acc])
            K.op("act", lambda e: e.activation(out=jk[:], in_=acc[:], func=AF.Square, accum_out=stE[:, 4:5]), reads=[b_acc], writes=[b_jk, b_stE])
            K.op("act", lambda e: e.activation(out=stE[:, 5:6], in_=stE[:, 4:5], func=AF.Sqrt, bias=EPS, scale=1.0 / D), reads=[b_stE], writes=[b_stE])
            K.op("dve", lambda e: e.reciprocal(out=stE[:, 6:7], in_=stE[:, 5:6]), reads=[b_stE], writes=[b_stE])
            K.op("dve", lambda e: e.scalar_tensor_tensor(out=ot[:], in0=acc[:], scalar=stE[:, 6:7], in1=fnw[:], op0=ALU.mult, op1=ALU.mult),
                 reads=[b_acc, b_stE, b_fnw], writes=[b_ot])
            store(out_d[tk, :], ot[:], b_ot, dOUT)
    return finish()


_NC_CACHE = {}


def kernel(**inputs):
    S = inputs["x"].shape[1]
    B = inputs["x"].shape[0]
    if S not in _NC_CACHE:
        _NC_CACHE[S] = build_nc(S)[0]
    nc = _NC_CACHE[S]
    in_maps = [host_layout(inputs, b, S) for b in range(B)]
    res = run_bass_kernel_spmd(nc, in_maps, core_ids=list(range(B)))
    return np.stack([np.asarray(r["out"], dtype=np.float32) for r in res.results], axis=0)
```

```python
import numpy as np
from contextlib import ExitStack
import concourse.bass as bass
import concourse.mybir as mybir
from concourse.bass_utils import run_bass_kernel_spmd

F32 = mybir.dt.float32
BF16 = mybir.dt.bfloat16
I32 = mybir.dt.int32
F32R = mybir.dt.float32r
U32 = mybir.dt.uint32
AF = mybir.ActivationFunctionType
ALU = mybir.AluOpType
AX = mybir.AxisListType

D = 1024
EPS = 1e-6
NEG = -1.0e30


class Buf:
    def __init__(self, name):
        self.name = name
        self.w = []
        self.r = []
        self.dsem = None


class _Rec:
    def __getattr__(self, name):
        return lambda *a, **kw: (name, a, kw)


_REC = _Rec()


class Sched:
    def __init__(self, nc, stack):
        self.nc = nc
        self.stack = stack
        self.eng = {"pe": nc.tensor, "act": nc.scalar, "dve": nc.vector, "pool": nc.gpsimd, "sp": nc.sync}
        self.sems = {}
        self.cnt = {}
        for e in self.eng:
            self.sems["e_" + e] = stack.enter_context(nc.semaphore("sem_" + e))
            self.cnt["e_" + e] = 0
        self.seen = {e: {} for e in self.eng}
        self.prog = {e: [] for e in self.eng}
        self.ndsem = 0
        self.n_ins = 0

    def dma_sem(self, buf):
        if buf.dsem is None:
            key = "d_%d" % self.ndsem
            self.ndsem += 1
            self.sems[key] = self.stack.enter_context(self.nc.semaphore("dsem_%d" % (self.ndsem - 1)))
            self.cnt[key] = 0
            buf.dsem = key
        return buf.dsem

    def _waits(self, e, deps):
        best = {}
        for (k, v) in deps:
            if e == "pe" and k == "e_pe":
                continue
            if best.get(k, 0) < v:
                best[k] = v
        out = []
        for k, v in best.items():
            if self.seen[e].get(k, 0) < v:
                self.seen[e][k] = v
                out.append((k, v))
        return out

    def op(self, e, fn, reads=(), writes=()):
        deps = []
        for b in reads:
            deps += b.w
        for b in writes:
            deps += b.w + b.r
        waits = self._waits(e, deps)
        key = "e_" + e
        self.cnt[key] += 1
        tok = (key, self.cnt[key])
        self.prog[e].append((waits, fn(_REC), key, 1))
        for b in writes:
            b.w = [tok]
            b.r = []
        for b in reads:
            if b not in writes:
                b.r.append(tok)
        self.n_ins += 1

    def dma(self, fn, sbuf, reads=(), writes=(), q="sp"):
        key = self.dma_sem(sbuf)
        deps = [(key, self.cnt[key])] if self.cnt[key] else []
        for b in reads:
            deps += b.w
        for b in writes:
            deps += b.w + b.r
        waits = self._waits(q, deps)
        self.cnt[key] += 16
        tok = (key, self.cnt[key])
        self.prog[q].append((waits, fn(_REC), key, 16))
        for b in writes:
            b.w = [tok]
            b.r = []
        for b in reads:
            if b not in writes:
                b.r.append(tok)
        self.n_ins += 1

    def barrier(self):
        allk = [(k, v) for k, v in self.cnt.items() if v > 0]
        for e in self.eng:
            waits = self._waits(e, allk)
            if waits:
                self.prog[e].append((waits, None, None, 0))

    def emit(self):
        nc = self.nc
        sems = self.sems
        prog = self.prog
        with nc.Block() as block:
            def run(e, engine):
                for waits, fn, key, inc in prog[e]:
                    if fn is None:
                        for (k, v) in waits:
                            engine.wait_ge(sems[k], v)
                        continue
                    for (k, v) in waits[1:]:
                        engine.wait_ge(sems[k], v)
                    ins = getattr(engine, fn[0])(*fn[1], **fn[2])
                    if waits:
                        ins._wait_ge(sems[waits[0][0]], waits[0][1])
                    ins.then_inc(sems[key], inc)

            @block.tensor
            def _(eng):
                run("pe", eng)

            @block.scalar
            def _(eng):
                run("act", eng)

            @block.vector
            def _(eng):
                run("dve", eng)

            @block.gpsimd
            def _(eng):
                run("pool", eng)

            @block.sync
            def _(eng):
                run("sp", eng)


A_HEADS, KV_HEADS, HD = 8, 2, 64
IDX_HEADS = 16
B_HEADS = 8
OFF_QA = 0
OFF_KA = 512
OFF_VA = 640
OFF_QI = 768
OFF_KI = 1792
OFF_WI = 1856
OFF_QB = 1872
OFF_KB = 2384
OFF_VB = 2896
OFF_ZB = 3408
OFF_BB = 3920
OFF_AB = 3928
OFF_GA = 3936
OFF_GB = 4960
IN_WIDTH = 5984

ROT_STARTS = [OFF_QA + 64 * h for h in range(8)] + [OFF_KA + 64 * g for g in range(2)] + \
             [OFF_QI + 64 * h for h in range(16)] + [OFF_KI]
N_ROT = len(ROT_STARTS)
CONV_STARTS = [OFF_QB + 64 * h for h in range(24)]
SWAP64 = np.concatenate([np.arange(8, 16), np.arange(0, 8), np.arange(16, 64)])


def host_layout(inputs, b, S):
    f = np.float32
    x = np.ascontiguousarray(inputs["x"][b, :S]).astype(f)
    c = inputs["c"][b].astype(f)
    w_in = inputs["w_in"][0].astype(f)
    m = {}
    m["x"] = x
    m["c_col"] = np.ascontiguousarray(c.reshape(8, 128).T)
    m["pos_rep"] = np.ascontiguousarray(np.broadcast_to(inputs["positions"][b, :S].astype(np.int32)[None, :], (128, S)))
    m["w_ada"] = np.ascontiguousarray(inputs["w_ada"][0].astype(f))
    b_ada = inputs["b_ada"][0].astype(f)
    m["b_ada_col"] = np.ascontiguousarray(b_ada.reshape(48, 128).T)
    m["b_ada_rep"] = np.ascontiguousarray(np.broadcast_to(b_ada[None, :], (128, 6144)))
    rot_cols = np.concatenate([np.arange(s, s + 64) for s in ROT_STARTS])
    rot_cols_sw = np.concatenate([s + SWAP64 for s in ROT_STARTS])
    zpad = np.zeros((w_in.shape[0], 64), f)
    m["w_rot"] = np.ascontiguousarray(np.concatenate([w_in[:, rot_cols], zpad], axis=1))
    m["w_rot_sw"] = np.ascontiguousarray(np.concatenate([w_in[:, rot_cols_sw], zpad], axis=1))
    m["w_conv"] = np.ascontiguousarray(w_in[:, OFF_QB:OFF_QB + 1536])
    m["w_gate"] = np.ascontiguousarray(w_in[:, OFF_GA:OFF_GA + 2048])
    m["w_tok"] = np.ascontiguousarray(np.concatenate(
        [w_in[:, OFF_VA:OFF_VA + 128], w_in[:, OFF_WI:OFF_WI + 16]], axis=1))
    m["w_z"] = np.ascontiguousarray(w_in[:, OFF_ZB:OFF_ZB + 512])
    m["w_bg"] = np.ascontiguousarray(w_in[:, OFF_BB:OFF_BB + 16])
    conv_w = inputs["conv_w"][0].astype(f)
    m["conv_col"] = np.ascontiguousarray(conv_w.T.reshape(12, 128, 4).transpose(1, 0, 2))
    m["a_log_rep"] = np.ascontiguousarray(np.broadcast_to(inputs["a_log"][0].astype(f)[None, :], (64, 8)))
    m["dt_bias_rep"] = np.ascontiguousarray(np.broadcast_to(inputs["dt_bias"][0].astype(f)[None, :], (64, 8)))
    m["norm_b_rep"] = np.ascontiguousarray(np.broadcast_to(inputs["norm_b_w"][0].astype(f)[None, :], (64, 64)))
    m["w_pa"] = np.ascontiguousarray(inputs["w_pa"][0].astype(f).reshape(8, 64, 1024).transpose(1, 0, 2))
    m["w_pb"] = np.ascontiguousarray(inputs["w_pb"][0].astype(f).reshape(8, 64, 1024).transpose(1, 0, 2))
    m["w_o"] = np.ascontiguousarray(inputs["w_o"][0].astype(f))
    m["peer_wq"] = np.ascontiguousarray(inputs["peer_wq"][0].astype(f))
    sk = inputs["peer_subkeys"][0].astype(f)
    m["subT"] = np.ascontiguousarray(sk.transpose(3, 0, 1, 2).reshape(64, 16, 128))
    m["peer_u"] = np.ascontiguousarray(inputs["peer_u"][0].astype(f))
    m["peer_v"] = np.ascontiguousarray(inputs["peer_v"][0].astype(f))
    m["fnw_rep"] = np.ascontiguousarray(np.broadcast_to(inputs["final_norm_w"].astype(f)[None, :], (128, 1024)))
    m.update(host_consts())
    return m


def host_consts():
    f = np.float32
    k = {}
    k["ident"] = np.eye(128, dtype=f)
    half = 8
    inv_freq = np.power(f(500000.0), -np.arange(half, dtype=f) * f(2.0 / 16)).astype(f)
    fr = np.zeros((64, 1), f)
    sg = np.zeros((64, 1), f)
    fr[0:8, 0] = inv_freq
    fr[8:16, 0] = inv_freq
    sg[0:8, 0] = -1.0
    sg[8:16, 0] = 1.0
    k["freq_col"] = np.concatenate([fr, fr], 0)
    k["sign_col"] = np.concatenate([sg, sg], 0)
    bd = np.zeros((128, 128), f)
    bd[0:64, 0:64] = 1.0
    bd[64:128, 64:128] = 1.0
    k["onesbd"] = bd
    p = np.arange(128)
    k["cmask"] = np.where(p[None, :] <= p[:, None], 0.0, NEG).astype(f)
    i = np.arange(64)
    k["ut64"] = (i[:, None] <= i[None, :]).astype(f)
    k["sl64"] = (i[:, None] > i[None, :]).astype(f)
    k["il64"] = (i[:, None] >= i[None, :]).astype(f)
    k["iu64"] = (i[:, None] <= i[None, :]).astype(f)
    k["su64"] = (i[:, None] < i[None, :]).astype(f)
    return k


def build_nc(S, dbg=False, stop_after=None):
    NT = S // 128
    NS = S // 512
    NCH = S // 64
    TOPK = min(256, S // 4)
    nc = bass.Bass("TRN2", target_bir_lowering=False)
    stack = ExitStack()
    K = Sched(nc, stack)

    def din(name, shape, dt=F32):
        return nc.dram_tensor(name, list(shape), dt, kind="ExternalInput").ap()

    def dscr(name, shape, dt=F32):
        return nc.dram_tensor(name, list(shape), dt).ap()

    x_d = din("x", [S, D])
    c_col_d = din("c_col", [128, 8])
    pos_d = din("pos_rep", [128, S], I32)
    w_ada_d = din("w_ada", [D, 6 * D])
    b_ada_col_d = din("b_ada_col", [128, 48])
    b_ada_rep_d = din("b_ada_rep", [128, 6 * D])
    w_rot_d = din("w_rot", [D, (N_ROT + 1) * 64])
    w_rot_sw_d = din("w_rot_sw", [D, (N_ROT + 1) * 64])
    w_conv_d = din("w_conv", [D, 1536])
    w_gate_d = din("w_gate", [D, 2048])
    w_tok_d = din("w_tok", [D, 144])
    w_z_d = din("w_z", [D, 512])
    w_bg_d = din("w_bg", [D, 16])
    conv_col_d = din("conv_col", [128, 12, 4])
    a_log_d = din("a_log_rep", [64, 8])
    dt_bias_d = din("dt_bias_rep", [64, 8])
    norm_b_d = din("norm_b_rep", [64, 64])
    w_pa_d = din("w_pa", [64, 8, D])
    w_pb_d = din("w_pb", [64, 8, D])
    w_o_d = din("w_o", [D, D])
    wq_d = din("peer_wq", [D, D])
    subT_d = din("subT", [64, 16, 128])
    u_d = din("peer_u", [16384, D])
    v_d = din("peer_v", [16384, D])
    fnw_d = din("fnw_rep", [128, D])
    ident_d = din("ident", [128, 128])
    freq_d = din("freq_col", [128, 1])
    sign_d = din("sign_col", [128, 1])
    onesbd_d = din("onesbd", [128, 128])
    cmask_d = din("cmask", [128, 128])
    ut64_d = din("ut64", [64, 64])
    sl64_d = din("sl64", [64, 64])
    il64_d = din("il64", [64, 64])
    iu64_d = din("iu64", [64, 64])
    su64_d = din("su64", [64, 64])
    out_d = nc.dram_tensor("out", [S, D], F32, kind="ExternalOutput").ap()

    ROT_d = dscr("ROT", [N_ROT, 64, S], BF16)
    CONV_d = dscr("CONVO", [24, 64, S], F32)
    GATE_d = dscr("GATE", [16, 128, S], BF16)
    VW_d = dscr("VW", [S, 144])
    ZS_d = dscr("ZS", [S, 512])
    BG_d = dscr("BG", [S, 16])
    OA_d = dscr("OA", [8, 64, S], BF16)
    OB_d = dscr("OB", [8, 64, S], BF16)
    H_d = dscr("H", [S, D])
    UV_d = dscr("UV", [16384, 2 * D], BF16)
    dUV = Buf("UV")
    dROT, dCONV, dGATE, dVW, dZS, dBG, dOA, dOB, dH = [Buf(n) for n in
                                                      ("ROT", "CONV", "GATE", "VW", "ZS", "BG", "OA", "OB", "H")]
    dOUT = Buf("OUT")
    dIN = Buf("IN")

    dbg_out = {}

    def dbg_tensor(name, shape, dt=F32):
        t = nc.dram_tensor(name, list(shape), dt, kind="ExternalOutput").ap()
        dbg_out[name] = t
        return t

    def sb(st, name, shape, dt=F32):
        t = st.enter_context(nc.sbuf_tensor("sb_" + name, list(shape), dt))
        return t, Buf(name)

    def ps(st, name, shape=(128, 512), dt=F32):
        t = st.enter_context(nc.psum_tensor(name, list(shape), dt))
        return t, Buf(name)

    def load(dst_ap, src_ap, buf, src_buf=dIN, q="sp"):
        K.dma(lambda e, o=dst_ap, i=src_ap: e.dma_start(out=o, in_=i), buf, reads=[src_buf], writes=[buf], q=q)

    def store(dst_ap, src_ap, buf, dst_buf, q="sp"):
        K.dma(lambda e, o=dst_ap, i=src_ap: e.dma_start(out=o, in_=i), buf, reads=[buf], writes=[dst_buf], q=q)

    PS = [ps(stack, "psb%d" % i) for i in range(8)]

    ident, b_ident = sb(stack, "ident", [128, 128])
    load(ident[:], ident_d, b_ident)
    modcol, b_modcol = sb(stack, "modcol", [128, 48])
    modbc, b_modbc = sb(stack, "modbc", [128, 4, D])
    ones128, b_ones = sb(stack, "ones128", [128, 128])
    K.op("pool", lambda e: e.memset(ones128[:], 1.0), writes=[b_ones])

    uvsem = [Buf("uvsem%d" % i) for i in range(8)]
    for qi_ in range(4):
        rs = slice(qi_ * 4096, (qi_ + 1) * 4096)
        K.dma(lambda e: e.dma_start(out=UV_d[rs, 0:D], in_=u_d[rs, :]), uvsem[qi_], reads=[dIN], writes=[], q="pool")
        K.dma(lambda e: e.dma_start(out=UV_d[rs, D:2 * D], in_=v_d[rs, :]), uvsem[4 + qi_], reads=[dIN], writes=[], q="pool")
    for bsem in uvsem:
        dUV.w.append((bsem.dsem, K.cnt[bsem.dsem]))

    with ExitStack() as st:
        ccol, b_ccol = sb(st, "ccol", [128, 8])
        cact, b_cact = sb(st, "cact", [128, 8])
        crep, b_crep = sb(st, "crep", [128, 8, 128])
        bcol, b_bcol = sb(st, "bcol", [128, 48])
        wst = [sb(st, "wada%d" % i, [128, 8, 1024]) for i in range(2)]
        brep, b_brep = sb(st, "brep", [128, 1024])
        load(ccol[:], c_col_d, b_ccol)
        load(bcol[:], b_ada_col_d, b_bcol)
        K.op("act", lambda e: e.activation(out=cact[:], in_=ccol[:], func=AF.Silu), reads=[b_ccol], writes=[b_cact])
        for k in range(8):
            K.op("dve", lambda e, k=k: e.tensor_copy(out=crep[:, k, :], in_=cact[:, k:k + 1].to_broadcast([128, 128])),
                 reads=[b_cact], writes=[b_crep])
        w_ada_v = w_ada_d.rearrange("(k p) n -> p k n", p=128)
        pcol_t, pcol_b = PS[0]
        bc_slot = {2: 0, 3: 1, 4: 2, 5: 3}
        for gi in range(6):
            wt, wb = wst[gi % 2]
            load(wt[:], w_ada_v[:, :, gi * 1024:(gi + 1) * 1024], wb)
            for jj in range(8):
                j = gi * 8 + jj
                for k in range(8):
                    K.op("pe", lambda e, j=j, jj=jj, k=k, wt=wt: e.matmul(
                        pcol_t[:, j:j + 1], lhsT=wt[:, k, jj * 128:(jj + 1) * 128], rhs=cact[:, k:k + 1],
                        start=(k == 0), stop=(k == 7)), reads=[wb, b_cact], writes=[pcol_b])
            if gi in bc_slot:
                sl = bc_slot[gi]
                load(brep[:], b_ada_rep_d[:, gi * 1024:(gi + 1) * 1024], b_brep)
                for hh in range(2):
                    pt, pb = PS[1 + hh]
                    for k in range(8):
                        K.op("pe", lambda e, hh=hh, k=k, wt=wt, pt=pt: e.matmul(
                            pt[:, :], lhsT=crep[:, k, :], rhs=wt[:, k, hh * 512:(hh + 1) * 512],
                            start=(k == 0), stop=(k == 7)), reads=[wb, b_crep], writes=[pb])
                    K.op("dve", lambda e, hh=hh, sl=sl, pt=pt: e.tensor_tensor(
                        out=modbc[:, sl, hh * 512:(hh + 1) * 512], in0=pt[:, :], in1=brep[:, hh * 512:(hh + 1) * 512],
                        op=ALU.add), reads=[pb, b_brep], writes=[b_modbc])
        K.op("dve", lambda e: e.tensor_tensor(out=modcol[:], in0=pcol_t[:, 0:48], in1=bcol[:], op=ALU.add),
             reads=[pcol_b, b_bcol], writes=[b_modcol])
        K.op("dve", lambda e: e.tensor_scalar(out=modcol[:, 8:16], in0=modcol[:, 8:16], scalar1=1.0, scalar2=None,
                                              op0=ALU.add), reads=[b_modcol], writes=[b_modcol])
        K.op("dve", lambda e: e.tensor_scalar(out=modbc[:, 2, :], in0=modbc[:, 2, :], scalar1=1.0, scalar2=None,
                                              op0=ALU.add), reads=[b_modbc], writes=[b_modbc])
        K.barrier()

    if dbg:
        t = dbg_tensor("dbg_modcol", [128, 48])
        store(t, modcol[:], b_modcol, Buf("x"))
        t = dbg_tensor("dbg_modbc", [128, 4 * D])
        store(t, modbc[:].rearrange("p a d -> p (a d)"), b_modbc, Buf("x"))

    def finish():
        K.barrier()
        K.emit()
        stack.close()
        return nc, dbg_out

    if stop_after == "A0":
        return finish()

    with ExitStack() as st:
        n1T, _ = sb(st, "n1T", [128, 8, S], BF16)
        b_n1T = [Buf("n1T%d" % i) for i in range(NT)]
        xts = [sb(st, "xt%d" % i, [128, D]) for i in range(2)]
        junk, b_junk = sb(st, "junkA", [128, D])
        stat, b_stat = sb(st, "statA", [128, 4])
        psT = [PS[0], PS[1]]
        for i in range(NT):
            xt, xb = xts[i % 2]
            load(xt[:], x_d[i * 128:(i + 1) * 128, :], xb)
            K.op("act", lambda e, xt=xt: e.activation(out=junk[:], in_=xt[:], func=AF.Square, accum_out=stat[:, 0:1]),
                 reads=[xb], writes=[b_junk, b_stat])
            K.op("act", lambda e: e.activation(out=stat[:, 1:2], in_=stat[:, 0:1], func=AF.Sqrt, bias=EPS, scale=1.0 / D),
                 reads=[b_stat], writes=[b_stat])
            K.op("dve", lambda e: e.reciprocal(out=stat[:, 2:3], in_=stat[:, 1:2]), reads=[b_stat], writes=[b_stat])
            K.op("dve", lambda e, xt=xt: e.tensor_scalar(out=junk[:], in0=xt[:], scalar1=stat[:, 2:3], scalar2=None,
                                                         op0=ALU.mult), reads=[xb, b_stat], writes=[b_junk])
            for k in range(8):
                pt, pb = psT[k // 4]
                K.op("pe", lambda e, k=k, pt=pt: e.transpose(pt[:, (k % 4) * 128:(k % 4 + 1) * 128],
                                                             junk[:, k * 128:(k + 1) * 128], ident[:]),
                     reads=[b_junk, b_ident], writes=[pb])
            for k in range(8):
                pt, pb = psT[k // 4]
                K.op("act", lambda e, k=k, pt=pt, i=i: e.activation(
                    out=n1T[:, k, i * 128:(i + 1) * 128], in_=pt[:, (k % 4) * 128:(k % 4 + 1) * 128],
                    func=AF.Identity, bias=modcol[:, k:k + 1], scale=modcol[:, 8 + k:9 + k]),
                    reads=[pb, b_modcol], writes=[b_n1T[i]])
        if dbg:
            t = dbg_tensor("dbg_n1T", [128, 8 * S], BF16)
            K.barrier()
            bx = Buf("n1Tall")
            store(t, n1T[:].rearrange("p k s -> p (k s)"), bx, Buf("x"))
        if stop_after == "A1":
            return finish()

        wstg = [sb(st, "wstg%d" % i, [128, 8, 144]) for i in range(2)]
        wbfs = [sb(st, "wbf%d" % i, [128, 8, 144], BF16) for i in range(3)]
        wstg_big = sb(st, "wstg_big", [128, 8, 512])
        wbf_big = sb(st, "wbf_big", [128, 8, 512], BF16)
        cnt = {"stg": 0, "bf": 0}

        def load_w(src_ap, ncols):
            if ncols > 144:
                stg, bs = wstg_big
                wbf, bw = wbf_big
            else:
                stg, bs = wstg[cnt["stg"] % 2]
                wbf, bw = wbfs[cnt["bf"] % 3]
                cnt["stg"] += 1
                cnt["bf"] += 1
            load(stg[:, :, 0:ncols], src_ap.rearrange("(k p) n -> p k n", p=128), bs)
            K.op("pool", lambda e: e.tensor_copy(out=wbf[:, :, 0:ncols], in_=stg[:, :, 0:ncols]), reads=[bs], writes=[bw])
            return wbf, bw

        def allb():
            return list(b_n1T)

        tmpa, b_tmpa = sb(st, "tmpa", [128, 512])
        tmpb, b_tmpb = sb(st, "tmpb", [128, 512])
        st_rot = ExitStack()
        cosT, b_cos = sb(st_rot, "cosT", [128, S])
        sinT, b_sin = sb(st_rot, "sinT", [128, S])
        with ExitStack() as st2:
            SH = S // 2
            posi, b_posi = sb(st2, "posi", [128, SH], I32)
            ang, b_ang = sb(st2, "ang", [128, SH])
            t1, b_t1 = sb(st2, "rr_t1", [128, SH])
            t2, b_t2 = sb(st2, "rr_t2", [128, SH])
            fcol, b_fcol = sb(st2, "fcol", [128, 2])
            load(fcol[:, 0:1], freq_d, b_fcol)
            load(fcol[:, 1:2], sign_d, b_fcol)
            MAGIC = 12582912.0
            C1 = 6.28125
            C2 = float(2.0 * np.pi - 6.28125)
            PIL = 3.1415925
            for hf in range(2):
                cs = slice(hf * SH, (hf + 1) * SH)
                load(posi[:], pos_d[:, cs], b_posi)
                K.op("dve", lambda e: e.tensor_copy(out=ang[:], in_=posi[:]), reads=[b_posi], writes=[b_ang])
                K.op("dve", lambda e: e.tensor_scalar(out=ang[:], in0=ang[:], scalar1=fcol[:, 0:1], scalar2=None, op0=ALU.mult),
                     reads=[b_ang, b_fcol], writes=[b_ang])

                def sin_of(dst, b_dst, shift):
                    K.op("dve", lambda e: e.tensor_scalar(out=t2[:], in0=ang[:], scalar1=float(shift), scalar2=None, op0=ALU.add),
                         reads=[b_ang], writes=[b_t2])
                    K.op("dve", lambda e: e.tensor_scalar(out=t1[:], in0=t2[:], scalar1=float(1.0 / (2 * np.pi)), scalar2=MAGIC,
                                                          op0=ALU.mult, op1=ALU.add), reads=[b_t2], writes=[b_t1])
                    K.op("dve", lambda e: e.tensor_scalar(out=t1[:], in0=t1[:], scalar1=-MAGIC, scalar2=None, op0=ALU.add),
                         reads=[b_t1], writes=[b_t1])
                    K.op("dve", lambda e: e.scalar_tensor_tensor(out=t2[:], in0=t1[:], scalar=-C1, in1=t2[:], op0=ALU.mult,
                                                                 op1=ALU.add), reads=[b_t1, b_t2], writes=[b_t2])
                    K.op("dve", lambda e: e.scalar_tensor_tensor(out=t2[:], in0=t1[:], scalar=-C2, in1=t2[:], op0=ALU.mult,
                                                                 op1=ALU.add), reads=[b_t1, b_t2], writes=[b_t2])
                    K.op("dve", lambda e: e.tensor_scalar(out=t2[:], in0=t2[:], scalar1=PIL, scalar2=-PIL, op0=ALU.min,
                                                          op1=ALU.max), reads=[b_t2], writes=[b_t2])
                    K.op("act", lambda e: e.activation(out=dst[:, cs], in_=t2[:], func=AF.Sin), reads=[b_t2], writes=[b_dst])

                sin_of(sinT, b_sin, 0.0)
                sin_of(cosT, b_cos, np.pi / 2)
            K.op("dve", lambda e: e.tensor_scalar(out=sinT[:], in0=sinT[:], scalar1=fcol[:, 1:2], scalar2=None, op0=ALU.mult),
                 reads=[b_sin, b_fcol], writes=[b_sin])
            K.barrier()

        rot_o = [sb(st_rot, "rot_o%d" % i, [128, S], BF16) for i in range(2)]
        for gi in range((N_ROT + 1) // 2):
            wa, bwa = load_w(w_rot_d[:, gi * 128:(gi + 1) * 128], 128)
            ws, bws = load_w(w_rot_sw_d[:, gi * 128:(gi + 1) * 128], 128)
            ro, bro = rot_o[gi % 2]
            for sc in range(NS):
                (p1, pb1), (p2, pb2) = PS[2 + (sc % 2) * 2], PS[3 + (sc % 2) * 2]
                tok = slice(sc * 512, (sc + 1) * 512)
                for k in range(8):
                    K.op("pe", lambda e: e.matmul(p1[:, :], lhsT=wa[:, k, 0:128], rhs=n1T[:, k, tok], start=(k == 0), stop=(k == 7)),
                         reads=[bwa] + allb(), writes=[pb1])
                for k in range(8):
                    K.op("pe", lambda e: e.matmul(p2[:, :], lhsT=ws[:, k, 0:128], rhs=n1T[:, k, tok], start=(k == 0), stop=(k == 7)),
                         reads=[bws] + allb(), writes=[pb2])
                K.op("dve", lambda e: e.tensor_tensor(out=tmpa[:], in0=p1[:, :], in1=cosT[:, tok], op=ALU.mult),
                     reads=[pb1, b_cos], writes=[b_tmpa])
                K.op("dve", lambda e: e.tensor_tensor(out=tmpb[:], in0=p2[:, :], in1=sinT[:, tok], op=ALU.mult),
                     reads=[pb2, b_sin], writes=[b_tmpb])
                K.op("pool", lambda e: e.tensor_tensor(out=ro[:, tok], in0=tmpa[:], in1=tmpb[:], op=ALU.add),
                     reads=[b_tmpa, b_tmpb], writes=[bro])
            store(ROT_d[2 * gi], ro[0:64, :], bro, dROT)
            if 2 * gi + 1 < N_ROT:
                store(ROT_d[2 * gi + 1], ro[64:128, :], bro, dROT)

        K.barrier()
        st_rot.close()
        st_cv = ExitStack()
        ccol, b_ccol = sb(st, "convcol", [128, 12, 4])
        load(ccol[:], conv_col_d, b_ccol)
        onesbd, b_onesbd = sb(st, "onesbd", [128, 128])
        load(onesbd[:], onesbd_d, b_onesbd)
        xc, b_xc = sb(st_cv, "xc", [128, S + 3])
        yc, b_yc = sb(st_cv, "yc", [128, S])
        conv_o = [sb(st_cv, "conv_o%d" % i, [128, S]) for i in range(1)]
        K.op("pool", lambda e: e.memset(xc[:, 0:3], 0.0), writes=[b_xc])
        for cg in range(12):
            wa, bwa = load_w(w_conv_d[:, cg * 128:(cg + 1) * 128], 128)
            co, bco = conv_o[0]
            for sc in range(NS):
                p1, pb1 = PS[2 + (sc % 2)]
                tok = slice(sc * 512, (sc + 1) * 512)
                for k in range(8):
                    K.op("pe", lambda e: e.matmul(p1[:, :], lhsT=wa[:, k, 0:128], rhs=n1T[:, k, tok], start=(k == 0), stop=(k == 7)),
                         reads=[bwa] + allb(), writes=[pb1])
                K.op("act", lambda e: e.activation(out=xc[:, 3 + sc * 512:3 + (sc + 1) * 512], in_=p1[:, :], func=AF.Copy),
                     reads=[pb1], writes=[b_xc])
            K.op("dve", lambda e: e.tensor_scalar(out=yc[:], in0=xc[:, 3:S + 3], scalar1=ccol[:, cg, 3:4], scalar2=None, op0=ALU.mult),
                 reads=[b_xc, b_ccol], writes=[b_yc])
            for i in range(3):
                K.op("dve", lambda e: e.scalar_tensor_tensor(out=yc[:], in0=xc[:, i:S + i], scalar=ccol[:, cg, i:i + 1], in1=yc[:],
                                                             op0=ALU.mult, op1=ALU.add), reads=[b_xc, b_ccol, b_yc], writes=[b_yc])
            if cg >= 8:
                K.op("act", lambda e: e.activation(out=co[:], in_=yc[:], func=AF.Silu), reads=[b_yc], writes=[bco])
            else:
                K.op("act", lambda e: e.activation(out=yc[:], in_=yc[:], func=AF.Silu), reads=[b_yc], writes=[b_yc])
                for sc in range(NS):
                    p1, pb1 = PS[4 + (sc % 2)]
                    tok = slice(sc * 512, (sc + 1) * 512)
                    K.op("pool", lambda e: e.tensor_tensor(out=tmpa[:], in0=yc[:, tok], in1=yc[:, tok], op=ALU.mult),
                         reads=[b_yc], writes=[b_tmpa])
                    K.op("pe", lambda e: e.matmul(p1[:, :], lhsT=onesbd[:], rhs=tmpa[:], start=True, stop=True),
                         reads=[b_onesbd, b_tmpa], writes=[pb1])
                    K.op("act", lambda e: e.activation(out=tmpb[:], in_=p1[:, :], func=AF.Sqrt, bias=EPS, scale=1.0),
                         reads=[pb1], writes=[b_tmpb])
                    K.op("dve", lambda e: e.reciprocal(out=tmpb[:], in_=tmpb[:]), reads=[b_tmpb], writes=[b_tmpb])
                    qs = 0.125 if cg < 4 else 1.0
                    K.op("dve", lambda e: e.scalar_tensor_tensor(out=co[:, tok], in0=yc[:, tok], scalar=qs, in1=tmpb[:], op0=ALU.mult, op1=ALU.mult),
                         reads=[b_yc, b_tmpb], writes=[bco])
            store(CONV_d[2 * cg], co[0:64, :], bco, dCONV)
            store(CONV_d[2 * cg + 1], co[64:128, :], bco, dCONV)
        K.barrier()
        st_cv.close()
        gate_o = [sb(st, "gate_o%d" % i, [128, S], BF16) for i in range(2)]
        for gc in range(16):
            wa, bwa = load_w(w_gate_d[:, gc * 128:(gc + 1) * 128], 128)
            go, bgo = gate_o[gc % 2]
            for sc in range(NS):
                p1, pb1 = PS[2 + (sc % 2)]
                tok = slice(sc * 512, (sc + 1) * 512)
                for k in range(8):
                    K.op("pe", lambda e, k=k, wa=wa, p1=p1, tok=tok: e.matmul(p1[:, :], lhsT=wa[:, k, 0:128], rhs=n1T[:, k, tok],
                                                                              start=(k == 0), stop=(k == 7)),
                         reads=[bwa] + allb(), writes=[pb1])
                K.op("act", lambda e, p1=p1, go=go, tok=tok: e.activation(out=go[:, tok], in_=p1[:, :], func=AF.Sigmoid),
                     reads=[pb1], writes=[bgo])
            store(GATE_d[gc], go[:], bgo, dGATE)

        wt_, bwt = load_w(w_tok_d, 144)
        wz_, bwz = load_w(w_z_d, 512)
        wg_, bwg = load_w(w_bg_d, 16)
        alog, b_alog = sb(st, "alog", [128, 8])
        dtb, b_dtb = sb(st, "dtb", [128, 8])
        load(alog[0:64, :], a_log_d, b_alog)
        load(alog[64:128, :], a_log_d, b_alog)
        load(dtb[0:64, :], dt_bias_d, b_dtb)
        load(dtb[64:128, :], dt_bias_d, b_dtb)
        K.op("act", lambda e: e.activation(out=alog[:], in_=alog[:], func=AF.Exp), reads=[b_alog], writes=[b_alog])
        K.op("dve", lambda e: e.tensor_scalar(out=alog[:], in0=alog[:], scalar1=-1.0, scalar2=None, op0=ALU.mult),
             reads=[b_alog], writes=[b_alog])
        vw_o = [sb(st, "vw_o%d" % i, [128, 144]) for i in range(2)]
        zs_o = [sb(st, "zs_o%d" % i, [128, 512]) for i in range(2)]
        bg_o = [sb(st, "bg_o%d" % i, [128, 16]) for i in range(2)]
        for i in range(NT):
            tk = slice(i * 128, (i + 1) * 128)
            (p1, pb1), (p2, pb2), (p3, pb3) = PS[2], PS[3], PS[4]
            vo, bvo = vw_o[i % 2]
            zo, bzo = zs_o[i % 2]
            bo, bbo = bg_o[i % 2]
            for k in range(8):
                K.op("pe", lambda e, k=k, tk=tk: e.matmul(p1[:, 0:144], lhsT=n1T[:, k, tk], rhs=wt_[:, k, 0:144],
                                                          start=(k == 0), stop=(k == 7)), reads=[bwt, b_n1T[i]], writes=[pb1])
            for k in range(8):
                K.op("pe", lambda e, k=k, tk=tk: e.matmul(p2[:, 0:512], lhsT=n1T[:, k, tk], rhs=wz_[:, k, 0:512],
                                                          start=(k == 0), stop=(k == 7)), reads=[bwz, b_n1T[i]], writes=[pb2])
            for k in range(8):
                K.op("pe", lambda e, k=k, tk=tk: e.matmul(p3[:, 0:16], lhsT=n1T[:, k, tk], rhs=wg_[:, k, 0:16],
                                                          start=(k == 0), stop=(k == 7)), reads=[bwg, b_n1T[i]], writes=[pb3])
            K.op("act", lambda e, vo=vo: e.activation(out=vo[:], in_=p1[:, 0:144], func=AF.Copy), reads=[pb1], writes=[bvo])
            K.op("act", lambda e, zo=zo: e.activation(out=zo[:], in_=p2[:, 0:512], func=AF.Silu), reads=[pb2], writes=[bzo])
            K.op("act", lambda e, bo=bo: e.activation(out=bo[:, 0:8], in_=p3[:, 0:8], func=AF.Sigmoid), reads=[pb3], writes=[bbo])
            K.op("dve", lambda e, bo=bo: e.tensor_tensor(out=bo[:, 8:16], in0=p3[:, 8:16], in1=dtb[:], op=ALU.add),
                 reads=[pb3, b_dtb, bbo], writes=[bbo])
            K.op("act", lambda e, bo=bo: e.activation(out=bo[:, 8:16], in_=bo[:, 8:16], func=AF.Exp), reads=[bbo], writes=[bbo])
            K.op("act", lambda e, bo=bo: e.activation(out=bo[:, 8:16], in_=bo[:, 8:16], func=AF.Ln, bias=1.0, scale=1.0),
                 reads=[bbo], writes=[bbo])
            K.op("dve", lambda e, bo=bo: e.tensor_tensor(out=bo[:, 8:16], in0=bo[:, 8:16], in1=alog[:], op=ALU.mult),
                 reads=[bbo, b_alog], writes=[bbo])
            store(VW_d[tk, :], vo[:], bvo, dVW)
            store(ZS_d[tk, :], zo[:], bzo, dZS)
            store(BG_d[tk, :], bo[:], bbo, dBG)
        K.barrier()

    if dbg:
        for nm, src in (("ROT", ROT_d), ("CONVO", CONV_d), ("GATE", GATE_d), ("VW", VW_d), ("ZS", ZS_d), ("BG", BG_d)):
            pass
    if stop_after == "A":
        return finish()

    with ExitStack() as st:
        kiT, b_kiT = sb(st, "kiT", [64, S], BF16)
        kaT, b_kaT = sb(st, "kaT", [64, 2, S], BF16)
        load(kiT[:], ROT_d[26], b_kiT, dROT)
        for g in range(2):
            load(kaT[:, g, :], ROT_d[8 + g], b_kaT, dROT)
        va_bf, b_va = sb(st, "va_bf", [128, NT, 128], BF16)
        wi_s, b_wi = sb(st, "wi_s", [128, NT, 16])
        with ExitStack() as st2:
            vw_f, b_vwf = sb(st2, "vw_f", [128, NT, 144])
            load(vw_f[:], VW_d.rearrange("(n p) c -> p n c", p=128), b_vwf, dVW)
            K.op("pool", lambda e: e.tensor_copy(out=va_bf[:], in_=vw_f[:, :, 0:128]), reads=[b_vwf], writes=[b_va])
            K.op("dve", lambda e: e.tensor_scalar(out=wi_s[:], in0=vw_f[:, :, 128:144], scalar1=1.0 / 32.0, scalar2=None,
                                                  op0=ALU.mult), reads=[b_vwf], writes=[b_wi])
            K.barrier()
        ones_bf, b_onesbf = sb(st, "ones_bf", [128, 64], BF16)
        K.op("pool", lambda e: e.memset(ones_bf[:], 1.0), writes=[b_onesbf])
        id4, b_id4 = sb(st, "id4", [128, 4, 128], BF16)
        for r4 in range(4):
            K.op("pool", lambda e, r4=r4: e.tensor_copy(out=id4[:, r4, :], in_=ident[:]), reads=[b_ident], writes=[b_id4])
        cmask, b_cmask = sb(st, "cmask", [128, 128])
        load(cmask[:], cmask_d, b_cmask)
        qi_ts = [sb(st, "qi_t%d" % i, [64, 16, 128], BF16) for i in range(2)]
        qa_ts = [sb(st, "qa_t%d" % i, [64, 8, 128], BF16) for i in range(2)]
        scores = [sb(st, "score%d" % i, [128, S]) for i in range(2)]
        work, b_work = sb(st, "work", [128, S])
        mnegs = [sb(st, "mneg%d" % i, [128, S], BF16) for i in range(2)]
        m8, b_m8 = sb(st, "m8", [128, 8])
        thr, b_thr = sb(st, "thr", [128, 8])
        rls = [sb(st, "rl%d" % i, [128, 512]) for i in range(2)]
        pTs = [sb(st, "pT%d" % i, [128, 512], BF16) for i in range(2)]
        rden, b_rden = sb(st, "rden", [64, 512])
        oas = [sb(st, "oa%d" % i, [64, 4, 128], BF16) for i in range(2)]
        wdiags = [sb(st, "wdiag%d" % i, [128, 16, 128], BF16) for i in range(2)]
        rlbs = [sb(st, "rlb%d" % i, [128, 512], BF16) for i in range(3)]
        ctr = {"nlog": 0, "nst": 0, "nch": 0}

        def tile_ctx(i):
            return dict(Si=128 * (i + 1), tk=slice(i * 128, (i + 1) * 128), qi=qi_ts[i % 2], qa=qa_ts[i % 2],
                        score=scores[i % 2], mneg=mnegs[i % 2], wd=wdiags[i % 2])

        def indexer(i):
            c = tile_ctx(i)
            Si, tk = c["Si"], c["tk"]
            qi_t, b_qi = c["qi"]
            qa_t, b_qa = c["qa"]
            score, b_score = c["score"]
            wd, b_wd = c["wd"]
            load(qi_t[:], ROT_d[10:26, :, tk].rearrange("h p t -> p h t"), b_qi, dROT)
            load(qa_t[:], ROT_d[0:8, :, tk].rearrange("h p t -> p h t"), b_qa, dROT)
            for h in range(16):
                K.op("pool", lambda e: e.tensor_scalar(out=wd[:, h, :], in0=ident[:], scalar1=wi_s[:, i, h:h + 1], scalar2=None, op0=ALU.mult),
                     reads=[b_ident, b_wi], writes=[b_wd])
            steps = [(c0, min(512, Si - c0), h) for c0 in range(0, Si, 512) for h in range(16)]
            state = {}

            def logits(k):
                c0, wc, h = steps[k]
                if h == 0:
                    state[c0] = PS[6 + ctr["nch"] % 2]
                    ctr["nch"] += 1
                pl, pbl = PS[ctr["nlog"] % 2]
                rl, brl = rlbs[ctr["nlog"] % 3]
                ctr["nlog"] += 1
                K.op("pe", lambda e: e.matmul(pl[:, 0:wc], lhsT=qi_t[:, h, :], rhs=kiT[:, c0:c0 + wc], start=True, stop=True),
                     reads=[b_qi, b_kiT], writes=[pbl])
                K.op("act", lambda e: e.activation(out=rl[:, 0:wc], in_=pl[:, 0:wc], func=AF.Relu), reads=[pbl], writes=[brl])
                return rl, brl

            def accum(k, rl, brl):
                c0, wc, h = steps[k]
                psc, pbsc = state[c0]
                K.op("pe", lambda e: e.matmul(psc[:, 0:wc], lhsT=wd[:, h, :], rhs=rl[:, 0:wc], start=(h == 0), stop=(h == 15)),
                     reads=[b_wd, brl], writes=[pbsc])
                if h == 15:
                    K.op("act", lambda e: e.activation(out=score[:, c0:c0 + wc], in_=psc[:, 0:wc], func=AF.Copy), reads=[pbsc], writes=[b_score])

            prev = logits(0)
            for k in range(len(steps)):
                nxt = logits(k + 1) if k + 1 < len(steps) else None
                accum(k, *prev)
                prev = nxt

        def threshold(i):
            c = tile_ctx(i)
            Si, tk = c["Si"], c["tk"]
            score, b_score = c["score"]
            mneg, b_mneg = c["mneg"]
            if Si > TOPK:
                K.op("dve", lambda e: e.tensor_reduce(out=thr[:, 2:3], in_=score[:, 0:Si], axis=AX.X, op=ALU.min), reads=[b_score], writes=[b_thr])
                K.op("dve", lambda e: e.tensor_reduce(out=thr[:, 3:4], in_=score[:, 0:Si], axis=AX.X, op=ALU.max), reads=[b_score], writes=[b_thr])
                NIT = 24
                K.op("dve", lambda e: e.tensor_tensor(out=score[:, tk], in0=score[:, tk], in1=cmask[:], op=ALU.add),
                     reads=[b_score, b_cmask], writes=[b_score])
                K.op("dve", lambda e: e.tensor_scalar(out=thr[:, 0:1], in0=thr[:, 2:3], scalar1=1.0, scalar2=None, op0=ALU.mult),
                     reads=[b_thr], writes=[b_thr])
                K.op("dve", lambda e: e.tensor_tensor(out=thr[:, 1:2], in0=thr[:, 3:4], in1=thr[:, 2:3], op=ALU.subtract),
                     reads=[b_thr], writes=[b_thr])
                for it in range(1, NIT + 1):
                    sc_ = float(2.0 ** (-it))
                    K.op("dve", lambda e: e.scalar_tensor_tensor(out=thr[:, 4:5], in0=thr[:, 1:2], scalar=sc_, in1=thr[:, 0:1],
                                                                 op0=ALU.mult, op1=ALU.add), reads=[b_thr], writes=[b_thr])
                    K.op("dve", lambda e: e.tensor_scalar(out=work[:, 0:Si], in0=score[:, 0:Si], scalar1=thr[:, 4:5], scalar2=0.0,
                                                          op0=ALU.is_ge, op1=ALU.add, accum_out=thr[:, 5:6]),
                         reads=[b_score, b_thr], writes=[b_work, b_thr])
                    K.op("dve", lambda e: e.tensor_scalar(out=thr[:, 6:7], in0=thr[:, 5:6], scalar1=float(TOPK) - 0.5, scalar2=sc_,
                                                          op0=ALU.is_ge, op1=ALU.mult), reads=[b_thr], writes=[b_thr])
                    K.op("dve", lambda e: e.scalar_tensor_tensor(out=thr[:, 0:1], in0=thr[:, 6:7], scalar=thr[:, 1:2], in1=thr[:, 0:1],
                                                                 op0=ALU.mult, op1=ALU.add), reads=[b_thr], writes=[b_thr])
                K.op("dve", lambda e: e.tensor_scalar(out=mneg[:, 0:Si], in0=score[:, 0:Si], scalar1=thr[:, 0:1], scalar2=-30000.0,
                                                      op0=ALU.is_lt, op1=ALU.mult), reads=[b_score, b_thr], writes=[b_mneg])
            else:
                K.op("dve", lambda e: e.tensor_tensor(out=score[:, tk], in0=score[:, tk], in1=cmask[:], op=ALU.add),
                     reads=[b_score, b_cmask], writes=[b_score])
                K.op("dve", lambda e: e.tensor_scalar(out=mneg[:, 0:Si], in0=score[:, 0:Si], scalar1=-1.0e29, scalar2=-30000.0,
                                                      op0=ALU.is_lt, op1=ALU.mult), reads=[b_score], writes=[b_mneg])

        def attention(i):
            c = tile_ctx(i)
            tk = c["tk"]
            qa_t, b_qa = c["qa"]
            mneg, b_mneg = c["mneg"]
            for g in range(2):
                pn, pbn = PS[4]
                pd, pbd = PS[5]

                def qk(j):
                    kk = slice(j * 128, (j + 1) * 128)
                    psT_, pbs = PS[2 + ctr["nst"] % 2]
                    pT, bpT = pTs[ctr["nst"] % 2]
                    ctr["nst"] += 1
                    K.op("pe", lambda e: e.matmul(psT_[:, :], lhsT=kaT[:, g, kk], rhs=qa_t[:, 4 * g:4 * g + 4, :], start=True, stop=False),
                         reads=[b_kaT, b_qa], writes=[pbs])
                    K.op("pe", lambda e: e.matmul(psT_[:, :], lhsT=mneg[:, kk], rhs=id4[:], start=False, stop=True),
                         reads=[b_mneg, b_id4], writes=[pbs])
                    K.op("act", lambda e: e.activation(out=pT[:], in_=psT_[:, :], func=AF.Exp, scale=0.125), reads=[pbs], writes=[bpT])
                    return pT, bpT

                def pv_(j, pT, bpT):
                    K.op("pe", lambda e: e.matmul(pn[0:64, :], lhsT=va_bf[:, j, g * 64:(g + 1) * 64], rhs=pT[:], start=(j == 0), stop=(j == i)),
                         reads=[b_va, bpT], writes=[pbn])
                    K.op("pe", lambda e: e.matmul(pd[0:64, :], lhsT=ones_bf[:], rhs=pT[:], start=(j == 0), stop=(j == i)),
                         reads=[b_onesbf, bpT], writes=[pbd])

                prev = qk(0)
                for j in range(i + 1):
                    nxt = qk(j + 1) if j + 1 <= i else None
                    pv_(j, *prev)
                    prev = nxt
                oa, boa = oas[g]
                K.op("dve", lambda e: e.reciprocal(out=rden[:], in_=pd[0:64, :]), reads=[pbd], writes=[b_rden])
                K.op("dve", lambda e: e.tensor_tensor(out=oa[:].rearrange("p h t -> p (h t)"), in0=pn[0:64, :], in1=rden[:], op=ALU.mult),
                     reads=[pbn, b_rden], writes=[boa])
                store(OA_d[4 * g:4 * g + 4, :, tk].rearrange("h p t -> p h t"), oa[:], boa, dOA)

        indexer(0)
        threshold(0)
        for i in range(NT):
            if i + 1 < NT:
                indexer(i + 1)
            attention(i)
            if i + 1 < NT:
                threshold(i + 1)
        K.barrier()
    if stop_after == "B":
        return finish()

    with ExitStack() as st:
        def cst(name, src):
            t, b = sb(st, name, [64, 64])
            load(t[:], src, b)
            return t, b
        ut64, b_ut = cst("ut64", ut64_d)
        sl64, b_sl = cst("sl64", sl64_d)
        iu64, b_iu = cst("iu64", iu64_d)
        normb, b_normb = cst("normb", norm_b_d)
        id64 = ident[0:64, 0:64]
        ones64f = ones128[0:64, 0:64]
        nones, b_nones = sb(st, "nones64", [64, 64])
        K.op("pool", lambda e: e.memset(nones[:], -1.0), writes=[b_nones])
        bg, b_bg = sb(st, "bg_all", [64, NCH, 16])
        load(bg[:], BG_d.rearrange("(n c) f -> c n f", c=64), b_bg, dBG)
        nbeta, b_nbeta = sb(st, "nbeta", [64, NCH, 8])
        K.op("dve", lambda e: e.tensor_scalar(out=nbeta[:], in0=bg[:, :, 0:8], scalar1=-1.0, scalar2=None, op0=ALU.mult),
             reads=[b_bg], writes=[b_nbeta])
        obT, b_obT = sb(st, "obT_all", [64, 8, S], BF16)
        Sst, b_S = sb(st, "Sstate", [64, 8, 64])
        K.op("pool", lambda e: e.memset(Sst[:], 0.0), writes=[b_S])

        def bc_h(t2d):
            return t2d.rearrange("p (o c) -> p o c", o=1).to_broadcast([64, 8, 64])

        def bc_l(tp8):
            return tp8.rearrange("p (h o) -> p h o", o=1).to_broadcast([64, 8, 64])

        def scr(name, shape=(64, 8, 64)):
            return [sb(st, "%s_%d" % (name, i), list(shape)) for i in range(2)]
        names = ("qc", "kc", "vc", "Gm", "tt", "Esl", "Eu", "M", "MT", "A", "AT", "P",
                 "VB", "KBg", "Kd", "IT", "U", "KC", "vn", "o2", "oo", "obc")
        S3 = {nm: scr(nm) for nm in names}
        s_zc = scr("zc", (64, 512))
        s_ex = scr("ex3", (64, 3, 8))
        s_sm = scr("smallc", (64, 4, 8))

        def pv(bank):
            return PS[bank][0][0:64, :].rearrange("p (h c) -> p h c", h=8), PS[bank][1]

        def mm(dst_ap, dst_b, lhsT, rhs, reads, start=True, stop=True, full=False):
            K.op("pe", lambda e: e.matmul(dst_ap, lhsT=lhsT, rhs=rhs, start=start, stop=stop), reads=reads, writes=[dst_b])

        def tr(dst_ap, dst_b, src, reads):
            K.op("pe", lambda e: e.transpose(dst_ap, src, id64), reads=reads + [b_ident], writes=[dst_b])

        for n in range(NCH):
            cs = slice(n * 64, (n + 1) * 64)
            pz = n % 2
            T = {nm: S3[nm][pz] for nm in names}
            (qc, b_q), (kc, b_k), (vc, b_v) = T["qc"], T["kc"], T["vc"]
            (Gm, bGm), (tt, btt), (Esl, bEsl), (Eu, bEu) = T["Gm"], T["tt"], T["Esl"], T["Eu"]
            (M, bM), (MT, bMT), (A, bA), (AT, bAT), (P, bP) = T["M"], T["MT"], T["A"], T["AT"], T["P"]
            (VB, bVB), (KBg, bKBg), (Kd, bKd), (IT, bIT) = T["VB"], T["KBg"], T["Kd"], T["IT"]
            (U, bU), (KC, bKC), (vn, bvn), (o2, bo2), (oo, boo), (obc, bobc) = T["U"], T["KC"], T["vn"], T["o2"], T["oo"], T["obc"]
            zc, b_z = s_zc[pz]
            ex3, bex = s_ex[pz]
            sm, bsm = s_sm[pz]
            load(qc[:], CONV_d[0:8, :, cs].rearrange("h p t -> p h t"), b_q, dCONV)
            load(kc[:], CONV_d[8:16, :, cs].rearrange("h p t -> p h t"), b_k, dCONV)
            load(vc[:], CONV_d[16:24, :, cs].rearrange("h p t -> p h t"), b_v, dCONV)
            load(zc[:], ZS_d[cs, :], b_z, dZS)
            g8 = bg[:, n, 8:16]
            beta8 = bg[:, n, 0:8]
            nb8 = nbeta[:, n, :]
            K.op("dve", lambda e: e.tensor_tensor(out=Gm[:], in0=bc_h(ut64[:]), in1=bc_l(g8), op=ALU.mult),
                 reads=[b_ut, b_bg], writes=[bGm])
            pD, bpD = pv(0)
            mm(PS[0][0][0:64, :], bpD, nones[:], Gm[:].rearrange("p h c -> p (h c)"), [bGm, b_nones], True, False, full=True)
            for h in range(8):
                mm(pD[:, h, :], bpD, Gm[:, h, :], ones64f, [bGm, b_ones], False, h == 7, full=True)
            pG, bpG = PS[1][0][0:64, 0:24].rearrange("p (a h) -> p a h", a=3), PS[1][1]
            mm(pG[:, 0, :], bpG, ut64[:], g8, [b_ut, b_bg], full=True)
            mm(pG[:, 1, :], bpG, ones64f, g8, [b_ones, b_bg], full=True)
            mm(pG[:, 2, :], bpG, sl64[:], g8, [b_sl, b_bg], full=True)
            K.op("act", lambda e: e.activation(out=ex3[:], in_=pG, func=AF.Exp), reads=[bpG], writes=[bex])
            K.op("dve", lambda e: e.tensor_scalar(out=tt[:], in0=pD, scalar1=0.0, scalar2=None, op0=ALU.min), reads=[bpD], writes=[btt])
            K.op("act", lambda e: e.activation(out=Esl[:], in_=tt[:], func=AF.Exp), reads=[btt], writes=[bEsl])
            K.op("pool", lambda e: e.tensor_tensor(out=Esl[:], in0=Esl[:], in1=bc_h(sl64[:]), op=ALU.mult), reads=[bEsl, b_sl], writes=[bEsl])
            K.op("dve", lambda e: e.tensor_scalar(out=tt[:], in0=pD, scalar1=-1.0, scalar2=0.0, op0=ALU.mult, op1=ALU.min),
                 reads=[bpD, bEsl], writes=[btt])
            K.op("act", lambda e: e.activation(out=Eu[:], in_=tt[:], func=AF.Exp), reads=[btt], writes=[bEu])
            K.op("pool", lambda e: e.tensor_tensor(out=Eu[:], in0=Eu[:], in1=bc_h(iu64[:]), op=ALU.mult), reads=[bEu, b_iu], writes=[bEu])
            pKK, bpKK = pv(2)
            for h in range(8):
                mm(pKK[:, h, :], bpKK, kc[:, h, :], kc[:, h, :], [b_k], full=True)
            K.op("dve", lambda e: e.tensor_tensor(out=M[:], in0=pKK, in1=bc_l(nb8), op=ALU.mult), reads=[bpKK, b_nbeta], writes=[bM])
            K.op("pool", lambda e: e.tensor_tensor(out=M[:], in0=M[:], in1=Esl[:], op=ALU.mult), reads=[bM, bEsl], writes=[bM])
            pMt, bpMt = pv(3)
            for h in range(8):
                tr(pMt[:, h, :], bpMt, M[:, h, :], [bM])
            K.op("act", lambda e: e.activation(out=MT[:], in_=pMt, func=AF.Copy), reads=[bpMt], writes=[bMT])
            K.op("pool", lambda e: e.tensor_tensor(out=P[:], in0=MT[:], in1=bc_h(id64), op=ALU.add), reads=[bMT, b_ident], writes=[bP])
            cA, cbA, cAT, cbAT = M, bM, MT, bMT
            pA2, bpA2 = pv(4)
            pA2T, bpA2T = pv(5)
            pPp, bpPp = pv(6)
            for lv in range(5):
                for h in range(8):
                    mm(pA2[:, h, :], bpA2, cAT[:, h, :], cA[:, h, :], [cbA, cbAT], full=True)
                if lv < 4:
                    for h in range(8):
                        mm(pA2T[:, h, :], bpA2T, cA[:, h, :], cAT[:, h, :], [cbA, cbAT], full=True)
                K.op("act", lambda e: e.activation(out=A[:], in_=pA2, func=AF.Copy), reads=[bpA2, cbA], writes=[bA])
                if lv < 4:
                    K.op("dve", lambda e: e.tensor_copy(out=AT[:], in_=pA2T), reads=[bpA2T, cbAT], writes=[bAT])
                cA, cbA, cAT, cbAT = A, bA, AT, bAT
                for h in range(8):
                    mm(pPp[:, h, :], bpPp, A[:, h, :], P[:, h, :], [bA, bP], full=True)
                K.op("dve", lambda e: e.tensor_tensor(out=P[:], in0=pPp, in1=P[:], op=ALU.add), reads=[bpPp, bP], writes=[bP])
            pVt, bpVt = pv(7)
            pKt, bpKt = pv(3)
            for h in range(8):
                tr(pVt[:, h, :], bpVt, vc[:, h, :], [b_v])
            for h in range(8):
                tr(pKt[:, h, :], bpKt, kc[:, h, :], [b_k])
            K.op("dve", lambda e: e.tensor_tensor(out=VB[:], in0=pVt, in1=bc_l(beta8), op=ALU.mult), reads=[bpVt, b_bg], writes=[bVB])
            K.op("pool", lambda e: e.tensor_tensor(out=sm[:, 0, :], in0=beta8, in1=ex3[:, 0, :], op=ALU.mult), reads=[b_bg, bex], writes=[bsm])
            K.op("dve", lambda e: e.tensor_tensor(out=KBg[:], in0=pKt, in1=bc_l(sm[:, 0, :]), op=ALU.mult), reads=[bpKt, bsm], writes=[bKBg])
            K.op("dve", lambda e: e.tensor_tensor(out=Kd[:], in0=pKt, in1=bc_l(ex3[:, 2, :]), op=ALU.mult), reads=[bpKt, bex], writes=[bKd])
            pU, bpU = pv(0)
            pKC, bpKC = pv(2)
            for h in range(8):
                mm(pU[:, h, :], bpU, P[:, h, :], VB[:, h, :], [bP, bVB])
            for h in range(8):
                mm(pKC[:, h, :], bpKC, KBg[:, h, :], P[:, h, :], [bP, bKBg])
            K.op("act", lambda e: e.activation(out=U[:], in_=pU, func=AF.Copy), reads=[bpU], writes=[bU])
            K.op("act", lambda e: e.activation(out=KC[:], in_=pKC, func=AF.Copy), reads=[bpKC], writes=[bKC])
            pKQ, bpKQ = pv(7)
            for h in range(8):
                mm(pKQ[:, h, :], bpKQ, kc[:, h, :], qc[:, h, :], [b_k, b_q])
            K.op("dve", lambda e: e.tensor_tensor(out=IT[:], in0=pKQ, in1=Eu[:], op=ALU.mult), reads=[bpKQ, bEu], writes=[bIT])
            pVN, bpVN = pv(4)
            pO1, bpO1 = pv(5)
            for h in range(8):
                mm(pVN[:, h, :], bpVN, KC[:, h, :], Sst[:, h, :], [bKC, b_S])
            for h in range(8):
                mm(pO1[:, h, :], bpO1, qc[:, h, :], Sst[:, h, :], [b_q, b_S])
            K.op("dve", lambda e: e.tensor_tensor(out=vn[:], in0=U[:], in1=pVN, op=ALU.subtract), reads=[bU, bpVN], writes=[bvn])
            pO2, bpO2 = pv(6)
            pSd, bpSd = pv(3)
            for h in range(8):
                mm(pO2[:, h, :], bpO2, IT[:, h, :], vn[:, h, :], [bIT, bvn])
            for h in range(8):
                mm(pSd[:, h, :], bpSd, Kd[:, h, :], vn[:, h, :], [bKd, bvn])
            K.op("act", lambda e: e.activation(out=o2[:], in_=pO2, func=AF.Copy), reads=[bpO2], writes=[bo2])
            K.op("dve", lambda e: e.tensor_tensor(out=oo[:], in0=pO1, in1=bc_l(ex3[:, 0, :]), op=ALU.mult), reads=[bpO1, bex], writes=[boo])
            K.op("pool", lambda e: e.tensor_tensor(out=oo[:], in0=oo[:], in1=o2[:], op=ALU.add), reads=[boo, bo2], writes=[boo])
            K.op("pool", lambda e: e.tensor_tensor(out=Sst[:], in0=Sst[:], in1=bc_l(ex3[:, 1, :]), op=ALU.mult), reads=[b_S, bex], writes=[b_S])
            K.op("dve", lambda e: e.tensor_tensor(out=Sst[:], in0=pSd, in1=Sst[:], op=ALU.add), reads=[bpSd, b_S], writes=[b_S])
            K.op("pool", lambda e: e.tensor_tensor(out=o2[:], in0=oo[:], in1=oo[:], op=ALU.mult), reads=[boo, bo2], writes=[bo2])
            K.op("dve", lambda e: e.tensor_reduce(out=sm[:, 1, :], in_=o2[:], axis=AX.X, op=ALU.add), reads=[bo2], writes=[bsm])
            K.op("act", lambda e: e.activation(out=sm[:, 2, :], in_=sm[:, 1, :], func=AF.Sqrt, bias=EPS, scale=1.0 / 64.0), reads=[bsm], writes=[bsm])
            K.op("dve", lambda e: e.reciprocal(out=sm[:, 3, :], in_=sm[:, 2, :]), reads=[bsm], writes=[bsm])
            K.op("dve", lambda e: e.tensor_tensor(out=obc[:], in0=oo[:], in1=bc_l(sm[:, 3, :]), op=ALU.mult), reads=[boo, bsm], writes=[bobc])
            K.op("pool", lambda e: e.tensor_tensor(out=obc[:], in0=obc[:], in1=bc_h(normb[:]), op=ALU.mult), reads=[bobc, b_normb], writes=[bobc])
            K.op("dve", lambda e: e.tensor_tensor(out=obc[:], in0=obc[:], in1=zc[:].rearrange("p (h c) -> p h c", h=8), op=ALU.mult),
                 reads=[bobc, b_z], writes=[bobc])
            pOT, bpOT = pv(1)
            for h in range(8):
                tr(pOT[:, h, :], bpOT, obc[:, h, :], [bobc])
            K.op("act", lambda e: e.activation(out=obT[:, :, cs], in_=pOT, func=AF.Copy), reads=[bpOT], writes=[b_obT])
        for h in range(8):
            store(OB_d[h], obT[:, h, :], b_obT, dOB)
        K.barrier()
    if stop_after == "C":
        return finish()

    with ExitStack() as st:
        wstage, b_wstage = sb(st, "wstageD", [128, 8, D])
        wpa, b_wpa = sb(st, "wpa_bf", [64, 8, D], BF16)
        wpb, b_wpb = sb(st, "wpb_bf", [64, 8, D], BF16)
        wo, b_wo = sb(st, "wo_bf", [128, 8, D], BF16)
        load(wstage[0:64], w_pa_d, b_wstage)
        K.op("pool", lambda e: e.tensor_copy(out=wpa[:], in_=wstage[0:64]), reads=[b_wstage], writes=[b_wpa])
        load(wstage[0:64], w_pb_d, b_wstage)
        K.op("pool", lambda e: e.tensor_copy(out=wpb[:], in_=wstage[0:64]), reads=[b_wstage], writes=[b_wpb])
        load(wstage[:], w_o_d.rearrange("(k p) n -> p k n", p=128), b_wstage)
        K.op("pool", lambda e: e.tensor_copy(out=wo[:], in_=wstage[:]), reads=[b_wstage], writes=[b_wo])
        oa_ss = [sb(st, "oa_s%d" % i, [64, 8, 512], BF16) for i in range(2)]
        ob_ss = [sb(st, "ob_s%d" % i, [64, 8, 512], BF16) for i in range(2)]
        ga_ss = [sb(st, "ga_s%d" % i, [128, 512], BF16) for i in range(2)]
        gb_ss = [sb(st, "gb_s%d" % i, [128, 512], BF16) for i in range(2)]
        mrgs = [sb(st, "mrg%d" % i, [128, 8, 512], BF16) for i in range(2)]
        m1, b_m1 = sb(st, "m1", [128, 512])
        m2, b_m2 = sb(st, "m2", [128, 512])
        xds = [sb(st, "xd%d" % i, [128, D]) for i in range(2)]
        hds = [sb(st, "hd%d" % i, [128, D]) for i in range(2)]
        ng = 0
        nt_ = 0
        for sc in range(NS):
            tok = slice(sc * 512, (sc + 1) * 512)
            oa_s, b_oas = oa_ss[sc % 2]
            ob_s, b_obs = ob_ss[sc % 2]
            mrg, b_mrg = mrgs[sc % 2]
            load(oa_s[:], OA_d[:, :, tok].rearrange("h p t -> p h t"), b_oas, dOA)
            load(ob_s[:], OB_d[:, :, tok].rearrange("h p t -> p h t"), b_obs, dOB)
            for nn in range(8):
                ga_s, b_gas = ga_ss[ng % 2]
                gb_s, b_gbs = gb_ss[ng % 2]
                (pa, pba), (pb_, pbb) = PS[(ng % 2) * 2], PS[(ng % 2) * 2 + 1]
                ng += 1
                load(ga_s[:], GATE_d[nn][:, tok], b_gas, dGATE)
                load(gb_s[:], GATE_d[8 + nn][:, tok], b_gbs, dGATE)
                for hh in range(8):
                    K.op("pe", lambda e: e.matmul(pa[:, :], lhsT=wpa[:, hh, nn * 128:(nn + 1) * 128], rhs=oa_s[:, hh, :],
                                                  start=(hh == 0), stop=(hh == 7)), reads=[b_wpa, b_oas], writes=[pba])
                for hh in range(8):
                    K.op("pe", lambda e: e.matmul(pb_[:, :], lhsT=wpb[:, hh, nn * 128:(nn + 1) * 128], rhs=ob_s[:, hh, :],
                                                  start=(hh == 0), stop=(hh == 7)), reads=[b_wpb, b_obs], writes=[pbb])
                K.op("dve", lambda e: e.tensor_tensor(out=m1[:], in0=pa[:, :], in1=ga_s[:], op=ALU.mult), reads=[pba, b_gas], writes=[b_m1])
                K.op("dve", lambda e: e.tensor_tensor(out=m2[:], in0=pb_[:, :], in1=gb_s[:], op=ALU.mult), reads=[pbb, b_gbs], writes=[b_m2])
                K.op("pool", lambda e: e.tensor_tensor(out=mrg[:, nn, :], in0=m1[:], in1=m2[:], op=ALU.add), reads=[b_m1, b_m2], writes=[b_mrg])
            for tt in range(4):
                ti = sc * 4 + tt
                tk = slice(ti * 128, (ti + 1) * 128)
                xd, b_xd = xds[nt_ % 2]
                hd, b_hd = hds[nt_ % 2]
                nt_ += 1
                load(xd[:], x_d[tk, :], b_xd)
                for mh in range(2):
                    py, pby = PS[4 + mh]
                    for nn in range(8):
                        K.op("pe", lambda e: e.matmul(py[:, :], lhsT=mrg[:, nn, tt * 128:(tt + 1) * 128], rhs=wo[:, nn, mh * 512:(mh + 1) * 512],
                                                      start=(nn == 0), stop=(nn == 7)), reads=[b_mrg, b_wo], writes=[pby])
                    K.op("dve", lambda e: e.tensor_tensor(out=hd[:, mh * 512:(mh + 1) * 512], in0=py[:, :], in1=modbc[:, 0, mh * 512:(mh + 1) * 512],
                                                          op=ALU.mult), reads=[pby, b_modbc], writes=[b_hd])
                K.op("pool", lambda e: e.tensor_tensor(out=hd[:], in0=hd[:], in1=xd[:], op=ALU.add), reads=[b_hd, b_xd], writes=[b_hd])
                store(H_d[tk, :], hd[:], b_hd, dH)
        K.barrier()
    if stop_after == "D":
        return finish()

    with ExitStack() as st:
        fnw, b_fnw = sb(st, "fnw", [128, D])
        load(fnw[:], fnw_d, b_fnw)
        subT, b_subT = sb(st, "subT", [64, 16, 128])
        load(subT[:], subT_d, b_subT)
        wq, b_wq = sb(st, "wq_bf", [128, 8, D], BF16)
        with ExitStack() as st2:
            wstage, b_wstage = sb(st2, "wstageE", [128, 8, D])
            load(wstage[:], wq_d.rearrange("(k p) n -> p k n", p=128), b_wstage)
            K.op("pool", lambda e: e.tensor_copy(out=wq[:], in_=wstage[:]), reads=[b_wstage], writes=[b_wq])
            K.barrier()
        hts = [sb(st, "ht%d" % i, [128, D]) for i in range(2)]
        n2, b_n2 = sb(st, "n2", [128, D])
        n2T, b_n2T = sb(st, "n2T", [128, 8, 128], BF16)
        qpT, b_qpT = sb(st, "qpT", [64, 16, 128])
        s_sb, b_ssb = sb(st, "s_sb", [128, 16, 128])
        v16, b_v16 = sb(st, "v16", [128, 16, 16])
        i16, b_i16 = sb(st, "i16", [128, 16, 16], U32)
        i16f, b_i16f = sb(st, "i16f", [128, 16, 16])
        wk2, b_wk2 = sb(st, "wk2", [128, 128])
        cand, b_cand = sb(st, "cand", [128, 8, 16, 16])
        eid, b_eid = sb(st, "eid", [128, 8, 16, 16])
        wk3, b_wk3 = sb(st, "wk3", [128, 256])
        jk3, b_jk3 = sb(st, "jk3", [128, 256])
        vals, b_vals = sb(st, "vals", [128, 8, 16])
        eids, b_eids = sb(st, "eids", [128, 128])
        idx, b_idx = sb(st, "idx", [128, 128], I32)
        gl, b_gl = sb(st, "gl", [128, 8, 16])
        gs, b_gs = sb(st, "gs", [128, 16])
        av, b_av = sb(st, "av", [128, 128])
        coef, b_coef = sb(st, "coef", [128, 128])
        acc, b_acc = sb(st, "acc", [128, D])
        jk, b_jk = sb(st, "jkE", [128, D])
        stE, b_stE = sb(st, "stE", [128, 8])
        outs = [sb(st, "outt%d" % i, [128, D]) for i in range(2)]
        NG = 16
        gbuf = [sb(st, "gath%d" % i, [128, 2 * D], BF16) for i in range(NG)]
        tvs = [sb(st, "tv%d" % i, [128, D], BF16) for i in range(3)]
        coefs = [sb(st, "coefg%d" % i, [128, 8]) for i in range(2)]
        ident_bf, b_identbf = sb(st, "ident_bf", [128, 128], BF16)
        K.op("pool", lambda e: e.tensor_copy(out=ident_bf[:], in_=ident[:]), reads=[b_ident], writes=[b_identbf])
        n2s = [sb(st, "n2_%d" % i, [128, D]) for i in range(2)]
        idxs = [sb(st, "idx_%d" % i, [128, 128], I32) for i in range(2)]
        gls = [sb(st, "gl_%d" % i, [128, 8, 16]) for i in range(2)]
        jkB, b_jkB = sb(st, "jkB", [128, D], BF16)
        stB, b_stB = sb(st, "stB", [128, 8])
        gctr = {"n": 0}

        def front(i):
            n2, b_n2 = n2s[i % 2]
            idx, b_idx = idxs[i % 2]
            gl, b_gl = gls[i % 2]
            tk = slice(i * 128, (i + 1) * 128)
            ht, b_ht = hts[i % 2]
            ot, b_ot = outs[i % 2]
            load(ht[:], H_d[tk, :], b_ht, dH)
            K.op("act", lambda e: e.activation(out=jk[:], in_=ht[:], func=AF.Square, accum_out=stE[:, 0:1]), reads=[b_ht], writes=[b_jk, b_stE])
            K.op("act", lambda e: e.activation(out=stE[:, 1:2], in_=stE[:, 0:1], func=AF.Sqrt, bias=EPS, scale=1.0 / D), reads=[b_stE], writes=[b_stE])
            K.op("dve", lambda e: e.reciprocal(out=stE[:, 2:3], in_=stE[:, 1:2]), reads=[b_stE], writes=[b_stE])
            K.op("dve", lambda e: e.scalar_tensor_tensor(out=n2[:], in0=ht[:], scalar=stE[:, 2:3], in1=modbc[:, 2, :], op0=ALU.mult, op1=ALU.mult),
                 reads=[b_ht, b_stE, b_modbc], writes=[b_n2])
            K.op("pool", lambda e: e.tensor_tensor(out=n2[:], in0=n2[:], in1=modbc[:, 1, :], op=ALU.add), reads=[b_n2, b_modbc], writes=[b_n2])
            for k in range(8):
                pt, pb = PS[k // 4]
                K.op("pe", lambda e: e.transpose(pt[:, (k % 4) * 128:(k % 4 + 1) * 128], n2[:, k * 128:(k + 1) * 128], ident[:]),
                     reads=[b_n2, b_ident], writes=[pb])
            for k2 in range(2):
                pt, pb = PS[k2]
                K.op("act", lambda e: e.activation(out=n2T[:, k2 * 4:(k2 + 1) * 4, :].rearrange("p a b -> p (a b)"), in_=pt[:, :], func=AF.Copy),
                     reads=[pb], writes=[b_n2T])
            for hp in range(16):
                pq, pbq = PS[2 + (hp // 4) % 2]
                c0 = (hp % 4) * 128
                for k in range(8):
                    K.op("pe", lambda e: e.matmul(pq[0:64, c0:c0 + 128], lhsT=wq[:, k, hp * 64:(hp + 1) * 64], rhs=n2T[:, k, :],
                                                  start=(k == 0), stop=(k == 7)), reads=[b_wq, b_n2T], writes=[pbq])
                if hp % 4 == 3:
                    g4 = hp // 4
                    K.op("act", lambda e: e.activation(out=qpT[:, g4 * 4:(g4 + 1) * 4, :].rearrange("p a b -> p (a b)"), in_=pq[0:64, :], func=AF.Copy),
                         reads=[pbq], writes=[b_qpT])
            for hp in range(16):
                psc, pbsc = PS[4 + (hp // 4) % 2]
                c0 = (hp % 4) * 128
                K.op("pe", lambda e: e.matmul(psc[:, c0:c0 + 128], lhsT=qpT[:, hp, :], rhs=subT[:, hp, :], start=True, stop=True),
                     reads=[b_qpT, b_subT], writes=[pbsc])
                if hp % 4 == 3:
                    g4 = hp // 4
                    K.op("act", lambda e: e.activation(out=s_sb[:, g4 * 4:(g4 + 1) * 4, :].rearrange("p a b -> p (a b)"), in_=psc[:, :], func=AF.Copy),
                         reads=[pbsc], writes=[b_ssb])
            for hp in range(16):
                K.op("dve", lambda e: e.max(out=v16[:, hp, 0:8], in_=s_sb[:, hp, :]), reads=[b_ssb], writes=[b_v16])
                K.op("dve", lambda e: e.max_index(out=i16[:, hp, 0:8], in_max=v16[:, hp, 0:8], in_values=s_sb[:, hp, :]),
                     reads=[b_ssb, b_v16], writes=[b_i16])
                K.op("dve", lambda e: e.match_replace(out=wk2[:], in_to_replace=v16[:, hp, 0:8], in_values=s_sb[:, hp, :], imm_value=NEG),
                     reads=[b_ssb, b_v16], writes=[b_wk2])
                K.op("dve", lambda e: e.max(out=v16[:, hp, 8:16], in_=wk2[:]), reads=[b_wk2], writes=[b_v16])
                K.op("dve", lambda e: e.max_index(out=i16[:, hp, 8:16], in_max=v16[:, hp, 8:16], in_values=wk2[:]),
                     reads=[b_wk2, b_v16], writes=[b_i16])
            K.op("dve", lambda e: e.tensor_copy(out=i16f[:], in_=i16[:]), reads=[b_i16], writes=[b_i16f])
            v16v = v16[:].rearrange("p (h two) k -> p h two k", two=2)
            i16v = i16f[:].rearrange("p (h two) k -> p h two k", two=2)
            K.op("dve", lambda e: e.tensor_scalar(out=i16v[:, :, 0, :], in0=i16v[:, :, 0, :], scalar1=128.0, scalar2=None, op0=ALU.mult),
                 reads=[b_i16f], writes=[b_i16f])
            for a in range(16):
                K.op("dve", lambda e: e.tensor_tensor(out=cand[:, :, a, :], in0=v16v[:, :, 1, :], in1=v16v[:, :, 0, a:a + 1].to_broadcast([128, 8, 16]),
                                                      op=ALU.add), reads=[b_v16], writes=[b_cand])
                K.op("pool", lambda e: e.tensor_tensor(out=eid[:, :, a, :], in0=i16v[:, :, 1, :], in1=i16v[:, :, 0, a:a + 1].to_broadcast([128, 8, 16]),
                                                       op=ALU.add), reads=[b_i16f], writes=[b_eid])
            for hh in range(8):
                ch = cand[:, hh].rearrange("p a b -> p (a b)")
                eh = eid[:, hh].rearrange("p a b -> p (a b)")
                K.op("dve", lambda e: e.max(out=vals[:, hh, 0:8], in_=ch), reads=[b_cand], writes=[b_vals])
                K.op("dve", lambda e: e.match_replace(out=wk3[:], in_to_replace=vals[:, hh, 0:8], in_values=ch, imm_value=NEG),
                     reads=[b_cand, b_vals], writes=[b_wk3])
                K.op("dve", lambda e: e.max(out=vals[:, hh, 8:16], in_=wk3[:]), reads=[b_wk3], writes=[b_vals])
                for j in range(16):
                    K.op("dve", lambda e: e.scalar_tensor_tensor(out=jk3[:], in0=ch, scalar=vals[:, hh, j:j + 1], in1=eh, op0=ALU.is_equal, op1=ALU.mult,
                                                                 accum_out=eids[:, hh * 16 + j:hh * 16 + j + 1]),
                         reads=[b_cand, b_vals, b_eid], writes=[b_jk3, b_eids])
            K.op("dve", lambda e: e.tensor_scalar(out=eids[:], in0=eids[:], scalar1=16383.0, scalar2=0.0, op0=ALU.min, op1=ALU.max),
                 reads=[b_eids], writes=[b_eids])
            K.op("dve", lambda e: e.tensor_copy(out=idx[:], in_=eids[:]), reads=[b_eids], writes=[b_idx])
            K.op("dve", lambda e: e.tensor_tensor(out=gl[:], in0=vals[:], in1=vals[:, :, 0:1].to_broadcast([128, 8, 16]), op=ALU.subtract),
                 reads=[b_vals], writes=[b_gl])
            K.op("act", lambda e: e.activation(out=gl[:], in_=gl[:], func=AF.Exp), reads=[b_gl], writes=[b_gl])
            K.op("dve", lambda e: e.tensor_reduce(out=gs[:, 0:8], in_=gl[:], axis=AX.X, op=ALU.add), reads=[b_gl], writes=[b_gs])
            K.op("dve", lambda e: e.reciprocal(out=gs[:, 8:16], in_=gs[:, 0:8]), reads=[b_gs], writes=[b_gs])
            K.op("dve", lambda e: e.tensor_tensor(out=gl[:], in0=gl[:], in1=gs[:, 8:16].rearrange("p (h o) -> p h o", o=1).to_broadcast([128, 8, 16]),
                                                  op=ALU.mult), reads=[b_gl, b_gs], writes=[b_gl])

        def back(i):
            n2, b_n2 = n2s[i % 2]
            idx, b_idx = idxs[i % 2]
            gl, b_gl = gls[i % 2]
            tk = slice(i * 128, (i + 1) * 128)
            ht, b_ht = hts[i % 2]
            ot, b_ot = outs[i % 2]
            for grp in range(16):
                gsl = []
                for s_ in range(8):
                    j = grp * 8 + s_
                    gb_, bgb = gbuf[gctr["n"] % NG]
                    gctr["n"] += 1
                    gsl.append((gb_, bgb))
                    K.dma(lambda e: e.indirect_dma_start(out=gb_[:], out_offset=None, in_=UV_d,
                                                         in_offset=bass.IndirectOffsetOnAxis(ap=idx[:, j:j + 1], axis=0)),
                          bgb, reads=[b_idx, dUV], writes=[bgb], q="pool")
                    K.op("dve", lambda e: e.scalar_tensor_tensor(out=jkB[:], in0=gb_[:, 0:D], scalar=1.0, in1=n2[:], op0=ALU.mult, op1=ALU.mult,
                                                                 accum_out=av[:, j:j + 1]), reads=[bgb, b_n2], writes=[b_jkB, b_av])
                g8 = slice(grp * 8, (grp + 1) * 8)
                cf, b_cf = coefs[grp % 2]
                K.op("act", lambda e: e.activation(out=cf[:], in_=av[:, g8], func=AF.Gelu), reads=[b_av], writes=[b_cf])
                K.op("dve", lambda e: e.tensor_tensor(out=cf[:], in0=cf[:], in1=gl[:].rearrange("p h k -> p (h k)")[:, g8], op=ALU.mult),
                     reads=[b_cf, b_gl], writes=[b_cf])
                for s_ in range(8):
                    j = grp * 8 + s_
                    gb_, bgb = gsl[s_]
                    tv, b_tv = tvs[j % 3]
                    K.op("act", lambda e: e.activation(out=tv[:], in_=gb_[:, D:2 * D], func=AF.Copy, scale=cf[:, s_:s_ + 1]),
                         reads=[bgb, b_cf], writes=[b_tv])
                    for mh in range(2):
                        K.op("pe", lambda e: e.matmul(PS[6 + mh][0][:, :], lhsT=ident_bf[:], rhs=tv[:, mh * 512:(mh + 1) * 512],
                                                      start=(j == 0), stop=(j == 127)), reads=[b_tv, b_identbf], writes=[PS[6 + mh][1]])
            for mh in range(2):
                K.op("dve", lambda e: e.tensor_tensor(out=acc[:, mh * 512:(mh + 1) * 512], in0=PS[6 + mh][0][:, :], in1=modbc[:, 3, mh * 512:(mh + 1) * 512],
                                                      op=ALU.mult), reads=[PS[6 + mh][1], b_modbc], writes=[b_acc])
            K.op("pool", lambda e: e.tensor_tensor(out=acc[:], in0=acc[:], in1=ht[:], op=ALU.add), reads=[b_acc, b_ht], writes=[b_acc])
            K.op("act", lambda e: e.activation(out=jkB[:], in_=acc[:], func=AF.Square, accum_out=stB[:, 4:5]), reads=[b_acc], writes=[b_jkB, b_stB])
            K.op("act", lambda e: e.activation(out=stB[:, 5:6], in_=stB[:, 4:5], func=AF.Sqrt, bias=EPS, scale=1.0 / D), reads=[b_stB], writes=[b_stB])
            K.op("dve", lambda e: e.reciprocal(out=stB[:, 6:7], in_=stB[:, 5:6]), reads=[b_stB], writes=[b_stB])
            K.op("dve", lambda e: e.scalar_tensor_tensor(out=ot[:], in0=acc[:], scalar=stB[:, 6:7], in1=fnw[:], op0=ALU.mult, op1=ALU.mult),
                 reads=[b_acc, b_stB, b_fnw], writes=[b_ot])
            store(out_d[tk, :], ot[:], b_ot, dOUT)

        front(0)
        for i in range(NT):
            if i + 1 < NT:
                front(i + 1)
            back(i)
    return finish()


_NC_CACHE = {}


def kernel(**inputs):
    S = inputs["x"].shape[1]
    B = inputs["x"].shape[0]
    if S not in _NC_CACHE:
        _NC_CACHE[S] = build_nc(S)[0]
    nc = _NC_CACHE[S]
    in_maps = [host_layout(inputs, b, S) for b in range(B)]
    res = run_bass_kernel_spmd(nc, in_maps, core_ids=list(range(B)))
    return np.stack([np.asarray(r["out"], dtype=np.float32) for r in res.results], axis=0)
```
